# Optimizing a Trainium2 kernel written in Bass

```python
import math
import jax, jax.numpy as jnp
from jax import lax
import numpy as np

D_MODEL = 1024
BATCH = 4
SEQ = 8192
DEPTH = 1

HG_HEADS = 4
HG_HEAD_K = 128
HG_HEAD_V = 128
HG_KEY = HG_HEADS * HG_HEAD_K
HG_VAL = HG_HEADS * HG_HEAD_V
GLA_HEADS = 4
GLA_HEAD_K = 64
GLA_HEAD_V = 128
GLA_KEY = GLA_HEADS * GLA_HEAD_K
GLA_VAL = GLA_HEADS * GLA_HEAD_V
GLA_GATE_RANK = 16
GLA_GATE_NORMALIZER = 16.0
MIX_WIDTH = HG_VAL + GLA_VAL
IN_SPLITS = (HG_KEY, HG_KEY, HG_VAL, HG_VAL, GLA_KEY, GLA_KEY, GLA_VAL, GLA_GATE_RANK, GLA_VAL)
IN_WIDTH = sum(IN_SPLITS)
CHUNK = 64
PEER_HEADS = 8
PEER_N_KEYS = 128
PEER_N_EXPERTS = PEER_N_KEYS * PEER_N_KEYS
PEER_QUERY_DIM = 256
PEER_HALF = PEER_QUERY_DIM // 2
PEER_TOPK = 16
PEER_BLOCK = 128
EPS = 1e-6

kernel_name = "hymba_hgrn2_gla_peer_block"


def rms_norm(x, g):
    xf = x.astype(jnp.float32)
    y = xf * lax.rsqrt(jnp.mean(xf * xf, axis=-1, keepdims=True) + EPS)
    return (y * g.astype(jnp.float32)).astype(x.dtype)


def split_heads(a, n_heads):
    b, t, w = a.shape
    return a.reshape(b, t, n_heads, w // n_heads).transpose(0, 2, 1, 3)


def merge_heads(a):
    b, n, t, d = a.shape
    return a.transpose(0, 2, 1, 3).reshape(b, t, n * d)


def chunk_gated_linear_attention(q, k, v, log_g):
    b, h, t, dk = q.shape
    dv = v.shape[-1]
    n_chunks = t // CHUNK

    def to_chunks(a):
        return a.astype(jnp.float32).reshape(b, h, n_chunks, CHUNK, a.shape[-1]).transpose(2, 0, 1, 3, 4)

    qc, kc, vc, gc = to_chunks(q), to_chunks(k), to_chunks(v), to_chunks(log_g)
    causal = jnp.tril(jnp.ones((CHUNK, CHUNK), dtype=bool))[:, :, None]

    def step(state, inp):
        qb, kb, vb, gb = inp
        cum = jnp.cumsum(gb, axis=2)
        rel = cum[:, :, :, None, :] - cum[:, :, None, :, :]
        decay = jnp.exp(jnp.where(causal, rel, -jnp.inf))
        scores = jnp.einsum('bhid,bhjd,bhijd->bhij', qb, kb, decay)
        out = (jnp.einsum('bhij,bhjv->bhiv', scores, vb)
               + jnp.einsum('bhid,bhdv->bhiv', qb * jnp.exp(cum), state))
        last = cum[:, :, -1:, :]
        state = (jnp.exp(last[:, :, 0, :])[..., None] * state
                 + jnp.einsum('bhjd,bhjv->bhdv', kb * jnp.exp(last - cum), vb))
        return state, out

    s0 = jnp.zeros((b, h, dk, dv), jnp.float32)
    _, o = lax.scan(step, s0, (qc, kc, vc, gc))
    return o.transpose(1, 2, 0, 3, 4).reshape(b, h, t, dv)


def hgrn2_mixer(q, f, i, gate, lower_bound, norm_g):
    q = jax.nn.silu(q)
    forget = lower_bound + (1.0 - lower_bound) * jax.nn.sigmoid(f.astype(jnp.float32))
    key = 1.0 - forget
    log_f = jnp.log(forget)
    o = chunk_gated_linear_attention(split_heads(q, HG_HEADS), split_heads(key, HG_HEADS),
                                     split_heads(i, HG_HEADS), split_heads(log_f, HG_HEADS))
    o = merge_heads(rms_norm(o, norm_g))
    return (o * jax.nn.silu(gate.astype(jnp.float32))).astype(gate.dtype)


def gla_mixer(q, k, v, gate_low, gate, w_gate_up, b_gate, norm_g):
    log_g = jax.nn.log_sigmoid((gate_low @ w_gate_up + b_gate).astype(jnp.float32)) / GLA_GATE_NORMALIZER
    q = q * (GLA_HEAD_K ** -0.5)
    o = chunk_gated_linear_attention(split_heads(q, GLA_HEADS), split_heads(k, GLA_HEADS),
                                     split_heads(v, GLA_HEADS), split_heads(log_g, GLA_HEADS))
    o = merge_heads(rms_norm(o, norm_g))
    return (o * jax.nn.silu(gate.astype(jnp.float32))).astype(gate.dtype)


def peer_ffn(xn, w_q, sub_keys, u, v):
    b, t, d = xn.shape
    blocks = xn.reshape(-1, PEER_BLOCK, d)

    def block(xb):
        q = (xb @ w_q).reshape(PEER_BLOCK, PEER_HEADS, 2, PEER_HALF)
        s = jnp.einsum('thpd,hpnd->thpn', q, sub_keys).astype(jnp.float32)
        s_top, i_top = lax.top_k(s, PEER_TOPK)
        cand = s_top[:, :, 0, :, None] + s_top[:, :, 1, None, :]
        cand_idx = i_top[:, :, 0, :, None] * PEER_N_KEYS + i_top[:, :, 1, None, :]
        cand = cand.reshape(PEER_BLOCK, PEER_HEADS, PEER_TOPK * PEER_TOPK)
        cand_idx = cand_idx.reshape(PEER_BLOCK, PEER_HEADS, PEER_TOPK * PEER_TOPK)
        best, pos = lax.top_k(cand, PEER_TOPK)
        expert = jnp.take_along_axis(cand_idx, pos, axis=-1)
        g = jax.nn.softmax(best, axis=-1)
        act = jax.nn.gelu(jnp.einsum('td,thkd->thk', xb, u[expert]), approximate=False)
        return jnp.einsum('thk,thkd->td', (g * act).astype(xb.dtype), v[expert])

    return lax.map(block, blocks).reshape(b, t, d)


def setup_inputs(seed: int = 0) -> dict:
    key = jax.random.key(seed)
    ks = jax.random.split(key, 16)
    nrm = jax.random.normal
    L = DEPTH
    return {
        "x": nrm(ks[0], (BATCH, SEQ, D_MODEL), jnp.float32),
        "norm1_g": 1.0 + 0.02 * nrm(ks[1], (L, D_MODEL), jnp.float32),
        "w_in": nrm(ks[2], (L, D_MODEL, IN_WIDTH), jnp.float32) * D_MODEL ** -0.5,
        "hg_lower_logits": 0.1 * nrm(ks[3], (L + 1, HG_KEY), jnp.float32),
        "hg_norm_g": 1.0 + 0.02 * nrm(ks[4], (L, HG_HEAD_V), jnp.float32),
        "gla_w_gate_up": nrm(ks[5], (L, GLA_GATE_RANK, GLA_KEY), jnp.float32) * GLA_GATE_RANK ** -0.5,
        "gla_b_gate": 0.1 * nrm(ks[6], (L, GLA_KEY), jnp.float32),
        "gla_norm_g": 1.0 + 0.02 * nrm(ks[7], (L, GLA_HEAD_V), jnp.float32),
        "w_out": nrm(ks[8], (L, MIX_WIDTH, D_MODEL), jnp.float32) * MIX_WIDTH ** -0.5,
        "norm2_g": 1.0 + 0.02 * nrm(ks[9], (L, D_MODEL), jnp.float32),
        "peer_w_q": nrm(ks[10], (L, D_MODEL, PEER_HEADS * PEER_QUERY_DIM), jnp.float32) * D_MODEL ** -0.5,
        "peer_sub_keys": nrm(ks[11], (L, PEER_HEADS, 2, PEER_N_KEYS, PEER_HALF), jnp.float32) * PEER_HALF ** -0.5,
        "peer_u": nrm(ks[12], (L, PEER_N_EXPERTS, D_MODEL), jnp.float32) * D_MODEL ** -0.5,
        "peer_v": nrm(ks[13], (L, PEER_N_EXPERTS, D_MODEL), jnp.float32) * 0.3,
        "norm_f_g": 1.0 + 0.02 * nrm(ks[14], (D_MODEL,), jnp.float32),
    }


def reference(x, norm1_g, w_in, hg_lower_logits, hg_norm_g, gla_w_gate_up, gla_b_gate, gla_norm_g,
              w_out, norm2_g, peer_w_q, peer_sub_keys, peer_u, peer_v, norm_f_g):
    lower_bounds = jnp.cumsum(jax.nn.softmax(hg_lower_logits.astype(jnp.float32), axis=0), axis=0)
    offsets = [int(o) for o in np.cumsum(IN_SPLITS)[:-1]]
    h = x
    for layer in range(DEPTH):
        xn = rms_norm(h, norm1_g[layer])
        proj = xn @ w_in[layer]
        hq, hf, hi, hgate, gq, gk, gv, glow, ggate = jnp.split(proj, offsets, axis=-1)
        o_hgrn = hgrn2_mixer(hq, hf, hi, hgate, lower_bounds[layer], hg_norm_g[layer])
        o_gla = gla_mixer(gq, gk, gv, glow, ggate, gla_w_gate_up[layer], gla_b_gate[layer], gla_norm_g[layer])
        mixed = jnp.concatenate([o_hgrn, o_gla], axis=-1)
        h = h + (mixed @ w_out[layer]).astype(h.dtype)
        hn = rms_norm(h, norm2_g[layer])
        h = h + peer_ffn(hn, peer_w_q[layer], peer_sub_keys[layer], peer_u[layer], peer_v[layer]).astype(h.dtype)
    return rms_norm(h, norm_f_g)
```

```python
import numpy as np
from contextlib import ExitStack
import concourse.bass as bass
import concourse.mybir as mybir
from concourse.bass_utils import run_bass_kernel_spmd

F32, BF16 = mybir.dt.float32, mybir.dt.bfloat16
AF = mybir.ActivationFunctionType
ALU = mybir.AluOpType
AX = mybir.AxisListType
EPS = 1e-6
STOP = 3
LIM = 99
EXP = 0
NEG = -1.0e30


class Sched:
    def __init__(self):
        self.q = {e: [] for e in ("pe", "act", "dve", "pool", "sp")}
        self.cnt = {e: 0 for e in self.q}
        self.dcnt = {}
        self.res = {}
        self.waited = {e: {} for e in self.q}

    def _deps(self, eng, reads, writes):
        deps = []
        for r in reads:
            st = self.res.get(r)
            if st and st[0]:
                deps.append(st[0])
        for w in writes:
            st = self.res.get(w)
            if st:
                if st[0]:
                    deps.append(st[0])
                deps.extend(st[1])
        if eng == "pe":
            deps = [d for d in deps if d[0] != "c_pe"]
        return deps

    def _commit(self, reads, writes, ticket):
        for r in reads:
            self.res.setdefault(r, [None, []])[1].append(ticket)
        for w in writes:
            self.res[w] = [ticket, []]

    def _waits(self, eng, deps):
        wl = self.waited[eng]
        need = {}
        for s, v in deps:
            if wl.get(s, 0) < v:
                need[s] = max(need.get(s, 0), v)
        for s, v in need.items():
            wl[s] = v
            self.q[eng].append(("wait", s, v))

    def op(self, eng, fn, reads=(), writes=(), sig=True):
        self._waits(eng, self._deps(eng, reads, writes))
        if sig:
            self.cnt[eng] += 1
            ticket = ("c_" + eng, self.cnt[eng])
            self.q[eng].append(("op", fn, "c_" + eng))
        else:
            ticket = ("c_" + eng, self.cnt[eng] + 1)
            self.q[eng].append(("op", fn, None))
        self._commit(reads, writes, ticket)

    def dma(self, eng, fn, key, reads=(), writes=()):
        deps = self._deps(eng, reads, writes)
        prev = self.dcnt.get(key, 0)
        if prev:
            deps.append(("d_" + key, prev * 16))
        self._waits(eng, deps)
        self.dcnt[key] = prev + 1
        self.q[eng].append(("dma", fn, "d_" + key))
        self._commit(reads, writes, ("d_" + key, (prev + 1) * 16))

    def barrier(self):
        allsem = [("c_" + e, c) for e, c in self.cnt.items() if c] + \
                 [("d_" + k, c * 16) for k, c in self.dcnt.items()]
        for e in self.q:
            self._waits(e, allsem)
        self.res = {}

    def sem_names(self):
        return ["c_" + e for e in self.cnt] + ["d_" + k for k in self.dcnt]


def build_program(NPRE, NMAIN, TB, debug=False):
    nc = bass.Bass("TRN2", target_bir_lowering=False)
    NT = NPRE + NMAIN
    NB = NMAIN // TB
    D = 1024

    def din(name, shape, dt=F32):
        return nc.dram_tensor(name, list(shape), dt, kind="ExternalInput").ap()

    xa = din("xa", [NT * 128, D])
    w_in = din("w_in", [D, 3600])
    glowT = din("glowT", [16, D])
    w_up = din("w_up", [16, 256])
    cols = din("cols", [128, 16])
    g1r_d = din("g1r", [128, D])
    g2r_d = din("g2r", [128, D])
    gfr_d = din("gfr", [128, D])
    w_out = din("w_out", [D, D])
    w_q = din("w_q", [D, 2048])
    kt_d = din("kt", [128, 2048])
    uT = din("uT", [D, 16384])
    v_d = din("v", [16384, D])
    cst_d = din("cst", [128, 770])
    out_d = nc.dram_tensor("out", [NMAIN * 128, D], F32, kind="ExternalOutput").ap()
    hs = nc.dram_tensor("hs", [NMAIN, 128, D], F32).ap()
    hnTs = nc.dram_tensor("hnTs", [NMAIN, 128, D], BF16).ap()
    ABs = nc.dram_tensor("ABs", [NMAIN, 128, 2048], F32).ap()
    ths = nc.dram_tensor("ths", [NMAIN, 128, 8], F32).ap()
    dbg = {}
    if debug:
        dbg["h"] = nc.dram_tensor("dbg_h", [NMAIN * 128, D], F32, kind="ExternalOutput").ap()

    S = Sched()
    stack = ExitStack()
    NW = 40000
    POOL = stack.enter_context(nc.sbuf_tensor("pool", [128, NW], F32))
    PS = stack.enter_context(nc.psum_tensor("ps", [128, 8, 512], F32))
    off = [0]

    def alloc(n, dt=F32, shape=None):
        nw = n if dt == F32 else (n + 1) // 2
        a = POOL[:, off[0]:off[0] + nw]
        off[0] += nw
        assert off[0] <= NW, ("sbuf overflow", off[0])
        if dt == BF16:
            a = a.bitcast(BF16)
        if shape is not None:
            names = " ".join("abc"[: len(shape)])
            kw = {"abc"[i]: shape[i] for i in range(len(shape) - 1)}
            a = a.rearrange("p (%s) -> p %s" % (names, names), **kw)
        return a

    def bank(k, dt=F32, shape=None):
        a = PS[:, k, :]
        if dt == BF16:
            a = a.bitcast(BF16)
        if shape is not None:
            names = " ".join("abc"[: len(shape)])
            kw = {"abc"[i]: shape[i] for i in range(len(shape) - 1)}
            a = a.rearrange("p (%s) -> p %s" % (names, names), **kw)
        return a

    def MM(out, lhsT, rhs, start, stop, reads, writes, sig):
        S.op("pe", lambda e: e.matmul(out, lhsT=lhsT, rhs=rhs, start=start, stop=stop), reads, writes, sig)

    def ACT(out, in_, func, reads, writes, bias=None, scale=None):
        kw = {}
        if bias is not None:
            kw["bias"] = bias
        if scale is not None:
            kw["scale"] = scale
        S.op("act", lambda e: e.activation(out=out, in_=in_, func=func, **kw), reads, writes)

    def TT(out, in0, in1, op, reads, writes, eng="dve"):
        S.op(eng, lambda e: e.tensor_tensor(out=out, in0=in0, in1=in1, op=op), reads, writes)

    def TS(out, in0, s1, s2, op0, op1, reads, writes):
        if op1 is None:
            S.op("dve", lambda e: e.tensor_scalar(out=out, in0=in0, scalar1=s1, scalar2=None, op0=op0), reads, writes)
        else:
            S.op("dve", lambda e: e.tensor_scalar(out=out, in0=in0, scalar1=s1, scalar2=s2, op0=op0, op1=op1), reads, writes)

    def STT(out, in0, scalar, in1, op0, op1, reads, writes):
        S.op("dve", lambda e: e.scalar_tensor_tensor(out=out, in0=in0, scalar=scalar, in1=in1, op0=op0, op1=op1), reads, writes)

    def CP(eng, out, in_, reads, writes):
        if eng == "act":
            S.op("act", lambda e: e.copy(out=out, in_=in_), reads, writes)
        else:
            S.op(eng, lambda e: e.tensor_copy(out, in_), reads, writes)

    def DMA(eng, out, in_, key, reads, writes):
        S.dma(eng, lambda e: e.dma_start(out=out, in_=in_), key, reads, writes)

    def bc(ap, shape, axis):
        return ap.unsqueeze(axis).to_broadcast(shape)

    def emit():
        sems = {n: stack.enter_context(nc.semaphore(n)) for n in S.sem_names()}
        with stack:
            with nc.Block() as block:
                def replay(name, e):
                    for it in S.q[name]:
                        if it[0] == "wait":
                            e.wait_ge(sems[it[1]], it[2])
                        else:
                            ins = it[1](e)
                            if it[2] is not None:
                                ins.then_inc(sems[it[2]], 16 if it[0] == "dma" else 1)

                @block.tensor
                def _(e):
                    replay("pe", e)

                @block.scalar
                def _(e):
                    replay("act", e)

                @block.vector
                def _(e):
                    replay("dve", e)

                @block.gpsimd
                def _(e):
                    replay("pool", e)

                @block.sync
                def _(e):
                    replay("sp", e)
        return nc

    cst = alloc(770)
    identb = alloc(128, BF16)
    colst = alloc(16)
    small = alloc(16)
    DMA("sp", cst, cst_d[:, :], "c0", [], ["cst"])
    DMA("pool", identb, cst_d[:, 0:128], "c1", [], ["identb"])
    DMA("sp", colst, cols[:, :], "c2", [], ["colst"])
    maskT = cst[:, 128:256]
    rpat = cst[:, 256:768]
    rm = cst[:, 768:770]
    S.op("dve", lambda e: e.memset(small[:, 6:7], 1.0), [], ["small"])
    S.op("dve", lambda e: e.memset(small[:, 7:8], EPS), ["small"], ["small"])
    onec = small[:, 6:7]
    TT(small[:, 8:12], colst[:, 4:8], colst[:, 0:4], ALU.subtract, ["colst", "small"], ["small"])
    ACT(small[:, 0:4], small[:, 8:12], AF.Sigmoid, ["small"], ["small"])
    TS(small[:, 4:6], colst[:, 8:10], -1.0, None, ALU.mult, None, ["colst", "small"], ["small"])
    omlc = small[:, 0:4]
    nbc = small[:, 4:6]
    gnc = colst[:, 10:12]
    base_off = off[0]

    def rstd_of(src, rs, n, tag, sq, st):
        TT(sq[:, 0:n], src, src, ALU.mult, [rs], ["sq"])
        S.op("dve", lambda e: e.reduce_sum(out=st[:, 1:2], in_=sq[:, 0:n], axis=AX.X), ["sq"], [tag])
        TS(st[:, 2:3], st[:, 1:2], 1.0 / n, small[:, 7:8] if False else EPS, ALU.mult, ALU.add, [tag], [tag])
        ACT(st[:, 3:4], st[:, 2:3], AF.Sqrt, [tag], [tag])
        S.op("dve", lambda e: e.reciprocal(out=st[:, 0:1], in_=st[:, 3:4]), [tag], [tag])

    win = alloc(8 * 3600, BF16, [8, 3600])
    wz = alloc(8 * 256, BF16, [8, 256])
    wout = alloc(8 * 1024, BF16, [8, 1024])
    g1r = alloc(1024)
    S32 = alloc(1024, F32, [8, 128])
    Sbf = alloc(1024, BF16, [8, 128])
    glT = alloc(1024)
    wupt = alloc(256)
    xt = [alloc(1024), alloc(1024)]
    sq = alloc(1024)
    st1 = alloc(8)
    st8 = alloc(40, F32, [5, 8])
    xn = alloc(1024, BF16)
    tT = alloc(1024, BF16, [8, 128])
    V = alloc(1024, BF16)
    gsil = alloc(1024)
    nsig = alloc(512, F32, [4, 128])
    kk = alloc(512, F32, [4, 128])
    lf = alloc(768, F32, [6, 128])
    cum = alloc(768, F32, [6, 128])
    ecum = alloc(768, F32, [6, 128])
    encum = alloc(768, F32, [6, 128])
    ez = alloc(256, F32, [2, 128])
    qtilT = alloc(768, BF16, [6, 128])
    ktilT = alloc(768, BF16, [6, 128])
    ktok = alloc(768, BF16, [6, 128])
    eclb = alloc(8)
    sT = alloc(1024, BF16, [8, 128])
    mixed = alloc(1024, BF16)
    qg = alloc(512, BF16, [4, 128])

    w_in_v = w_in.rearrange("(k p) c -> p k c", p=128)
    for k in range(8):
        DMA("pool", win[:, k, :], w_in_v[:, k, :], "w%d" % (k % 4), [], ["win"])
    DMA("pool", wout, w_out.rearrange("(k p) c -> p k c", p=128), "w0", [], ["wout"])
    DMA("sp", g1r, g1r_d[:, :], "c0", [], ["g1r"])
    DMA("sp", glT[0:16, :], glowT[:, :], "c2", [], ["glT"])
    DMA("sp", wupt[0:16, :], w_up[:, :], "c2", [], ["wupt"])
    for k in range(8):
        TS(wout[:, k, :], wout[:, k, :], gnc[:, (k // 4):(k // 4) + 1], None, ALU.mult, None, ["wout", "colst"], ["wout"])
    for k in range(8):
        b = bank(k // 2, F32, [2, 256])
        MM(b[:, k % 2, :], glT[0:16, k * 128:(k + 1) * 128], wupt[0:16, :], True, True,
           ["glT", "wupt"], ["ps%d" % (k // 2)], True)
    for kb in range(4):
        CP("dve", wz[:, 2 * kb:2 * kb + 2, :], bank(kb, F32, [2, 256]), ["ps%d" % kb], ["wz"])
    S.op("dve", lambda e: e.memset(S32, 0.0), [], ["S32"])
    S.op("dve", lambda e: e.memset(Sbf, 0.0), [], ["Sbf"])

    C_HQ, C_HF, C_HI, C_HG, C_GQ, C_GK, C_GV, C_GG = 0, 512, 1024, 1536, 2048, 2304, 2560, 3088

    def fm_block(dst, wsrc, col, wres, pres, last):
        for k in range(8):
            MM(dst, wsrc[:, k, col:col + 128], tT[:, k, :], k == 0, k == 7, [wres, "tT"], [pres], (k == 7) and last)

    def tm_block(bk, col, pres):
        for k in range(8):
            MM(bank(bk), tT[:, k, :], win[:, k, col:col + 512], k == 0, k == 7, ["win", "tT"], [pres], k == 7)

    if STOP == 0:
        S.barrier()
        return emit()
    for c in range(NT):
        main = c >= NPRE
        x_ = xt[c % 2]
        xr = "xt%d" % (c % 2)
        DMA("sp", x_, xa[c * 128:(c + 1) * 128, :], "x%d" % (c % 2), [], [xr])
        rstd_of(x_, xr, 1024, "st1", sq, st1)
        STT(xn, x_, st1[:, 0:1], g1r, ALU.mult, ALU.mult, [xr, "st1", "g1r"], ["xn"])
        for k in range(8):
            S.op("pe", lambda e, k=k: e.transpose(bank(0, BF16, [8, 128])[:, k, :], xn[:, k * 128:(k + 1) * 128], identb),
                 ["xn", "identb"], ["ps0"], k == 7)
        CP("act", tT, bank(0, BF16, [8, 128]), ["ps0"], ["tT"])
        if LIM == 1:
            continue
        tm_block(1, C_HI, "ps1")
        tm_block(2, C_GV, "ps2")
        CP("act", V[:, 0:512], bank(1), ["ps1"], ["V"])
        CP("act", V[:, 512:1024], bank(2), ["ps2"], ["V"])
        if main:
            tm_block(3, C_HG, "ps3")
            tm_block(4, C_GG, "ps4")
            ACT(gsil[:, 0:512], bank(3), AF.Silu, ["ps3"], ["gsil"])
            ACT(gsil[:, 512:1024], bank(4), AF.Silu, ["ps4"], ["gsil"])
        if LIM == 2:
            continue
        b5 = bank(5, F32, [4, 128])
        b6 = bank(6, F32, [4, 128])
        b7 = bank(7, F32, [4, 128])
        b1 = bank(1, F32, [4, 128])
        for b in range(4):
            fm_block(b5[:, b, :], win, C_HF + b * 128, "win", "ps5", b == 3)
        for b in range(2):
            fm_block(b6[:, b, :], win, C_GK + b * 128, "win", "ps6", (b == 1) and not main)
        for b in range(2):
            fm_block(b7[:, b, :], wz, b * 128, "wz", "ps7", b == 1)
        if main:
            for b in range(2):
                fm_block(b6[:, 2 + b, :], win, C_GQ + b * 128, "win", "ps6", b == 1)
            for b in range(4):
                fm_block(b1[:, b, :], win, C_HQ + b * 128, "win", "ps1", b == 3)
        if LIM == 3:
            continue
        ACT(nsig, b5, AF.Sigmoid, ["ps5"], ["nsig"], scale=-1.0)
        TT(kk, nsig, bc(omlc, [128, 4, 128], 2), ALU.mult, ["nsig", "small"], ["kk"])
        ACT(lf[:, 0:4, :], kk, AF.Ln, ["kk", "small"], ["lf"], bias=onec, scale=-1.0)
        for b in range(2):
            ACT(ez[:, b, :], b7[:, b, :], AF.Exp, ["ps7", "small"], ["ez"], bias=nbc[:, b:b + 1], scale=-1.0)
        ACT(lf[:, 4:6, :], ez, AF.Ln, ["ez", "small"], ["lf"], bias=onec, scale=1.0)
        lf2 = lf.rearrange("p a b -> p (a b)")
        cum2 = cum.rearrange("p a b -> p (a b)")
        S.op("dve", lambda e: e.tensor_tensor_scan(out=cum2[:, 0:512], data0=rpat, data1=lf2[:, 0:512], initial=0.0,
                                                   op0=ALU.mult, op1=ALU.add), ["lf", "cst"], ["cum"])
        S.op("dve", lambda e: e.tensor_tensor_scan(out=cum2[:, 512:768], data0=rpat[:, 0:256], data1=lf2[:, 512:768],
                                                   initial=0.0, op0=ALU.mult, op1=ALU.add), ["lf", "cst", "cum"], ["cum"])
        ACT(ecum[:, 0:4, :], cum[:, 0:4, :], AF.Exp, ["cum"], ["ecum"])
        ACT(encum[:, 0:4, :], cum[:, 0:4, :], AF.Exp, ["cum"], ["encum"], scale=-1.0)
        ACT(ecum[:, 4:6, :], cum[:, 4:6, :], AF.Exp, ["cum", "ecum"], ["ecum"], scale=-1.0 / 16.0)
        ACT(encum[:, 4:6, :], cum[:, 4:6, :], AF.Exp, ["cum", "encum"], ["encum"], scale=1.0 / 16.0)
        TT(ktilT[:, 0:4, :], kk, encum[:, 0:4, :], ALU.mult, ["kk", "encum"], ["ktilT"])
        TT(ktilT[:, 4:6, :], b6[:, 0:2, :], encum[:, 4:6, :], ALU.mult, ["ps6", "encum", "ktilT"], ["ktilT"])
        CP("dve", eclb[:, 0:4], ecum[:, 0:4, 127], ["ecum"], ["eclb"])
        CP("dve", eclb[:, 4:8].rearrange("p (a b) -> p a b", a=2), bc(ecum[:, 4:6, 127], [128, 2, 2], 2), ["ecum", "eclb"], ["eclb"])
        if main:
            ACT(nsig, b1, AF.Silu, ["ps1", "kk"], ["nsig"])
            TT(qtilT[:, 0:4, :], nsig, ecum[:, 0:4, :], ALU.mult, ["nsig", "ecum"], ["qtilT"])
            STT(qtilT[:, 4:6, :], b6[:, 2:4, :], 0.125, ecum[:, 4:6, :], ALU.mult, ALU.mult, ["ps6", "ecum", "qtilT"], ["qtilT"])
        if LIM == 4:
            continue
        b2b = bank(2, BF16, [8, 128])
        for b in range(6):
            S.op("pe", lambda e, b=b: e.transpose(b2b[:, b, :], ktilT[:, b, :], identb), ["ktilT", "identb"], ["ps2"], b == 5)
        CP("act", ktok, b2b[:, 0:6, :], ["ps2"], ["ktok"])
        if LIM == 5:
            continue
        if main:
            b3 = bank(3, F32, [4, 128])
            b4 = bank(4, F32, [4, 128])
            for b in range(4):
                MM(b3[:, b, :], ktilT[:, b, :], qtilT[:, b, :], True, True, ["ktilT", "qtilT"], ["ps3"], b == 3)
            for g in range(4):
                TS(qg[:, g, :], qtilT[:, 4 + g // 2, :], rm[:, (g % 2):(g % 2) + 1], None, ALU.mult, None,
                   ["qtilT", "cst", "qg"], ["qg"])
            for g in range(4):
                MM(b4[:, g, :], ktilT[:, 4 + g // 2, :], qg[:, g, :], True, True, ["ktilT", "qg"], ["ps4"], g == 3)
            TT(sT[:, 0:4, :], b3, bc(maskT, [128, 4, 128], 1), ALU.mult, ["ps3", "cst"], ["sT"])
            TT(sT[:, 4:8, :], b4, bc(maskT, [128, 4, 128], 1), ALU.mult, ["ps4", "cst", "sT"], ["sT"])
            if LIM == 55:
                continue
            for hh in range(8):
                ob = (b5 if hh < 4 else b6)[:, hh % 4, :]
                pres = "ps5" if hh < 4 else "ps6"
                MM(ob, sT[:, hh, :], V[:, hh * 128:(hh + 1) * 128], True, False, ["sT", "V"], [pres], False)
                if hh < 4:
                    MM(ob, qtilT[:, hh, :], Sbf[:, hh, :], False, True, ["qtilT", "Sbf"], [pres], hh == 3)
                else:
                    g = hh - 4
                    MM(ob, qg[:, g, :], Sbf[:, hh, :], False, True, ["qg", "Sbf"], [pres], hh == 7)
        if LIM == 6:
            continue
        b0 = bank(0, F32, [4, 128])
        for hh in range(8):
            ub = (b7 if hh < 4 else b0)[:, hh % 4, :]
            pres = "ps7" if hh < 4 else "ps0"
            kb = hh if hh < 4 else 4 + (hh - 4) // 2
            MM(ub, ktok[:, kb, :], V[:, hh * 128:(hh + 1) * 128], True, True, ["ktok", "V"], [pres], hh in (3, 7))
        Us = sq.rearrange("p (a b) -> p a b", a=8)
        TT(Us[:, 0:4, :], b7, bc(eclb[:, 0:4], [128, 4, 128], 2), ALU.mult, ["ps7", "eclb"], ["sq"])
        TT(Us[:, 4:8, :], b0, bc(eclb[:, 4:8], [128, 4, 128], 2), ALU.mult, ["ps0", "eclb", "sq"], ["sq"])
        TT(S32, S32, bc(eclb[:, 0:8], [128, 8, 128], 2), ALU.mult, ["S32", "eclb"], ["S32"])
        TT(S32, S32, Us, ALU.add, ["S32", "sq"], ["S32"])
        CP("act", Sbf, S32, ["S32"], ["Sbf"])
        if LIM == 7:
            continue
        if main:
            sq3 = sq.rearrange("p (a b) -> p a b", a=8)
            ACT(sq3[:, 0:4, :], b5, AF.Square, ["ps5"], ["sq"])
            ACT(sq3[:, 4:8, :], b6, AF.Square, ["ps6", "sq"], ["sq"])
            S.op("dve", lambda e: e.reduce_sum(out=st8[:, 0, :], in_=sq3, axis=AX.X), ["sq"], ["st8"])
            TS(st8[:, 1, :], st8[:, 0, :], 1.0 / 128.0, EPS, ALU.mult, ALU.add, ["st8"], ["st8"])
            ACT(st8[:, 2, :], st8[:, 1, :], AF.Sqrt, ["st8"], ["st8"])
            S.op("dve", lambda e: e.reciprocal(out=st8[:, 3, :], in_=st8[:, 2, :]), ["st8"], ["st8"])
            TT(sq3[:, 0:4, :], b5, bc(st8[:, 3, 0:4], [128, 4, 128], 2), ALU.mult, ["ps5", "st8", "sq"], ["sq"])
            TT(sq3[:, 4:8, :], b6, bc(st8[:, 3, 4:8], [128, 4, 128], 2), ALU.mult, ["ps6", "st8", "sq"], ["sq"])
            TT(mixed, sq, gsil, ALU.mult, ["sq", "gsil"], ["mixed"])
            b1b = bank(1, BF16, [8, 128])
            for k in range(8):
                S.op("pe", lambda e, k=k: e.transpose(b1b[:, k, :], mixed[:, k * 128:(k + 1) * 128], identb),
                     ["mixed", "identb"], ["ps1"], k == 7)
            CP("act", tT, b1b, ["ps1"], ["tT"])
            for hf_ in range(2):
                for k in range(8):
                    MM(bank(2 + hf_), tT[:, k, :], wout[:, k, hf_ * 512:(hf_ + 1) * 512], k == 0, k == 7,
                       ["tT", "wout"], ["ps%d" % (2 + hf_)], k == 7)
            TT(x_[:, 0:512], x_[:, 0:512], bank(2), ALU.add, [xr, "ps2"], [xr])
            TT(x_[:, 512:1024], x_[:, 512:1024], bank(3), ALU.add, [xr, "ps3"], [xr])
            t = c - NPRE
            DMA("sp", hs[t], x_, "hs", [xr], [])
            if debug:
                DMA("sp", dbg["h"][t * 128:(t + 1) * 128, :], x_, "dbgh", [xr], [])

    S.barrier()
    if STOP == 1:
        return emit()
    off[0] = base_off
    wq = alloc(8 * 2048, BF16, [8, 2048])
    kt = alloc(2048, BF16, [16, 128])
    g2r = alloc(1024)
    hl = [alloc(1024), alloc(1024)]
    sq = alloc(1024)
    st1 = alloc(8)
    hn = alloc(1024, BF16)
    hnT = alloc(1024, BF16, [8, 128])
    qT = alloc(2048, BF16, [16, 128])
    ssb = alloc(2048, F32, [16, 128])
    tmp = alloc(128)
    m16 = alloc(256, F32, [16, 16])
    cand = alloc(256)
    ctmp = alloc(256)
    c16 = alloc(128, F32, [8, 16])
    e16 = alloc(128, F32, [8, 16])
    zz = alloc(24, F32, [3, 8])
    w_q_v = w_q.rearrange("(k p) c -> p k c", p=128)
    for k in range(8):
        DMA("pool", wq[:, k, :], w_q_v[:, k, :], "w%d" % (k % 4), [], ["wq"])
    DMA("pool", kt.rearrange("p a b -> p (a b)"), kt_d[:, :], "w0", [], ["kt"])
    DMA("sp", g2r, g2r_d[:, :], "c0", [], ["g2r"])
    for t in range(NMAIN):
        h_ = hl[t % 2]
        hr = "hl%d" % (t % 2)
        DMA("sp", h_, hs[t], "x%d" % (t % 2), [], [hr])
        rstd_of(h_, hr, 1024, "st1", sq, st1)
        STT(hn, h_, st1[:, 0:1], g2r, ALU.mult, ALU.mult, [hr, "st1", "g2r"], ["hn"])
        b0b = bank(0, BF16, [8, 128])
        for k in range(8):
            S.op("pe", lambda e, k=k: e.transpose(b0b[:, k, :], hn[:, k * 128:(k + 1) * 128], identb),
                 ["hn", "identb"], ["ps0"], k == 7)
        CP("act", hnT, b0b, ["ps0"], ["hnT"])
        DMA("sp", hnTs[t], hnT.rearrange("p a b -> p (a b)"), "hnTs", ["hnT"], [])
        for cb in range(16):
            bk = 1 + cb // 4
            dst = bank(bk, F32, [4, 128])[:, cb % 4, :]
            for k in range(8):
                MM(dst, wq[:, k, cb * 128:(cb + 1) * 128], hnT[:, k, :], k == 0, k == 7, ["wq", "hnT"], ["ps%d" % bk],
                   (k == 7) and (cb % 4 == 3))
        for q4 in range(4):
            CP("act", qT[:, 4 * q4:4 * q4 + 4, :], bank(1 + q4, F32, [4, 128]), ["ps%d" % (1 + q4)], ["qT"])
        sbk = [5, 6, 7, 0]
        for cb in range(16):
            bk = sbk[cb // 4]
            MM(bank(bk, F32, [4, 128])[:, cb % 4, :], qT[:, cb, :], kt[:, cb, :], True, True, ["qT", "kt"], ["ps%d" % bk],
               cb % 4 == 3)
        for q4 in range(4):
            CP("act", ssb[:, 4 * q4:4 * q4 + 4, :], bank(sbk[q4], F32, [4, 128]), ["ps%d" % sbk[q4]], ["ssb"])
        for cb in range(16):
            S.op("dve", lambda e, cb=cb: e.max(out=m16[:, cb, 0:8], in_=ssb[:, cb, :]), ["ssb"], ["m16"])
            S.op("dve", lambda e, cb=cb: e.match_replace(out=tmp, in_to_replace=m16[:, cb, 0:8], in_values=ssb[:, cb, :],
                                                         imm_value=NEG), ["ssb", "m16"], ["tmp"])
            S.op("dve", lambda e, cb=cb: e.max(out=m16[:, cb, 8:16], in_=tmp), ["tmp", "m16"], ["m16"])
        for h in range(8):
            cand3 = cand.rearrange("p (a b) -> p a b", a=16)
            TT(cand3, bc(m16[:, 2 * h, :], [128, 16, 16], 2), bc(m16[:, 2 * h + 1, :], [128, 16, 16], 1), ALU.add,
               ["m16"], ["cand"])
            S.op("dve", lambda e, h=h: e.max(out=c16[:, h, 0:8], in_=cand), ["cand"], ["c16"])
            S.op("dve", lambda e, h=h: e.match_replace(out=ctmp, in_to_replace=c16[:, h, 0:8], in_values=cand,
                                                       imm_value=NEG), ["cand", "c16"], ["ctmp"])
            S.op("dve", lambda e, h=h: e.max(out=c16[:, h, 8:16], in_=ctmp), ["ctmp", "c16"], ["c16"])
        TT(e16, c16, bc(c16[:, :, 0], [128, 8, 16], 2), ALU.subtract, ["c16"], ["e16"])
        ACT(e16, e16, AF.Exp, ["e16"], ["e16"])
        S.op("dve", lambda e: e.reduce_sum(out=zz[:, 0, :], in_=e16, axis=AX.X), ["e16"], ["zz"])
        S.op("dve", lambda e: e.reciprocal(out=zz[:, 1, :], in_=zz[:, 0, :]), ["zz"], ["zz"])
        STT(zz[:, 2, :], e16[:, :, 15], 0.9998, zz[:, 1, :], ALU.mult, ALU.mult, ["e16", "zz"], ["zz"])
        TT(ssb, ssb, bc(m16[:, :, 0], [128, 16, 128], 2), ALU.subtract, ["ssb", "m16"], ["ssb"])
        ACT(ssb, ssb, AF.Exp, ["ssb"], ["ssb"])
        ssb4 = ssb.rearrange("p (h two) n -> p h two n", two=2)
        TT(ssb4[:, :, 0, :], ssb4[:, :, 0, :], bc(zz[:, 1, :], [128, 8, 128], 2), ALU.mult, ["ssb", "zz"], ["ssb"])
        DMA("sp", ABs[t], ssb.rearrange("p a b -> p (a b)"), "ABs", ["ssb"], [])
        DMA("sp", ths[t], zz[:, 2, :], "ths", ["zz"], [])

    S.barrier()
    if STOP == 2:
        return emit()
    off[0] = base_off
    uS = [alloc(8 * 1024, BF16, [8, 1024]) for _ in range(2)]
    vS = [alloc(8 * 1024, BF16, [8, 1024]) for _ in range(2)]
    ABb = alloc(TB * 2048, F32, [TB, 2048])
    thb = alloc(TB * 8, F32, [TB, 8])
    hnTb = alloc(TB * 1024, BF16, [TB, 8, 128])
    acc = alloc(TB * 1024, F32, [TB, 1024])
    Eb = alloc(1024, F32, [8, 128])
    Gh = alloc(1024, BF16)
    Gt = alloc(1024, BF16)
    ge = alloc(1024, BF16)
    Wt = alloc(1024, BF16)
    WT = alloc(1024, BF16, [8, 128])
    hl = alloc(1024)
    sq = alloc(1024)
    st1 = alloc(8)
    gfr = alloc(1024)
    DMA("sp", gfr, gfr_d[:, :], "c0", [], ["gfr"])
    uT_v = uT.rearrange("(k p) e -> p k e", p=128)
    v_v = v_d.rearrange("(g t p) d -> g p t d", t=8, p=128)
    step = 0
    for blk in range(NB):
        t0 = blk * TB
        DMA("sp", ABb, ABs[t0:t0 + TB].rearrange("t p c -> p t c"), "ldAB", [], ["ABb"])
        DMA("sp", thb, ths[t0:t0 + TB].rearrange("t p c -> p t c"), "ldth", [], ["thb"])
        DMA("sp", hnTb.rearrange("p t a b -> p t (a b)"), hnTs[t0:t0 + TB].rearrange("t p c -> p t c"), "ldhn", [], ["hnTb"])
        S.op("dve", lambda e: e.memset(acc, 0.0), [], ["acc"])
        for eg in range(16):
            sl = eg % 2
            DMA("pool", uS[sl], uT_v[:, :, eg * 1024:(eg + 1) * 1024], "u%d" % sl, [], ["uS%d" % sl])
            DMA("pool", vS[sl], v_v[eg], "v%d" % sl, [], ["vS%d" % sl])
            for tt in range(TB):
                par = step % 2
                step += 1
                ab = [0, 1] if par == 0 else [2, 3]
                for nb in range(2):
                    for k in range(8):
                        MM(bank(ab[nb]), hnTb[:, tt, k, :], uS[sl][:, k, nb * 512:(nb + 1) * 512], k == 0, k == 7,
                           ["hnTb", "uS%d" % sl], ["ps%d" % ab[nb]], k == 7)
                for nb in range(2):
                    ACT(ge[:, nb * 512:(nb + 1) * 512], bank(ab[nb]), AF.Gelu, ["ps%d" % ab[nb], "ge"], ["ge"])
                for h in range(8):
                    a_ = ABb[:, tt, (2 * h) * 128 + 8 * eg:(2 * h) * 128 + 8 * eg + 8]
                    b_ = ABb[:, tt, (2 * h + 1) * 128:(2 * h + 2) * 128]
                    TT(Eb, bc(a_, [128, 8, 128], 2), bc(b_, [128, 8, 128], 1), ALU.mult, ["ABb"], ["Eb"])
                    E2 = Eb.rearrange("p a b -> p (a b)")
                    dst = Gt if h == 0 else Gh
                    STT(dst, E2, thb[:, tt, h:h + 1], E2, ALU.is_ge, ALU.mult, ["Eb", "thb"], ["Gt" if h == 0 else "Gh"])
                    if h > 0:
                        TT(Gt, Gt, Gh, ALU.add, ["Gt", "Gh"], ["Gt"])
                TT(Wt, Gt, ge, ALU.mult, ["Gt", "ge"], ["Wt"])
                wb = 4 + par
                wtb = bank(wb, BF16, [8, 128])
                for et in range(8):
                    S.op("pe", lambda e, et=et, wtb=wtb: e.transpose(wtb[:, et, :], Wt[:, et * 128:(et + 1) * 128], identb),
                         ["Wt", "identb"], ["ps%d" % wb], et == 7)
                CP("act", WT, wtb, ["ps%d" % wb], ["WT"])
                for hf_ in range(2):
                    for et in range(8):
                        MM(bank(6 + hf_), WT[:, et, :], vS[sl][:, et, hf_ * 512:(hf_ + 1) * 512], et == 0, et == 7,
                           ["WT", "vS%d" % sl], ["ps%d" % (6 + hf_)], et == 7)
                TT(acc[:, tt, 0:512], acc[:, tt, 0:512], bank(6), ALU.add, ["acc", "ps6"], ["acc"])
                TT(acc[:, tt, 512:1024], acc[:, tt, 512:1024], bank(7), ALU.add, ["acc", "ps7"], ["acc"])
        for tt in range(TB):
            t = t0 + tt
            DMA("sp", hl, hs[t], "x0", [], ["hl"])
            TT(hl, hl, acc[:, tt, :], ALU.add, ["hl", "acc"], ["hl"])
            rstd_of(hl, "hl", 1024, "st1", sq, st1)
            STT(hl, hl, st1[:, 0:1], gfr, ALU.mult, ALU.mult, ["hl", "st1", "gfr"], ["hl"])
            DMA("sp", out_d[t * 128:(t + 1) * 128, :], hl, "out", ["hl"], [])
    S.barrier()

    return emit()


def host_inputs(x_main, x_pre, P):
    d = dict(P)
    d["xa"] = np.ascontiguousarray(np.concatenate([x_pre, x_main], axis=0))
    return d


def prep_params(norm1_g, w_in, hg_lower_logits, hg_norm_g, gla_w_gate_up, gla_b_gate, gla_norm_g, w_out, norm2_g,
                peer_w_q, peer_sub_keys, peer_u, peer_v, norm_f_g):
    f = np.float32
    cols = np.zeros((128, 16), f)
    cols[:, 0:4] = hg_lower_logits[0].reshape(4, 128).T
    cols[:, 4:8] = hg_lower_logits[1].reshape(4, 128).T
    cols[:, 8:10] = gla_b_gate[0].reshape(2, 128).T
    cols[:, 10] = hg_norm_g[0]
    cols[:, 11] = gla_norm_g[0]
    cst = np.zeros((128, 770), f)
    cst[0:64, 768] = 1.0
    cst[64:128, 769] = 1.0
    cst[:, 0:128] = np.eye(128, dtype=f)
    cst[:, 128:256] = np.triu(np.ones((128, 128), f))
    rp = np.ones((128, 512), f)
    rp[:, 0::128] = 0.0
    cst[:, 256:768] = rp
    rep = lambda g: np.ascontiguousarray(np.broadcast_to(g.reshape(1, -1), (128, g.size))).astype(f)
    kt = np.ascontiguousarray(peer_sub_keys[0].reshape(16, 128, 128).transpose(2, 0, 1).reshape(128, 2048))
    return {
        "w_in": np.ascontiguousarray(w_in[0]),
        "glowT": np.ascontiguousarray(w_in[0][:, 3072:3088].T),
        "w_up": np.ascontiguousarray(gla_w_gate_up[0]),
        "cols": cols,
        "g1r": rep(norm1_g[0]), "g2r": rep(norm2_g[0]), "gfr": rep(norm_f_g),
        "w_out": np.ascontiguousarray(w_out[0]),
        "w_q": np.ascontiguousarray(peer_w_q[0]),
        "kt": kt,
        "uT": np.ascontiguousarray(peer_u[0].T),
        "v": np.ascontiguousarray(peer_v[0]),
        "cst": cst,
    }


def kernel(x, norm1_g, w_in, hg_lower_logits, hg_norm_g, gla_w_gate_up, gla_b_gate, gla_norm_g, w_out, norm2_g,
           peer_w_q, peer_sub_keys, peer_u, peer_v, norm_f_g):
    args = [np.asarray(a, dtype=np.float32) for a in (norm1_g, w_in, hg_lower_logits, hg_norm_g, gla_w_gate_up,
            gla_b_gate, gla_norm_g, w_out, norm2_g, peer_w_q, peer_sub_keys, peer_u, peer_v, norm_f_g)]
    x = np.asarray(x, dtype=np.float32)
    P = prep_params(*args)
    B, T, D = x.shape
    half = T // 2
    in_maps = []
    for c in range(8):
        b, hf = c // 2, c % 2
        xm = x[b, hf * half:(hf + 1) * half]
        xp = x[b, 0:half] if hf == 1 else np.zeros((half, D), np.float32)
        in_maps.append(host_inputs(xm, xp, P))
    nc = build_program(32, 32, 4)
    res = run_bass_kernel_spmd(nc, in_maps, core_ids=list(range(8)))
    out = np.empty((B, T, D), np.float32)
    for c in range(8):
        b, hf = c // 2, c % 2
        out[b, hf * half:(hf + 1) * half] = res.results[c]["out"]
    return out
```

```python
import numpy as np
from contextlib import ExitStack
import concourse.bass as bass
import concourse.mybir as mybir
from concourse.bass_utils import run_bass_kernel_spmd

F32, BF16 = mybir.dt.float32, mybir.dt.bfloat16
AF = mybir.ActivationFunctionType
ALU = mybir.AluOpType
AX = mybir.AxisListType
EPS = 1e-6
STOP = 3
NA = 5
MARGIN = 2.0e-4
LIM = 99
EXP = 0
NEG = -1.0e30


class Sched:
    def __init__(self):
        self.q = {e: [] for e in ("pe", "act", "dve", "pool", "sp")}
        self.cnt = {e: 0 for e in self.q}
        self.dcnt = {}
        self.res = {}
        self.waited = {e: {} for e in self.q}

    def _deps(self, eng, reads, writes):
        deps = []
        for r in reads:
            st = self.res.get(r)
            if st and st[0]:
                deps.append(st[0])
        for w in writes:
            st = self.res.get(w)
            if st:
                if st[0]:
                    deps.append(st[0])
                deps.extend(st[1])
        if eng == "pe":
            deps = [d for d in deps if d[0] != "c_pe"]
        return deps

    def _commit(self, reads, writes, ticket):
        for r in reads:
            self.res.setdefault(r, [None, []])[1].append(ticket)
        for w in writes:
            self.res[w] = [ticket, []]

    def _waits(self, eng, deps):
        wl = self.waited[eng]
        need = {}
        for s, v in deps:
            if wl.get(s, 0) < v:
                need[s] = max(need.get(s, 0), v)
        for s, v in need.items():
            wl[s] = v
            self.q[eng].append(("wait", s, v))

    def op(self, eng, fn, reads=(), writes=(), sig=True):
        self._waits(eng, self._deps(eng, reads, writes))
        if sig:
            self.cnt[eng] += 1
            ticket = ("c_" + eng, self.cnt[eng])
            self.q[eng].append(("op", fn, "c_" + eng))
        else:
            ticket = ("c_" + eng, self.cnt[eng] + 1)
            self.q[eng].append(("op", fn, None))
        self._commit(reads, writes, ticket)

    def dma(self, eng, fn, key, reads=(), writes=()):
        deps = self._deps(eng, reads, writes)
        prev = self.dcnt.get(key, 0)
        if prev:
            deps.append(("d_" + key, prev * 16))
        self._waits(eng, deps)
        self.dcnt[key] = prev + 1
        self.q[eng].append(("dma", fn, "d_" + key))
        self._commit(reads, writes, ("d_" + key, (prev + 1) * 16))

    def barrier(self):
        allsem = [("c_" + e, c) for e, c in self.cnt.items() if c] + \
                 [("d_" + k, c * 16) for k, c in self.dcnt.items()]
        for e in self.q:
            self._waits(e, allsem)
        self.res = {}

    def sem_names(self):
        return ["c_" + e for e in self.cnt] + ["d_" + k for k in self.dcnt]


def build_program(NPRE, NMAIN, TB, debug=False):
    nc = bass.Bass("TRN2", target_bir_lowering=False)
    NT = NPRE + NMAIN
    NB = NMAIN // TB
    D = 1024

    def din(name, shape, dt=F32):
        return nc.dram_tensor(name, list(shape), dt, kind="ExternalInput").ap()

    xa = din("xa", [NT * 128, D])
    w_in = din("w_in", [D, 3600])
    glowT = din("glowT", [16, D])
    w_up = din("w_up", [16, 256])
    cols = din("cols", [128, 16])
    g1r_d = din("g1r", [128, D])
    g2r_d = din("g2r", [128, D])
    gfr_d = din("gfr", [128, D])
    w_out = din("w_out", [D, D])
    w_q = din("w_q", [D, 2048])
    kt_d = din("kt", [128, 2048])
    uT = din("uT", [D, 16384])
    v_d = din("v", [16384, D])
    cst_d = din("cst", [128, 770])
    out_d = nc.dram_tensor("out", [NMAIN * 128, D], F32, kind="ExternalOutput").ap()
    hs = nc.dram_tensor("hs", [NMAIN, 128, D], F32).ap()
    hnTs = nc.dram_tensor("hnTs", [NMAIN, 128, D], BF16).ap()
    ABs = nc.dram_tensor("ABs", [NMAIN, 128, 2048], F32).ap()
    ths = nc.dram_tensor("ths", [NMAIN, 128, 8], F32).ap()
    dbg = {}
    if debug:
        dbg["h"] = nc.dram_tensor("dbg_h", [NMAIN * 128, D], F32, kind="ExternalOutput").ap()

    S = Sched()
    stack = ExitStack()
    NW = 53000
    POOL = stack.enter_context(nc.sbuf_tensor("pool", [128, NW], F32))
    PS = stack.enter_context(nc.psum_tensor("ps", [128, 8, 512], F32))
    off = [0]

    def alloc(n, dt=F32, shape=None):
        nw = n if dt == F32 else (n + 1) // 2
        a = POOL[:, off[0]:off[0] + nw]
        off[0] += nw
        assert off[0] <= NW, ("sbuf overflow", off[0])
        if dt == BF16:
            a = a.bitcast(BF16)
        if shape is not None:
            names = " ".join("abc"[: len(shape)])
            kw = {"abc"[i]: shape[i] for i in range(len(shape) - 1)}
            a = a.rearrange("p (%s) -> p %s" % (names, names), **kw)
        return a

    def bank(k, dt=F32, shape=None):
        a = PS[:, k, :]
        if dt == BF16:
            a = a.bitcast(BF16)
        if shape is not None:
            names = " ".join("abc"[: len(shape)])
            kw = {"abc"[i]: shape[i] for i in range(len(shape) - 1)}
            a = a.rearrange("p (%s) -> p %s" % (names, names), **kw)
        return a

    def MM(out, lhsT, rhs, start, stop, reads, writes, sig):
        S.op("pe", lambda e: e.matmul(out, lhsT=lhsT, rhs=rhs, start=start, stop=stop), reads, writes, sig)

    def ACT(out, in_, func, reads, writes, bias=None, scale=None):
        kw = {}
        if bias is not None:
            kw["bias"] = bias
        if scale is not None:
            kw["scale"] = scale
        S.op("act", lambda e: e.activation(out=out, in_=in_, func=func, **kw), reads, writes)

    def TT(out, in0, in1, op, reads, writes, eng="dve"):
        S.op(eng, lambda e: e.tensor_tensor(out=out, in0=in0, in1=in1, op=op), reads, writes)

    def TS(out, in0, s1, s2, op0, op1, reads, writes):
        if op1 is None:
            S.op("dve", lambda e: e.tensor_scalar(out=out, in0=in0, scalar1=s1, scalar2=None, op0=op0), reads, writes)
        else:
            S.op("dve", lambda e: e.tensor_scalar(out=out, in0=in0, scalar1=s1, scalar2=s2, op0=op0, op1=op1), reads, writes)

    def STT(out, in0, scalar, in1, op0, op1, reads, writes):
        S.op("dve", lambda e: e.scalar_tensor_tensor(out=out, in0=in0, scalar=scalar, in1=in1, op0=op0, op1=op1), reads, writes)

    def CP(eng, out, in_, reads, writes):
        if eng == "act":
            S.op("act", lambda e: e.copy(out=out, in_=in_), reads, writes)
        else:
            S.op(eng, lambda e: e.tensor_copy(out, in_), reads, writes)

    def DMA(eng, out, in_, key, reads, writes):
        S.dma(eng, lambda e: e.dma_start(out=out, in_=in_), key, reads, writes)

    def bc(ap, shape, axis):
        return ap.unsqueeze(axis).to_broadcast(shape)

    def emit():
        sems = {n: stack.enter_context(nc.semaphore(n)) for n in S.sem_names()}
        with stack:
            with nc.Block() as block:
                def replay(name, e):
                    for it in S.q[name]:
                        if it[0] == "wait":
                            e.wait_ge(sems[it[1]], it[2])
                        else:
                            ins = it[1](e)
                            if it[2] is not None:
                                ins.then_inc(sems[it[2]], 16 if it[0] == "dma" else 1)

                @block.tensor
                def _(e):
                    replay("pe", e)

                @block.scalar
                def _(e):
                    replay("act", e)

                @block.vector
                def _(e):
                    replay("dve", e)

                @block.gpsimd
                def _(e):
                    replay("pool", e)

                @block.sync
                def _(e):
                    replay("sp", e)
        return nc

    cst = alloc(770)
    identb = alloc(128, BF16)
    colst = alloc(16)
    small = alloc(16)
    DMA("sp", cst, cst_d[:, :], "c0", [], ["cst"])
    DMA("pool", identb, cst_d[:, 0:128], "c1", [], ["identb"])
    DMA("sp", colst, cols[:, :], "c2", [], ["colst"])
    maskT = cst[:, 128:256]
    rpat = cst[:, 256:768]
    rm = cst[:, 768:770]
    S.op("dve", lambda e: e.memset(small[:, 6:7], 1.0), [], ["small"])
    S.op("dve", lambda e: e.memset(small[:, 7:8], EPS), ["small"], ["small"])
    onec = small[:, 6:7]
    TT(small[:, 8:12], colst[:, 4:8], colst[:, 0:4], ALU.subtract, ["colst", "small"], ["small"])
    ACT(small[:, 0:4], small[:, 8:12], AF.Sigmoid, ["small"], ["small"])
    TS(small[:, 4:6], colst[:, 8:10], -1.0, None, ALU.mult, None, ["colst", "small"], ["small"])
    omlc = small[:, 0:4]
    nbc = small[:, 4:6]
    gnc = colst[:, 10:12]
    base_off = off[0]

    def rstd_of(src, rs, n, tag, sq, st):
        TT(sq[:, 0:n], src, src, ALU.mult, [rs], ["sq"])
        S.op("dve", lambda e: e.reduce_sum(out=st[:, 1:2], in_=sq[:, 0:n], axis=AX.X), ["sq"], [tag])
        TS(st[:, 2:3], st[:, 1:2], 1.0 / n, small[:, 7:8] if False else EPS, ALU.mult, ALU.add, [tag], [tag])
        ACT(st[:, 3:4], st[:, 2:3], AF.Sqrt, [tag], [tag])
        S.op("dve", lambda e: e.reciprocal(out=st[:, 0:1], in_=st[:, 3:4]), [tag], [tag])

    win = alloc(8 * 3600, BF16, [8, 3600])
    wz = alloc(8 * 256, BF16, [8, 256])
    wout = alloc(8 * 1024, BF16, [8, 1024])
    g1r = alloc(1024)
    S32 = alloc(1024, F32, [8, 128])
    Sbf = alloc(1024, BF16, [8, 128])
    glT = alloc(1024)
    wupt = alloc(256)
    xt = [alloc(1024), alloc(1024)]
    sq = alloc(1024)
    st1 = alloc(8)
    st8 = alloc(40, F32, [5, 8])
    xn = alloc(1024, BF16)
    tT = alloc(1024, BF16, [8, 128])
    V = alloc(1024, BF16)
    gsil = alloc(1024)
    nsig = alloc(512, F32, [4, 128])
    kk = alloc(512, F32, [4, 128])
    lf = alloc(768, F32, [6, 128])
    cum = alloc(768, F32, [6, 128])
    ecum = alloc(768, F32, [6, 128])
    encum = alloc(768, F32, [6, 128])
    ez = alloc(256, F32, [2, 128])
    qtilT = alloc(768, BF16, [6, 128])
    ktilT = alloc(768, BF16, [6, 128])
    ktok = alloc(768, BF16, [6, 128])
    eclb = alloc(8)
    sT = alloc(1024, BF16, [8, 128])
    mixed = alloc(1024, BF16)
    qg = alloc(512, BF16, [4, 128])

    w_in_v = w_in.rearrange("(k p) c -> p k c", p=128)
    for k in range(8):
        DMA("pool", win[:, k, :], w_in_v[:, k, :], "w%d" % (k % 4), [], ["win"])
    DMA("pool", wout, w_out.rearrange("(k p) c -> p k c", p=128), "w0", [], ["wout"])
    DMA("sp", g1r, g1r_d[:, :], "c0", [], ["g1r"])
    DMA("sp", glT[0:16, :], glowT[:, :], "c2", [], ["glT"])
    DMA("sp", wupt[0:16, :], w_up[:, :], "c2", [], ["wupt"])
    for k in range(8):
        TS(wout[:, k, :], wout[:, k, :], gnc[:, (k // 4):(k // 4) + 1], None, ALU.mult, None, ["wout", "colst"], ["wout"])
    for k in range(8):
        b = bank(k // 2, F32, [2, 256])
        MM(b[:, k % 2, :], glT[0:16, k * 128:(k + 1) * 128], wupt[0:16, :], True, True,
           ["glT", "wupt"], ["ps%d" % (k // 2)], True)
    for kb in range(4):
        CP("dve", wz[:, 2 * kb:2 * kb + 2, :], bank(kb, F32, [2, 256]), ["ps%d" % kb], ["wz"])
    S.op("dve", lambda e: e.memset(S32, 0.0), [], ["S32"])
    S.op("dve", lambda e: e.memset(Sbf, 0.0), [], ["Sbf"])

    C_HQ, C_HF, C_HI, C_HG, C_GQ, C_GK, C_GV, C_GG = 0, 512, 1024, 1536, 2048, 2304, 2560, 3088

    def fm_block(dst, wsrc, col, wres, pres, last):
        for k in range(8):
            MM(dst, wsrc[:, k, col:col + 128], tT[:, k, :], k == 0, k == 7, [wres, "tT"], [pres], (k == 7) and last)

    def tm_block(bk, col, pres):
        for k in range(8):
            MM(bank(bk), tT[:, k, :], win[:, k, col:col + 512], k == 0, k == 7, ["win", "tT"], [pres], k == 7)

    if STOP == 0:
        S.barrier()
        return emit()
    for c in range(NT):
        main = c >= NPRE
        x_ = xt[c % 2]
        xr = "xt%d" % (c % 2)
        DMA("sp", x_, xa[c * 128:(c + 1) * 128, :], "x%d" % (c % 2), [], [xr])
        rstd_of(x_, xr, 1024, "st1", sq, st1)
        STT(xn, x_, st1[:, 0:1], g1r, ALU.mult, ALU.mult, [xr, "st1", "g1r"], ["xn"])
        for k in range(8):
            S.op("pe", lambda e, k=k: e.transpose(bank(0, BF16, [8, 128])[:, k, :], xn[:, k * 128:(k + 1) * 128], identb),
                 ["xn", "identb"], ["ps0"], k == 7)
        CP("act", tT, bank(0, BF16, [8, 128]), ["ps0"], ["tT"])
        if LIM == 1:
            continue
        tm_block(1, C_HI, "ps1")
        tm_block(2, C_GV, "ps2")
        CP("act", V[:, 0:512], bank(1), ["ps1"], ["V"])
        CP("act", V[:, 512:1024], bank(2), ["ps2"], ["V"])
        if main:
            tm_block(3, C_HG, "ps3")
            tm_block(4, C_GG, "ps4")
            ACT(gsil[:, 0:512], bank(3), AF.Silu, ["ps3"], ["gsil"])
            ACT(gsil[:, 512:1024], bank(4), AF.Silu, ["ps4"], ["gsil"])
        if LIM == 2:
            continue
        b5 = bank(5, F32, [4, 128])
        b6 = bank(6, F32, [4, 128])
        b7 = bank(7, F32, [4, 128])
        b1 = bank(1, F32, [4, 128])
        for b in range(4):
            fm_block(b5[:, b, :], win, C_HF + b * 128, "win", "ps5", b == 3)
        for b in range(2):
            fm_block(b6[:, b, :], win, C_GK + b * 128, "win", "ps6", (b == 1) and not main)
        for b in range(2):
            fm_block(b7[:, b, :], wz, b * 128, "wz", "ps7", b == 1)
        if main:
            for b in range(2):
                fm_block(b6[:, 2 + b, :], win, C_GQ + b * 128, "win", "ps6", b == 1)
            for b in range(4):
                fm_block(b1[:, b, :], win, C_HQ + b * 128, "win", "ps1", b == 3)
        if LIM == 3:
            continue
        ACT(nsig, b5, AF.Sigmoid, ["ps5"], ["nsig"], scale=-1.0)
        TT(kk, nsig, bc(omlc, [128, 4, 128], 2), ALU.mult, ["nsig", "small"], ["kk"])
        ACT(lf[:, 0:4, :], kk, AF.Ln, ["kk", "small"], ["lf"], bias=onec, scale=-1.0)
        for b in range(2):
            ACT(ez[:, b, :], b7[:, b, :], AF.Exp, ["ps7", "small"], ["ez"], bias=nbc[:, b:b + 1], scale=-1.0)
        ACT(lf[:, 4:6, :], ez, AF.Ln, ["ez", "small"], ["lf"], bias=onec, scale=1.0)
        lf2 = lf.rearrange("p a b -> p (a b)")
        cum2 = cum.rearrange("p a b -> p (a b)")
        S.op("dve", lambda e: e.tensor_tensor_scan(out=cum2[:, 0:512], data0=rpat, data1=lf2[:, 0:512], initial=0.0,
                                                   op0=ALU.mult, op1=ALU.add), ["lf", "cst"], ["cum"])
        S.op("dve", lambda e: e.tensor_tensor_scan(out=cum2[:, 512:768], data0=rpat[:, 0:256], data1=lf2[:, 512:768],
                                                   initial=0.0, op0=ALU.mult, op1=ALU.add), ["lf", "cst", "cum"], ["cum"])
        ACT(ecum[:, 0:4, :], cum[:, 0:4, :], AF.Exp, ["cum"], ["ecum"])
        ACT(encum[:, 0:4, :], cum[:, 0:4, :], AF.Exp, ["cum"], ["encum"], scale=-1.0)
        ACT(ecum[:, 4:6, :], cum[:, 4:6, :], AF.Exp, ["cum", "ecum"], ["ecum"], scale=-1.0 / 16.0)
        ACT(encum[:, 4:6, :], cum[:, 4:6, :], AF.Exp, ["cum", "encum"], ["encum"], scale=1.0 / 16.0)
        TT(ktilT[:, 0:4, :], kk, encum[:, 0:4, :], ALU.mult, ["kk", "encum"], ["ktilT"])
        TT(ktilT[:, 4:6, :], b6[:, 0:2, :], encum[:, 4:6, :], ALU.mult, ["ps6", "encum", "ktilT"], ["ktilT"])
        CP("dve", eclb[:, 0:4], ecum[:, 0:4, 127], ["ecum"], ["eclb"])
        CP("dve", eclb[:, 4:8].rearrange("p (a b) -> p a b", a=2), bc(ecum[:, 4:6, 127], [128, 2, 2], 2), ["ecum", "eclb"], ["eclb"])
        if main:
            ACT(nsig, b1, AF.Silu, ["ps1", "kk"], ["nsig"])
            TT(qtilT[:, 0:4, :], nsig, ecum[:, 0:4, :], ALU.mult, ["nsig", "ecum"], ["qtilT"])
            STT(qtilT[:, 4:6, :], b6[:, 2:4, :], 0.125, ecum[:, 4:6, :], ALU.mult, ALU.mult, ["ps6", "ecum", "qtilT"], ["qtilT"])
        if LIM == 4:
            continue
        b2b = bank(2, BF16, [8, 128])
        for b in range(6):
            S.op("pe", lambda e, b=b: e.transpose(b2b[:, b, :], ktilT[:, b, :], identb), ["ktilT", "identb"], ["ps2"], b == 5)
        CP("act", ktok, b2b[:, 0:6, :], ["ps2"], ["ktok"])
        if LIM == 5:
            continue
        if main:
            b3 = bank(3, F32, [4, 128])
            b4 = bank(4, F32, [4, 128])
            for b in range(4):
                MM(b3[:, b, :], ktilT[:, b, :], qtilT[:, b, :], True, True, ["ktilT", "qtilT"], ["ps3"], b == 3)
            for g in range(4):
                TS(qg[:, g, :], qtilT[:, 4 + g // 2, :], rm[:, (g % 2):(g % 2) + 1], None, ALU.mult, None,
                   ["qtilT", "cst", "qg"], ["qg"])
            for g in range(4):
                MM(b4[:, g, :], ktilT[:, 4 + g // 2, :], qg[:, g, :], True, True, ["ktilT", "qg"], ["ps4"], g == 3)
            TT(sT[:, 0:4, :], b3, bc(maskT, [128, 4, 128], 1), ALU.mult, ["ps3", "cst"], ["sT"])
            TT(sT[:, 4:8, :], b4, bc(maskT, [128, 4, 128], 1), ALU.mult, ["ps4", "cst", "sT"], ["sT"])
            if LIM == 55:
                continue
            for hh in range(8):
                ob = (b5 if hh < 4 else b6)[:, hh % 4, :]
                pres = "ps5" if hh < 4 else "ps6"
                MM(ob, sT[:, hh, :], V[:, hh * 128:(hh + 1) * 128], True, False, ["sT", "V"], [pres], False)
                if hh < 4:
                    MM(ob, qtilT[:, hh, :], Sbf[:, hh, :], False, True, ["qtilT", "Sbf"], [pres], hh == 3)
                else:
                    g = hh - 4
                    MM(ob, qg[:, g, :], Sbf[:, hh, :], False, True, ["qg", "Sbf"], [pres], hh == 7)
        if LIM == 6:
            continue
        b0 = bank(0, F32, [4, 128])
        for hh in range(8):
            ub = (b7 if hh < 4 else b0)[:, hh % 4, :]
            pres = "ps7" if hh < 4 else "ps0"
            kb = hh if hh < 4 else 4 + (hh - 4) // 2
            MM(ub, ktok[:, kb, :], V[:, hh * 128:(hh + 1) * 128], True, True, ["ktok", "V"], [pres], hh in (3, 7))
        Us = sq.rearrange("p (a b) -> p a b", a=8)
        TT(Us[:, 0:4, :], b7, bc(eclb[:, 0:4], [128, 4, 128], 2), ALU.mult, ["ps7", "eclb"], ["sq"])
        TT(Us[:, 4:8, :], b0, bc(eclb[:, 4:8], [128, 4, 128], 2), ALU.mult, ["ps0", "eclb", "sq"], ["sq"])
        TT(S32, S32, bc(eclb[:, 0:8], [128, 8, 128], 2), ALU.mult, ["S32", "eclb"], ["S32"])
        TT(S32, S32, Us, ALU.add, ["S32", "sq"], ["S32"])
        CP("act", Sbf, S32, ["S32"], ["Sbf"])
        if LIM == 7:
            continue
        if main:
            sq3 = sq.rearrange("p (a b) -> p a b", a=8)
            ACT(sq3[:, 0:4, :], b5, AF.Square, ["ps5"], ["sq"])
            ACT(sq3[:, 4:8, :], b6, AF.Square, ["ps6", "sq"], ["sq"])
            S.op("dve", lambda e: e.reduce_sum(out=st8[:, 0, :], in_=sq3, axis=AX.X), ["sq"], ["st8"])
            TS(st8[:, 1, :], st8[:, 0, :], 1.0 / 128.0, EPS, ALU.mult, ALU.add, ["st8"], ["st8"])
            ACT(st8[:, 2, :], st8[:, 1, :], AF.Sqrt, ["st8"], ["st8"])
            S.op("dve", lambda e: e.reciprocal(out=st8[:, 3, :], in_=st8[:, 2, :]), ["st8"], ["st8"])
            TT(sq3[:, 0:4, :], b5, bc(st8[:, 3, 0:4], [128, 4, 128], 2), ALU.mult, ["ps5", "st8", "sq"], ["sq"])
            TT(sq3[:, 4:8, :], b6, bc(st8[:, 3, 4:8], [128, 4, 128], 2), ALU.mult, ["ps6", "st8", "sq"], ["sq"])
            TT(mixed, sq, gsil, ALU.mult, ["sq", "gsil"], ["mixed"])
            b1b = bank(1, BF16, [8, 128])
            for k in range(8):
                S.op("pe", lambda e, k=k: e.transpose(b1b[:, k, :], mixed[:, k * 128:(k + 1) * 128], identb),
                     ["mixed", "identb"], ["ps1"], k == 7)
            CP("act", tT, b1b, ["ps1"], ["tT"])
            for hf_ in range(2):
                for k in range(8):
                    MM(bank(2 + hf_), tT[:, k, :], wout[:, k, hf_ * 512:(hf_ + 1) * 512], k == 0, k == 7,
                       ["tT", "wout"], ["ps%d" % (2 + hf_)], k == 7)
            TT(x_[:, 0:512], x_[:, 0:512], bank(2), ALU.add, [xr, "ps2"], [xr])
            TT(x_[:, 512:1024], x_[:, 512:1024], bank(3), ALU.add, [xr, "ps3"], [xr])
            t = c - NPRE
            DMA("sp", hs[t], x_, "hs", [xr], [])
            if debug:
                DMA("sp", dbg["h"][t * 128:(t + 1) * 128, :], x_, "dbgh", [xr], [])

    S.barrier()
    if STOP == 1:
        return emit()
    off[0] = base_off
    wq = alloc(8 * 2048, BF16, [8, 2048])
    kt = alloc(2048, BF16, [16, 128])
    g2r = alloc(1024)
    hl = [alloc(1024), alloc(1024)]
    sq = alloc(1024)
    st1 = alloc(8)
    hn = alloc(1024, BF16)
    hnT = alloc(1024, BF16, [8, 128])
    qT = alloc(2048, BF16, [16, 128])
    ssb = alloc(2048, F32, [16, 128])
    tmp = alloc(128)
    m16 = alloc(256, F32, [16, 16])
    cand = alloc(256)
    ctmp = alloc(256)
    c16 = alloc(128, F32, [8, 16])
    e16 = alloc(128, F32, [8, 16])
    zz = alloc(48, F32, [6, 8])
    w_q_v = w_q.rearrange("(k p) c -> p k c", p=128)
    for k in range(8):
        DMA("pool", wq[:, k, :], w_q_v[:, k, :], "w%d" % (k % 4), [], ["wq"])
    DMA("pool", kt.rearrange("p a b -> p (a b)"), kt_d[:, :], "w0", [], ["kt"])
    DMA("sp", g2r, g2r_d[:, :], "c0", [], ["g2r"])
    for t in range(NMAIN):
        h_ = hl[t % 2]
        hr = "hl%d" % (t % 2)
        DMA("sp", h_, hs[t], "x%d" % (t % 2), [], [hr])
        rstd_of(h_, hr, 1024, "st1", sq, st1)
        STT(hn, h_, st1[:, 0:1], g2r, ALU.mult, ALU.mult, [hr, "st1", "g2r"], ["hn"])
        b0b = bank(0, BF16, [8, 128])
        for k in range(8):
            S.op("pe", lambda e, k=k: e.transpose(b0b[:, k, :], hn[:, k * 128:(k + 1) * 128], identb),
                 ["hn", "identb"], ["ps0"], k == 7)
        CP("act", hnT, b0b, ["ps0"], ["hnT"])
        DMA("sp", hnTs[t], hnT.rearrange("p a b -> p (a b)"), "hnTs", ["hnT"], [])
        for cb in range(16):
            bk = 1 + cb // 4
            dst = bank(bk, F32, [4, 128])[:, cb % 4, :]
            for k in range(8):
                MM(dst, wq[:, k, cb * 128:(cb + 1) * 128], hnT[:, k, :], k == 0, k == 7, ["wq", "hnT"], ["ps%d" % bk],
                   (k == 7) and (cb % 4 == 3))
        for q4 in range(4):
            CP("act", qT[:, 4 * q4:4 * q4 + 4, :], bank(1 + q4, F32, [4, 128]), ["ps%d" % (1 + q4)], ["qT"])
        sbk = [5, 6, 7, 0]
        for cb in range(16):
            bk = sbk[cb // 4]
            MM(bank(bk, F32, [4, 128])[:, cb % 4, :], qT[:, cb, :], kt[:, cb, :], True, True, ["qT", "kt"], ["ps%d" % bk],
               cb % 4 == 3)
        for q4 in range(4):
            CP("act", ssb[:, 4 * q4:4 * q4 + 4, :], bank(sbk[q4], F32, [4, 128]), ["ps%d" % sbk[q4]], ["ssb"])
        for cb in range(16):
            S.op("dve", lambda e, cb=cb: e.max(out=m16[:, cb, 0:8], in_=ssb[:, cb, :]), ["ssb"], ["m16"])
            S.op("dve", lambda e, cb=cb: e.match_replace(out=tmp, in_to_replace=m16[:, cb, 0:8], in_values=ssb[:, cb, :],
                                                         imm_value=NEG), ["ssb", "m16"], ["tmp"])
            S.op("dve", lambda e, cb=cb: e.max(out=m16[:, cb, 8:16], in_=tmp), ["tmp", "m16"], ["m16"])
        for h in range(8):
            cand3 = cand.rearrange("p (a b) -> p a b", a=16)
            TT(cand3, bc(m16[:, 2 * h, :], [128, 16, 16], 2), bc(m16[:, 2 * h + 1, :], [128, 16, 16], 1), ALU.add,
               ["m16"], ["cand"])
            S.op("dve", lambda e, h=h: e.max(out=c16[:, h, 0:8], in_=cand), ["cand"], ["c16"])
            S.op("dve", lambda e, h=h: e.match_replace(out=ctmp, in_to_replace=c16[:, h, 0:8], in_values=cand,
                                                       imm_value=NEG), ["cand", "c16"], ["ctmp"])
            S.op("dve", lambda e, h=h: e.max(out=c16[:, h, 8:16], in_=ctmp), ["ctmp", "c16"], ["c16"])
        TT(e16, c16, bc(c16[:, :, 0], [128, 8, 16], 2), ALU.subtract, ["c16"], ["e16"])
        ACT(e16, e16, AF.Exp, ["e16"], ["e16"])
        S.op("dve", lambda e: e.reduce_sum(out=zz[:, 0, :], in_=e16, axis=AX.X), ["e16"], ["zz"])
        S.op("dve", lambda e: e.reciprocal(out=zz[:, 1, :], in_=zz[:, 0, :]), ["zz"], ["zz"])
        STT(zz[:, 2, :], e16[:, :, 15], 1.0 - MARGIN, zz[:, 1, :], ALU.mult, ALU.mult, ["e16", "zz"], ["zz"])
        ACT(zz[:, 3, :], zz[:, 0, :], AF.Ln, ["zz"], ["zz"])
        TS(zz[:, 4, :], c16[:, :, 15], -MARGIN, None, ALU.add, None, ["c16", "zz"], ["zz"])
        TT(zz[:, 5, :], zz[:, 4, :], c16[:, :, 0], ALU.subtract, ["zz", "c16"], ["zz"])
        TT(zz[:, 5, :], zz[:, 5, :], zz[:, 3, :], ALU.subtract, ["zz"], ["zz"])
        if NA > 0:
            CP("dve", zz[:, 2, 0:NA], zz[:, 5, 0:NA], ["zz"], ["zz"])
        for h in range(NA):
            TS(ssb[:, 2 * h, :], ssb[:, 2 * h, :], zz[:, 4, h:h + 1], None, ALU.subtract, None, ["ssb", "zz"], ["ssb"])
        if NA < 8:
            lo = 2 * NA
            TT(ssb[:, lo:16, :], ssb[:, lo:16, :], bc(m16[:, lo:16, 0], [128, 16 - lo, 128], 2), ALU.subtract,
               ["ssb", "m16"], ["ssb"])
            ACT(ssb[:, lo:16, :], ssb[:, lo:16, :], AF.Exp, ["ssb"], ["ssb"])
            ssb4 = ssb.rearrange("p (h two) n -> p h two n", two=2)
            TT(ssb4[:, NA:8, 0, :], ssb4[:, NA:8, 0, :], bc(zz[:, 1, NA:8], [128, 8 - NA, 128], 2), ALU.mult,
               ["ssb", "zz"], ["ssb"])
        DMA("sp", ABs[t], ssb.rearrange("p a b -> p (a b)"), "ABs", ["ssb"], [])
        DMA("sp", ths[t], zz[:, 2, :], "ths", ["zz"], [])

    S.barrier()
    if STOP == 2:
        return emit()
    off[0] = base_off
    uS = [alloc(8 * 1024, BF16, [8, 1024]) for _ in range(2)]
    vS = [alloc(8 * 1024, BF16, [8, 1024]) for _ in range(2)]
    ABb = alloc(TB * 2048, F32, [TB, 2048])
    thb = alloc(TB * 8, F32, [TB, 8])
    hnTb = alloc(TB * 1024, BF16, [TB, 8, 128])
    acc = alloc(TB * 1024, F32, [TB, 1024])
    Eb = alloc(1024, F32, [8, 128])
    Cb = [alloc(1024, F32, [8, 128]) for _ in range(2)]
    C2 = [alloc(1024, F32, [8, 128]) for _ in range(2)]
    Gh = [alloc(1024, BF16) for _ in range(16)]
    geT = alloc(1024, BF16, [8, 128])
    WT = alloc(1024, BF16, [8, 128])
    hl = alloc(1024)
    sq = alloc(1024)
    st1 = alloc(8)
    gfr = alloc(1024)
    DMA("sp", gfr, gfr_d[:, :], "c0", [], ["gfr"])
    uT_v = uT.rearrange("(k p) e -> p k e", p=128)
    v_v = v_d.rearrange("(g t p) d -> g p t d", t=8, p=128)
    step = 0
    gcnt = [0]
    for blk in range(NB):
        t0 = blk * TB
        DMA("sp", ABb, ABs[t0:t0 + TB].rearrange("t p c -> p t c"), "ldAB", [], ["ABb"])
        DMA("sp", thb, ths[t0:t0 + TB].rearrange("t p c -> p t c"), "ldth", [], ["thb"])
        DMA("sp", hnTb.rearrange("p t a b -> p t (a b)"), hnTs[t0:t0 + TB].rearrange("t p c -> p t c"), "ldhn", [], ["hnTb"])
        S.op("dve", lambda e: e.memset(acc, 0.0), [], ["acc"])
        for eg in range(16):
            sl = eg % 2
            DMA("pool", uS[sl], uT_v[:, :, eg * 1024:(eg + 1) * 1024], "u%d" % sl, [], ["uS%d" % sl])
            DMA("pool", vS[sl], v_v[eg], "v%d" % sl, [], ["vS%d" % sl])
            for tt in range(TB):
                par = step % 2
                step += 1
                ab = [0, 1] if par == 0 else [2, 3]
                for et in range(8):
                    pa = bank(ab[et // 4], F32, [4, 128])[:, et % 4, :]
                    for k in range(8):
                        MM(pa, uS[sl][:, k, et * 128:(et + 1) * 128], hnTb[:, tt, k, :], k == 0, k == 7,
                           ["hnTb", "uS%d" % sl], ["ps%d" % ab[et // 4]], (k == 7) and (et % 4 == 3))
                for nb in range(2):
                    ACT(geT[:, 4 * nb:4 * nb + 4, :], bank(ab[nb], F32, [4, 128]), AF.Gelu, ["ps%d" % ab[nb], "geT"], ["geT"])
                for h in range(8):
                    a_ = ABb[:, tt, (2 * h) * 128 + 8 * eg:(2 * h) * 128 + 8 * eg + 8]
                    b_ = ABb[:, tt, (2 * h + 1) * 128:(2 * h + 2) * 128]
                    gs = 8 * par + h
                    g_ = Gh[gs]
                    gr = "Gh%d" % gs
                    if h < NA:
                        cs = h % 2
                        TT(Cb[cs], bc(a_, [128, 8, 128], 2), bc(b_, [128, 8, 128], 1), ALU.add, ["ABb"], ["Cb%d" % cs])
                        S.op("act", lambda e, cs=cs: e.activation(out=C2[cs], in_=Cb[cs], func=AF.Prelu, alpha=1.0e6),
                             ["Cb%d" % cs], ["C2%d" % cs])
                        ACT(g_, C2[cs].rearrange("p a b -> p (a b)"), AF.Exp, ["C2%d" % cs, "thb"], [gr],
                            bias=thb[:, tt, h:h + 1])
                    else:
                        TT(Eb, bc(a_, [128, 8, 128], 2), bc(b_, [128, 8, 128], 1), ALU.mult, ["ABb"], ["Eb"])
                        E2 = Eb.rearrange("p a b -> p (a b)")
                        STT(g_, E2, thb[:, tt, h:h + 1], E2, ALU.is_ge, ALU.mult, ["Eb", "thb"], [gr])
                for et in range(8):
                    pg = bank(4 + et // 4, F32, [4, 128])[:, et % 4, :]
                    for h in range(8):
                        MM(pg, Gh[8 * par + h][:, et * 128:(et + 1) * 128], identb, h == 0, h == 7,
                           ["Gh%d" % (8 * par + h), "identb"], ["ps%d" % (4 + et // 4)], (h == 7) and (et % 4 == 3))
                for nb in range(2):
                    TT(WT[:, 4 * nb:4 * nb + 4, :], bank(4 + nb, F32, [4, 128]), geT[:, 4 * nb:4 * nb + 4, :], ALU.mult,
                       ["ps%d" % (4 + nb), "geT", "WT"], ["WT"])
                for hf_ in range(2):
                    for et in range(8):
                        MM(bank(6 + hf_), WT[:, et, :], vS[sl][:, et, hf_ * 512:(hf_ + 1) * 512], et == 0, et == 7,
                           ["WT", "vS%d" % sl], ["ps%d" % (6 + hf_)], et == 7)
                TT(acc[:, tt, 0:512], acc[:, tt, 0:512], bank(6), ALU.add, ["acc", "ps6"], ["acc"])
                TT(acc[:, tt, 512:1024], acc[:, tt, 512:1024], bank(7), ALU.add, ["acc", "ps7"], ["acc"])
        for tt in range(TB):
            t = t0 + tt
            DMA("sp", hl, hs[t], "x0", [], ["hl"])
            TT(hl, hl, acc[:, tt, :], ALU.add, ["hl", "acc"], ["hl"])
            rstd_of(hl, "hl", 1024, "st1", sq, st1)
            STT(hl, hl, st1[:, 0:1], gfr, ALU.mult, ALU.mult, ["hl", "st1", "gfr"], ["hl"])
            DMA("sp", out_d[t * 128:(t + 1) * 128, :], hl, "out", ["hl"], [])
    S.barrier()

    return emit()


def host_inputs(x_main, x_pre, P):
    d = dict(P)
    d["xa"] = np.ascontiguousarray(np.concatenate([x_pre, x_main], axis=0))
    return d


def prep_params(norm1_g, w_in, hg_lower_logits, hg_norm_g, gla_w_gate_up, gla_b_gate, gla_norm_g, w_out, norm2_g,
                peer_w_q, peer_sub_keys, peer_u, peer_v, norm_f_g):
    f = np.float32
    cols = np.zeros((128, 16), f)
    cols[:, 0:4] = hg_lower_logits[0].reshape(4, 128).T
    cols[:, 4:8] = hg_lower_logits[1].reshape(4, 128).T
    cols[:, 8:10] = gla_b_gate[0].reshape(2, 128).T
    cols[:, 10] = hg_norm_g[0]
    cols[:, 11] = gla_norm_g[0]
    cst = np.zeros((128, 770), f)
    cst[0:64, 768] = 1.0
    cst[64:128, 769] = 1.0
    cst[:, 0:128] = np.eye(128, dtype=f)
    cst[:, 128:256] = np.triu(np.ones((128, 128), f))
    rp = np.ones((128, 512), f)
    rp[:, 0::128] = 0.0
    cst[:, 256:768] = rp
    rep = lambda g: np.ascontiguousarray(np.broadcast_to(g.reshape(1, -1), (128, g.size))).astype(f)
    kt = np.ascontiguousarray(peer_sub_keys[0].reshape(16, 128, 128).transpose(2, 0, 1).reshape(128, 2048))
    return {
        "w_in": np.ascontiguousarray(w_in[0]),
        "glowT": np.ascontiguousarray(w_in[0][:, 3072:3088].T),
        "w_up": np.ascontiguousarray(gla_w_gate_up[0]),
        "cols": cols,
        "g1r": rep(norm1_g[0]), "g2r": rep(norm2_g[0]), "gfr": rep(norm_f_g),
        "w_out": np.ascontiguousarray(w_out[0]),
        "w_q": np.ascontiguousarray(peer_w_q[0]),
        "kt": kt,
        "uT": np.ascontiguousarray(peer_u[0].T),
        "v": np.ascontiguousarray(peer_v[0]),
        "cst": cst,
    }


def kernel(x, norm1_g, w_in, hg_lower_logits, hg_norm_g, gla_w_gate_up, gla_b_gate, gla_norm_g, w_out, norm2_g,
           peer_w_q, peer_sub_keys, peer_u, peer_v, norm_f_g):
    args = [np.asarray(a, dtype=np.float32) for a in (norm1_g, w_in, hg_lower_logits, hg_norm_g, gla_w_gate_up,
            gla_b_gate, gla_norm_g, w_out, norm2_g, peer_w_q, peer_sub_keys, peer_u, peer_v, norm_f_g)]
    x = np.asarray(x, dtype=np.float32)
    P = prep_params(*args)
    B, T, D = x.shape
    half = T // 2
    in_maps = []
    for c in range(8):
        b, hf = c // 2, c % 2
        xm = x[b, hf * half:(hf + 1) * half]
        xp = x[b, 0:half] if hf == 1 else np.zeros((half, D), np.float32)
        in_maps.append(host_inputs(xm, xp, P))
    nc = build_program(32, 32, 4)
    res = run_bass_kernel_spmd(nc, in_maps, core_ids=list(range(8)))
    out = np.empty((B, T, D), np.float32)
    for c in range(8):
        b, hf = c // 2, c % 2
        out[b, hf * half:(hf + 1) * half] = res.results[c]["out"]
    return out
```

```python
import numpy as np
from contextlib import ExitStack
import concourse.bass as bass
import concourse.mybir as mybir
from concourse.bass_utils import run_bass_kernel_spmd

F32, BF16 = mybir.dt.float32, mybir.dt.bfloat16
AF = mybir.ActivationFunctionType
ALU = mybir.AluOpType
AX = mybir.AxisListType
EPS = 1e-6
STOP = 3
NA = 5
MARGIN = 2.0e-4
LIM = 99
EXP = 0
NEG = -1.0e30


class Sched:
    def __init__(self):
        self.q = {e: [] for e in ("pe", "act", "dve", "pool", "sp")}
        self.cnt = {e: 0 for e in self.q}
        self.dcnt = {}
        self.res = {}
        self.waited = {e: {} for e in self.q}

    def _deps(self, eng, reads, writes):
        deps = []
        for r in reads:
            st = self.res.get(r)
            if st and st[0]:
                deps.append(st[0])
        for w in writes:
            st = self.res.get(w)
            if st:
                if st[0]:
                    deps.append(st[0])
                deps.extend(st[1])
        if eng == "pe":
            deps = [d for d in deps if d[0] != "c_pe"]
        return deps

    def _commit(self, reads, writes, ticket):
        for r in reads:
            self.res.setdefault(r, [None, []])[1].append(ticket)
        for w in writes:
            self.res[w] = [ticket, []]

    def _waits(self, eng, deps):
        wl = self.waited[eng]
        need = {}
        for s, v in deps:
            if wl.get(s, 0) < v:
                need[s] = max(need.get(s, 0), v)
        for s, v in need.items():
            wl[s] = v
            self.q[eng].append(("wait", s, v))

    def op(self, eng, fn, reads=(), writes=(), sig=True):
        self._waits(eng, self._deps(eng, reads, writes))
        if sig:
            self.cnt[eng] += 1
            ticket = ("c_" + eng, self.cnt[eng])
            self.q[eng].append(("op", fn, "c_" + eng))
        else:
            ticket = ("c_" + eng, self.cnt[eng] + 1)
            self.q[eng].append(("op", fn, None))
        self._commit(reads, writes, ticket)

    def dma(self, eng, fn, key, reads=(), writes=()):
        deps = self._deps(eng, reads, writes)
        prev = self.dcnt.get(key, 0)
        if prev:
            deps.append(("d_" + key, prev * 16))
        self._waits(eng, deps)
        self.dcnt[key] = prev + 1
        self.q[eng].append(("dma", fn, "d_" + key))
        self._commit(reads, writes, ("d_" + key, (prev + 1) * 16))

    def barrier(self):
        allsem = [("c_" + e, c) for e, c in self.cnt.items() if c] + \
                 [("d_" + k, c * 16) for k, c in self.dcnt.items()]
        for e in self.q:
            self._waits(e, allsem)
        self.res = {}

    def sem_names(self):
        return ["c_" + e for e in self.cnt] + ["d_" + k for k in self.dcnt]


def build_program(NPRE, NMAIN, TB, debug=False):
    nc = bass.Bass("TRN2", target_bir_lowering=False)
    NT = NPRE + NMAIN
    NB = NMAIN // TB
    D = 1024

    def din(name, shape, dt=F32):
        return nc.dram_tensor(name, list(shape), dt, kind="ExternalInput").ap()

    xa = din("xa", [NT * 128, D])
    w_in = din("w_in", [D, 3600])
    glowT = din("glowT", [16, D])
    w_up = din("w_up", [16, 256])
    cols = din("cols", [128, 16])
    g1r_d = din("g1r", [128, D])
    g2r_d = din("g2r", [128, D])
    gfr_d = din("gfr", [128, D])
    w_out = din("w_out", [D, D])
    w_q = din("w_q", [D, 2048])
    kt_d = din("kt", [128, 2048])
    uT = din("uT", [D, 16384])
    v_d = din("v", [16384, D])
    cst_d = din("cst", [128, 770])
    out_d = nc.dram_tensor("out", [NMAIN * 128, D], F32, kind="ExternalOutput").ap()
    hs = nc.dram_tensor("hs", [NMAIN, 128, D], F32).ap()
    hnTs = nc.dram_tensor("hnTs", [NMAIN, 128, D], BF16).ap()
    ABs = nc.dram_tensor("ABs", [NMAIN, 128, 2048], F32).ap()
    ths = nc.dram_tensor("ths", [NMAIN, 128, 8], F32).ap()
    dbg = {}
    if debug:
        dbg["h"] = nc.dram_tensor("dbg_h", [NMAIN * 128, D], F32, kind="ExternalOutput").ap()

    S = Sched()
    stack = ExitStack()
    NW = 53000
    POOL = stack.enter_context(nc.sbuf_tensor("pool", [128, NW], F32))
    PS = stack.enter_context(nc.psum_tensor("ps", [128, 8, 512], F32))
    off = [0]

    def alloc(n, dt=F32, shape=None):
        nw = n if dt == F32 else (n + 1) // 2
        a = POOL[:, off[0]:off[0] + nw]
        off[0] += nw
        assert off[0] <= NW, ("sbuf overflow", off[0])
        if dt == BF16:
            a = a.bitcast(BF16)
        if shape is not None:
            names = " ".join("abc"[: len(shape)])
            kw = {"abc"[i]: shape[i] for i in range(len(shape) - 1)}
            a = a.rearrange("p (%s) -> p %s" % (names, names), **kw)
        return a

    def bank(k, dt=F32, shape=None):
        a = PS[:, k, :]
        if dt == BF16:
            a = a.bitcast(BF16)
        if shape is not None:
            names = " ".join("abc"[: len(shape)])
            kw = {"abc"[i]: shape[i] for i in range(len(shape) - 1)}
            a = a.rearrange("p (%s) -> p %s" % (names, names), **kw)
        return a

    def MM(out, lhsT, rhs, start, stop, reads, writes, sig):
        S.op("pe", lambda e: e.matmul(out, lhsT=lhsT, rhs=rhs, start=start, stop=stop), reads, writes, sig)

    def ACT(out, in_, func, reads, writes, bias=None, scale=None):
        kw = {}
        if bias is not None:
            kw["bias"] = bias
        if scale is not None:
            kw["scale"] = scale
        S.op("act", lambda e: e.activation(out=out, in_=in_, func=func, **kw), reads, writes)

    def TT(out, in0, in1, op, reads, writes, eng="dve"):
        S.op(eng, lambda e: e.tensor_tensor(out=out, in0=in0, in1=in1, op=op), reads, writes)

    def TS(out, in0, s1, s2, op0, op1, reads, writes):
        if op1 is None:
            S.op("dve", lambda e: e.tensor_scalar(out=out, in0=in0, scalar1=s1, scalar2=None, op0=op0), reads, writes)
        else:
            S.op("dve", lambda e: e.tensor_scalar(out=out, in0=in0, scalar1=s1, scalar2=s2, op0=op0, op1=op1), reads, writes)

    def STT(out, in0, scalar, in1, op0, op1, reads, writes):
        S.op("dve", lambda e: e.scalar_tensor_tensor(out=out, in0=in0, scalar=scalar, in1=in1, op0=op0, op1=op1), reads, writes)

    def CP(eng, out, in_, reads, writes):
        if eng == "act":
            S.op("act", lambda e: e.copy(out=out, in_=in_), reads, writes)
        else:
            S.op(eng, lambda e: e.tensor_copy(out, in_), reads, writes)

    def DMA(eng, out, in_, key, reads, writes):
        S.dma(eng, lambda e: e.dma_start(out=out, in_=in_), key, reads, writes)

    def bc(ap, shape, axis):
        return ap.unsqueeze(axis).to_broadcast(shape)

    def emit():
        sems = {n: stack.enter_context(nc.semaphore(n)) for n in S.sem_names()}
        with stack:
            with nc.Block() as block:
                def replay(name, e):
                    for it in S.q[name]:
                        if it[0] == "wait":
                            e.wait_ge(sems[it[1]], it[2])
                        else:
                            ins = it[1](e)
                            if it[2] is not None:
                                ins.then_inc(sems[it[2]], 16 if it[0] == "dma" else 1)

                @block.tensor
                def _(e):
                    replay("pe", e)

                @block.scalar
                def _(e):
                    replay("act", e)

                @block.vector
                def _(e):
                    replay("dve", e)

                @block.gpsimd
                def _(e):
                    replay("pool", e)

                @block.sync
                def _(e):
                    replay("sp", e)
        return nc

    cst = alloc(770)
    identb = alloc(128, BF16)
    colst = alloc(16)
    small = alloc(16)
    DMA("sp", cst, cst_d[:, :], "c0", [], ["cst"])
    DMA("pool", identb, cst_d[:, 0:128], "c1", [], ["identb"])
    DMA("sp", colst, cols[:, :], "c2", [], ["colst"])
    maskT = cst[:, 128:256]
    rpat = cst[:, 256:768]
    rm = cst[:, 768:770]
    S.op("dve", lambda e: e.memset(small[:, 6:7], 1.0), [], ["small"])
    S.op("dve", lambda e: e.memset(small[:, 7:8], EPS), ["small"], ["small"])
    onec = small[:, 6:7]
    TT(small[:, 8:12], colst[:, 4:8], colst[:, 0:4], ALU.subtract, ["colst", "small"], ["small"])
    ACT(small[:, 0:4], small[:, 8:12], AF.Sigmoid, ["small"], ["small"])
    TS(small[:, 4:6], colst[:, 8:10], -1.0, None, ALU.mult, None, ["colst", "small"], ["small"])
    omlc = small[:, 0:4]
    nbc = small[:, 4:6]
    gnc = colst[:, 10:12]
    base_off = off[0]

    def rstd_of(src, rs, n, tag, sq, st):
        TT(sq[:, 0:n], src, src, ALU.mult, [rs], ["sq"])
        S.op("dve", lambda e: e.reduce_sum(out=st[:, 1:2], in_=sq[:, 0:n], axis=AX.X), ["sq"], [tag])
        TS(st[:, 2:3], st[:, 1:2], 1.0 / n, small[:, 7:8] if False else EPS, ALU.mult, ALU.add, [tag], [tag])
        ACT(st[:, 3:4], st[:, 2:3], AF.Sqrt, [tag], [tag])
        S.op("dve", lambda e: e.reciprocal(out=st[:, 0:1], in_=st[:, 3:4]), [tag], [tag])

    win = alloc(8 * 3600, BF16, [8, 3600])
    wz = alloc(8 * 256, BF16, [8, 256])
    wout = alloc(8 * 1024, BF16, [8, 1024])
    g1r = alloc(1024)
    S32 = alloc(1024, F32, [8, 128])
    Sbf = alloc(1024, BF16, [8, 128])
    glT = alloc(1024)
    wupt = alloc(256)
    xt = [alloc(1024), alloc(1024)]
    sq = alloc(1024)
    st1 = alloc(8)
    st8 = alloc(40, F32, [5, 8])
    xn = alloc(1024, BF16)
    tT = alloc(1024, BF16, [8, 128])
    V = alloc(1024, BF16)
    gsil = alloc(1024)
    nsig = alloc(512, F32, [4, 128])
    kk = alloc(512, F32, [4, 128])
    lf = alloc(768, F32, [6, 128])
    cum = alloc(768, F32, [6, 128])
    ecum = alloc(768, F32, [6, 128])
    encum = alloc(768, F32, [6, 128])
    ez = alloc(256, F32, [2, 128])
    qtilT = alloc(768, BF16, [6, 128])
    ktilT = alloc(768, BF16, [6, 128])
    ktok = alloc(768, BF16, [6, 128])
    eclb = alloc(8)
    sT = alloc(1024, BF16, [8, 128])
    mixed = alloc(1024, BF16)
    qg = alloc(512, BF16, [4, 128])

    w_in_v = w_in.rearrange("(k p) c -> p k c", p=128)
    for k in range(8):
        DMA("pool", win[:, k, :], w_in_v[:, k, :], "w%d" % (k % 4), [], ["win"])
    DMA("pool", wout, w_out.rearrange("(k p) c -> p k c", p=128), "w0", [], ["wout"])
    DMA("sp", g1r, g1r_d[:, :], "c0", [], ["g1r"])
    DMA("sp", glT[0:16, :], glowT[:, :], "c2", [], ["glT"])
    DMA("sp", wupt[0:16, :], w_up[:, :], "c2", [], ["wupt"])
    for k in range(8):
        TS(wout[:, k, :], wout[:, k, :], gnc[:, (k // 4):(k // 4) + 1], None, ALU.mult, None, ["wout", "colst"], ["wout"])
    for k in range(8):
        b = bank(k // 2, F32, [2, 256])
        MM(b[:, k % 2, :], glT[0:16, k * 128:(k + 1) * 128], wupt[0:16, :], True, True,
           ["glT", "wupt"], ["ps%d" % (k // 2)], True)
    for kb in range(4):
        CP("dve", wz[:, 2 * kb:2 * kb + 2, :], bank(kb, F32, [2, 256]), ["ps%d" % kb], ["wz"])
    S.op("dve", lambda e: e.memset(S32, 0.0), [], ["S32"])
    S.op("dve", lambda e: e.memset(Sbf, 0.0), [], ["Sbf"])

    C_HQ, C_HF, C_HI, C_HG, C_GQ, C_GK, C_GV, C_GG = 0, 512, 1024, 1536, 2048, 2304, 2560, 3088

    def fm_block(dst, wsrc, col, wres, pres, last):
        for k in range(8):
            MM(dst, wsrc[:, k, col:col + 128], tT[:, k, :], k == 0, k == 7, [wres, "tT"], [pres], (k == 7) and last)

    def tm_block(bk, col, pres):
        for k in range(8):
            MM(bank(bk), tT[:, k, :], win[:, k, col:col + 512], k == 0, k == 7, ["win", "tT"], [pres], k == 7)

    if STOP == 0:
        S.barrier()
        return emit()
    for c in range(NT):
        main = c >= NPRE
        x_ = xt[c % 2]
        xr = "xt%d" % (c % 2)
        DMA("sp", x_, xa[c * 128:(c + 1) * 128, :], "x%d" % (c % 2), [], [xr])
        rstd_of(x_, xr, 1024, "st1", sq, st1)
        STT(xn, x_, st1[:, 0:1], g1r, ALU.mult, ALU.mult, [xr, "st1", "g1r"], ["xn"])
        for k in range(8):
            S.op("pe", lambda e, k=k: e.transpose(bank(0, BF16, [8, 128])[:, k, :], xn[:, k * 128:(k + 1) * 128], identb),
                 ["xn", "identb"], ["ps0"], k == 7)
        CP("act", tT, bank(0, BF16, [8, 128]), ["ps0"], ["tT"])
        if LIM == 1:
            continue
        tm_block(1, C_HI, "ps1")
        tm_block(2, C_GV, "ps2")
        CP("act", V[:, 0:512], bank(1), ["ps1"], ["V"])
        CP("act", V[:, 512:1024], bank(2), ["ps2"], ["V"])
        if main:
            tm_block(3, C_HG, "ps3")
            tm_block(4, C_GG, "ps4")
            ACT(gsil[:, 0:512], bank(3), AF.Silu, ["ps3"], ["gsil"])
            ACT(gsil[:, 512:1024], bank(4), AF.Silu, ["ps4"], ["gsil"])
        if LIM == 2:
            continue
        b5 = bank(5, F32, [4, 128])
        b6 = bank(6, F32, [4, 128])
        b7 = bank(7, F32, [4, 128])
        b1 = bank(1, F32, [4, 128])
        for b in range(4):
            fm_block(b5[:, b, :], win, C_HF + b * 128, "win", "ps5", b == 3)
        for b in range(2):
            fm_block(b6[:, b, :], win, C_GK + b * 128, "win", "ps6", (b == 1) and not main)
        for b in range(2):
            fm_block(b7[:, b, :], wz, b * 128, "wz", "ps7", b == 1)
        if main:
            for b in range(2):
                fm_block(b6[:, 2 + b, :], win, C_GQ + b * 128, "win", "ps6", b == 1)
            for b in range(4):
                fm_block(b1[:, b, :], win, C_HQ + b * 128, "win", "ps1", b == 3)
        if LIM == 3:
            continue
        ACT(nsig, b5, AF.Sigmoid, ["ps5"], ["nsig"], scale=-1.0)
        TT(kk, nsig, bc(omlc, [128, 4, 128], 2), ALU.mult, ["nsig", "small"], ["kk"])
        ACT(lf[:, 0:4, :], kk, AF.Ln, ["kk", "small"], ["lf"], bias=onec, scale=-1.0)
        for b in range(2):
            ACT(ez[:, b, :], b7[:, b, :], AF.Exp, ["ps7", "small"], ["ez"], bias=nbc[:, b:b + 1], scale=-1.0)
        ACT(lf[:, 4:6, :], ez, AF.Ln, ["ez", "small"], ["lf"], bias=onec, scale=1.0)
        lf2 = lf.rearrange("p a b -> p (a b)")
        cum2 = cum.rearrange("p a b -> p (a b)")
        S.op("dve", lambda e: e.tensor_tensor_scan(out=cum2[:, 0:512], data0=rpat, data1=lf2[:, 0:512], initial=0.0,
                                                   op0=ALU.mult, op1=ALU.add), ["lf", "cst"], ["cum"])
        S.op("dve", lambda e: e.tensor_tensor_scan(out=cum2[:, 512:768], data0=rpat[:, 0:256], data1=lf2[:, 512:768],
                                                   initial=0.0, op0=ALU.mult, op1=ALU.add), ["lf", "cst", "cum"], ["cum"])
        ACT(ecum[:, 0:4, :], cum[:, 0:4, :], AF.Exp, ["cum"], ["ecum"])
        ACT(encum[:, 0:4, :], cum[:, 0:4, :], AF.Exp, ["cum"], ["encum"], scale=-1.0)
        ACT(ecum[:, 4:6, :], cum[:, 4:6, :], AF.Exp, ["cum", "ecum"], ["ecum"], scale=-1.0 / 16.0)
        ACT(encum[:, 4:6, :], cum[:, 4:6, :], AF.Exp, ["cum", "encum"], ["encum"], scale=1.0 / 16.0)
        TT(ktilT[:, 0:4, :], kk, encum[:, 0:4, :], ALU.mult, ["kk", "encum"], ["ktilT"])
        TT(ktilT[:, 4:6, :], b6[:, 0:2, :], encum[:, 4:6, :], ALU.mult, ["ps6", "encum", "ktilT"], ["ktilT"])
        CP("dve", eclb[:, 0:4], ecum[:, 0:4, 127], ["ecum"], ["eclb"])
        CP("dve", eclb[:, 4:8].rearrange("p (a b) -> p a b", a=2), bc(ecum[:, 4:6, 127], [128, 2, 2], 2), ["ecum", "eclb"], ["eclb"])
        if main:
            ACT(nsig, b1, AF.Silu, ["ps1", "kk"], ["nsig"])
            TT(qtilT[:, 0:4, :], nsig, ecum[:, 0:4, :], ALU.mult, ["nsig", "ecum"], ["qtilT"])
            STT(qtilT[:, 4:6, :], b6[:, 2:4, :], 0.125, ecum[:, 4:6, :], ALU.mult, ALU.mult, ["ps6", "ecum", "qtilT"], ["qtilT"])
        if LIM == 4:
            continue
        b2b = bank(2, BF16, [8, 128])
        for b in range(6):
            S.op("pe", lambda e, b=b: e.transpose(b2b[:, b, :], ktilT[:, b, :], identb), ["ktilT", "identb"], ["ps2"], b == 5)
        CP("act", ktok, b2b[:, 0:6, :], ["ps2"], ["ktok"])
        if LIM == 5:
            continue
        if main:
            b3 = bank(3, F32, [4, 128])
            b4 = bank(4, F32, [4, 128])
            for b in range(4):
                MM(b3[:, b, :], ktilT[:, b, :], qtilT[:, b, :], True, True, ["ktilT", "qtilT"], ["ps3"], b == 3)
            for g in range(4):
                TS(qg[:, g, :], qtilT[:, 4 + g // 2, :], rm[:, (g % 2):(g % 2) + 1], None, ALU.mult, None,
                   ["qtilT", "cst", "qg"], ["qg"])
            for g in range(4):
                MM(b4[:, g, :], ktilT[:, 4 + g // 2, :], qg[:, g, :], True, True, ["ktilT", "qg"], ["ps4"], g == 3)
            TT(sT[:, 0:4, :], b3, bc(maskT, [128, 4, 128], 1), ALU.mult, ["ps3", "cst"], ["sT"])
            TT(sT[:, 4:8, :], b4, bc(maskT, [128, 4, 128], 1), ALU.mult, ["ps4", "cst", "sT"], ["sT"])
            if LIM == 55:
                continue
            for hh in range(8):
                ob = (b5 if hh < 4 else b6)[:, hh % 4, :]
                pres = "ps5" if hh < 4 else "ps6"
                MM(ob, sT[:, hh, :], V[:, hh * 128:(hh + 1) * 128], True, False, ["sT", "V"], [pres], False)
                if hh < 4:
                    MM(ob, qtilT[:, hh, :], Sbf[:, hh, :], False, True, ["qtilT", "Sbf"], [pres], hh == 3)
                else:
                    g = hh - 4
                    MM(ob, qg[:, g, :], Sbf[:, hh, :], False, True, ["qg", "Sbf"], [pres], hh == 7)
        if LIM == 6:
            continue
        b0 = bank(0, F32, [4, 128])
        for hh in range(8):
            ub = (b7 if hh < 4 else b0)[:, hh % 4, :]
            pres = "ps7" if hh < 4 else "ps0"
            kb = hh if hh < 4 else 4 + (hh - 4) // 2
            MM(ub, ktok[:, kb, :], V[:, hh * 128:(hh + 1) * 128], True, True, ["ktok", "V"], [pres], hh in (3, 7))
        Us = sq.rearrange("p (a b) -> p a b", a=8)
        TT(Us[:, 0:4, :], b7, bc(eclb[:, 0:4], [128, 4, 128], 2), ALU.mult, ["ps7", "eclb"], ["sq"])
        TT(Us[:, 4:8, :], b0, bc(eclb[:, 4:8], [128, 4, 128], 2), ALU.mult, ["ps0", "eclb", "sq"], ["sq"])
        TT(S32, S32, bc(eclb[:, 0:8], [128, 8, 128], 2), ALU.mult, ["S32", "eclb"], ["S32"])
        TT(S32, S32, Us, ALU.add, ["S32", "sq"], ["S32"])
        CP("act", Sbf, S32, ["S32"], ["Sbf"])
        if LIM == 7:
            continue
        if main:
            sq3 = sq.rearrange("p (a b) -> p a b", a=8)
            ACT(sq3[:, 0:4, :], b5, AF.Square, ["ps5"], ["sq"])
            ACT(sq3[:, 4:8, :], b6, AF.Square, ["ps6", "sq"], ["sq"])
            S.op("dve", lambda e: e.reduce_sum(out=st8[:, 0, :], in_=sq3, axis=AX.X), ["sq"], ["st8"])
            TS(st8[:, 1, :], st8[:, 0, :], 1.0 / 128.0, EPS, ALU.mult, ALU.add, ["st8"], ["st8"])
            ACT(st8[:, 2, :], st8[:, 1, :], AF.Sqrt, ["st8"], ["st8"])
            S.op("dve", lambda e: e.reciprocal(out=st8[:, 3, :], in_=st8[:, 2, :]), ["st8"], ["st8"])
            TT(sq3[:, 0:4, :], b5, bc(st8[:, 3, 0:4], [128, 4, 128], 2), ALU.mult, ["ps5", "st8", "sq"], ["sq"])
            TT(sq3[:, 4:8, :], b6, bc(st8[:, 3, 4:8], [128, 4, 128], 2), ALU.mult, ["ps6", "st8", "sq"], ["sq"])
            TT(mixed, sq, gsil, ALU.mult, ["sq", "gsil"], ["mixed"])
            b1b = bank(1, BF16, [8, 128])
            for k in range(8):
                S.op("pe", lambda e, k=k: e.transpose(b1b[:, k, :], mixed[:, k * 128:(k + 1) * 128], identb),
                     ["mixed", "identb"], ["ps1"], k == 7)
            CP("act", tT, b1b, ["ps1"], ["tT"])
            for hf_ in range(2):
                for k in range(8):
                    MM(bank(2 + hf_), tT[:, k, :], wout[:, k, hf_ * 512:(hf_ + 1) * 512], k == 0, k == 7,
                       ["tT", "wout"], ["ps%d" % (2 + hf_)], k == 7)
            TT(x_[:, 0:512], x_[:, 0:512], bank(2), ALU.add, [xr, "ps2"], [xr])
            TT(x_[:, 512:1024], x_[:, 512:1024], bank(3), ALU.add, [xr, "ps3"], [xr])
            t = c - NPRE
            DMA("sp", hs[t], x_, "hs", [xr], [])
            if debug:
                DMA("sp", dbg["h"][t * 128:(t + 1) * 128, :], x_, "dbgh", [xr], [])

    S.barrier()
    if STOP == 1:
        return emit()
    off[0] = base_off
    wq = alloc(8 * 2048, BF16, [8, 2048])
    kt = alloc(2048, BF16, [16, 128])
    g2r = alloc(1024)
    hl = [alloc(1024), alloc(1024)]
    sq = alloc(1024)
    st1 = alloc(8)
    hn = alloc(1024, BF16)
    hnT = alloc(1024, BF16, [8, 128])
    qT = alloc(2048, BF16, [16, 128])
    ssb = alloc(2048, F32, [16, 128])
    tmp = alloc(128)
    m16 = alloc(256, F32, [16, 16])
    cand = alloc(256)
    ctmp = alloc(256)
    c16 = alloc(128, F32, [8, 16])
    e16 = alloc(128, F32, [8, 16])
    zz = alloc(48, F32, [6, 8])
    w_q_v = w_q.rearrange("(k p) c -> p k c", p=128)
    for k in range(8):
        DMA("pool", wq[:, k, :], w_q_v[:, k, :], "w%d" % (k % 4), [], ["wq"])
    DMA("pool", kt.rearrange("p a b -> p (a b)"), kt_d[:, :], "w0", [], ["kt"])
    DMA("sp", g2r, g2r_d[:, :], "c0", [], ["g2r"])
    for t in range(NMAIN):
        h_ = hl[t % 2]
        hr = "hl%d" % (t % 2)
        DMA("sp", h_, hs[t], "x%d" % (t % 2), [], [hr])
        rstd_of(h_, hr, 1024, "st1", sq, st1)
        STT(hn, h_, st1[:, 0:1], g2r, ALU.mult, ALU.mult, [hr, "st1", "g2r"], ["hn"])
        b0b = bank(0, BF16, [8, 128])
        for k in range(8):
            S.op("pe", lambda e, k=k: e.transpose(b0b[:, k, :], hn[:, k * 128:(k + 1) * 128], identb),
                 ["hn", "identb"], ["ps0"], k == 7)
        CP("act", hnT, b0b, ["ps0"], ["hnT"])
        DMA("sp", hnTs[t], hnT.rearrange("p a b -> p (a b)"), "hnTs", ["hnT"], [])
        for cb in range(16):
            bk = 1 + cb // 4
            dst = bank(bk, F32, [4, 128])[:, cb % 4, :]
            for k in range(8):
                MM(dst, wq[:, k, cb * 128:(cb + 1) * 128], hnT[:, k, :], k == 0, k == 7, ["wq", "hnT"], ["ps%d" % bk],
                   (k == 7) and (cb % 4 == 3))
        for q4 in range(4):
            CP("act", qT[:, 4 * q4:4 * q4 + 4, :], bank(1 + q4, F32, [4, 128]), ["ps%d" % (1 + q4)], ["qT"])
        sbk = [5, 6, 7, 0]
        for cb in range(16):
            bk = sbk[cb // 4]
            MM(bank(bk, F32, [4, 128])[:, cb % 4, :], qT[:, cb, :], kt[:, cb, :], True, True, ["qT", "kt"], ["ps%d" % bk],
               cb % 4 == 3)
        for q4 in range(4):
            CP("act", ssb[:, 4 * q4:4 * q4 + 4, :], bank(sbk[q4], F32, [4, 128]), ["ps%d" % sbk[q4]], ["ssb"])
        for cb in range(16):
            S.op("dve", lambda e, cb=cb: e.max(out=m16[:, cb, 0:8], in_=ssb[:, cb, :]), ["ssb"], ["m16"])
            S.op("dve", lambda e, cb=cb: e.match_replace(out=tmp, in_to_replace=m16[:, cb, 0:8], in_values=ssb[:, cb, :],
                                                         imm_value=NEG), ["ssb", "m16"], ["tmp"])
            S.op("dve", lambda e, cb=cb: e.max(out=m16[:, cb, 8:16], in_=tmp), ["tmp", "m16"], ["m16"])
        for h in range(8):
            cand3 = cand.rearrange("p (a b) -> p a b", a=16)
            TT(cand3, bc(m16[:, 2 * h, :], [128, 16, 16], 2), bc(m16[:, 2 * h + 1, :], [128, 16, 16], 1), ALU.add,
               ["m16"], ["cand"])
            S.op("dve", lambda e, h=h: e.max(out=c16[:, h, 0:8], in_=cand), ["cand"], ["c16"])
            S.op("dve", lambda e, h=h: e.match_replace(out=ctmp, in_to_replace=c16[:, h, 0:8], in_values=cand,
                                                       imm_value=NEG), ["cand", "c16"], ["ctmp"])
            S.op("dve", lambda e, h=h: e.max(out=c16[:, h, 8:16], in_=ctmp), ["ctmp", "c16"], ["c16"])
        TT(e16, c16, bc(c16[:, :, 0], [128, 8, 16], 2), ALU.subtract, ["c16"], ["e16"])
        ACT(e16, e16, AF.Exp, ["e16"], ["e16"])
        S.op("dve", lambda e: e.reduce_sum(out=zz[:, 0, :], in_=e16, axis=AX.X), ["e16"], ["zz"])
        S.op("dve", lambda e: e.reciprocal(out=zz[:, 1, :], in_=zz[:, 0, :]), ["zz"], ["zz"])
        STT(zz[:, 2, :], e16[:, :, 15], 1.0 - MARGIN, zz[:, 1, :], ALU.mult, ALU.mult, ["e16", "zz"], ["zz"])
        ACT(zz[:, 3, :], zz[:, 0, :], AF.Ln, ["zz"], ["zz"])
        TS(zz[:, 4, :], c16[:, :, 15], -MARGIN, None, ALU.add, None, ["c16", "zz"], ["zz"])
        TT(zz[:, 5, :], zz[:, 4, :], c16[:, :, 0], ALU.subtract, ["zz", "c16"], ["zz"])
        TT(zz[:, 5, :], zz[:, 5, :], zz[:, 3, :], ALU.subtract, ["zz"], ["zz"])
        if NA > 0:
            CP("dve", zz[:, 2, 0:NA], zz[:, 5, 0:NA], ["zz"], ["zz"])
        for h in range(NA):
            TS(ssb[:, 2 * h, :], ssb[:, 2 * h, :], zz[:, 4, h:h + 1], None, ALU.subtract, None, ["ssb", "zz"], ["ssb"])
        if NA < 8:
            lo = 2 * NA
            TT(ssb[:, lo:16, :], ssb[:, lo:16, :], bc(m16[:, lo:16, 0], [128, 16 - lo, 128], 2), ALU.subtract,
               ["ssb", "m16"], ["ssb"])
            ACT(ssb[:, lo:16, :], ssb[:, lo:16, :], AF.Exp, ["ssb"], ["ssb"])
            ssb4 = ssb.rearrange("p (h two) n -> p h two n", two=2)
            TT(ssb4[:, NA:8, 0, :], ssb4[:, NA:8, 0, :], bc(zz[:, 1, NA:8], [128, 8 - NA, 128], 2), ALU.mult,
               ["ssb", "zz"], ["ssb"])
        DMA("sp", ABs[t], ssb.rearrange("p a b -> p (a b)"), "ABs", ["ssb"], [])
        DMA("sp", ths[t], zz[:, 2, :], "ths", ["zz"], [])

    S.barrier()
    if STOP == 2:
        return emit()
    off[0] = base_off
    uS = [alloc(8 * 1024, BF16, [8, 1024]) for _ in range(2)]
    vS = [alloc(8 * 1024, BF16, [8, 1024]) for _ in range(2)]
    ABb = alloc(TB * 2048, F32, [TB, 2048])
    thb = alloc(TB * 8, F32, [TB, 8])
    hnTb = alloc(TB * 1024, BF16, [TB, 8, 128])
    acc = alloc(TB * 1024, F32, [TB, 1024])
    Eb = alloc(1024, F32, [8, 128])
    Cb = [alloc(1024, F32, [8, 128]) for _ in range(2)]
    C2 = [alloc(1024, F32, [8, 128]) for _ in range(2)]
    Gh = [alloc(1024, BF16) for _ in range(16)]
    WT = [alloc(1024, BF16, [8, 128]) for _ in range(2)]
    hl = alloc(1024)
    sq = alloc(1024)
    st1 = alloc(8)
    gfr = alloc(1024)
    DMA("sp", gfr, gfr_d[:, :], "c0", [], ["gfr"])
    uT_v = uT.rearrange("(k p) e -> p k e", p=128)
    v_v = v_d.rearrange("(g t p) d -> g p t d", t=8, p=128)
    geTa = alloc(8 * TB * 128, BF16, [8, TB, 128])
    step = 0
    def emit_WT(par, tt, sl):
        pgb = [2, 3] if par == 0 else [4, 5]
        for nb in range(2):
            TT(WT[par][:, 4 * nb:4 * nb + 4, :], bank(pgb[nb], F32, [4, 128]), geTa[:, 4 * nb:4 * nb + 4, tt, :], ALU.mult,
               ["ps%d" % pgb[nb], "geTa", "WT%d" % par], ["WT%d" % par])

    def emit_out(par, tt, sl):
        for hf_ in range(2):
            for et in range(8):
                MM(bank(6 + hf_), WT[par][:, et, :], vS[sl][:, et, hf_ * 512:(hf_ + 1) * 512], et == 0, et == 7,
                   ["WT%d" % par, "vS%d" % sl], ["ps%d" % (6 + hf_)], et == 7)

    def emit_B2(tt):
        TT(acc[:, tt, 0:512], acc[:, tt, 0:512], bank(6), ALU.add, ["acc", "ps6"], ["acc"])
        TT(acc[:, tt, 512:1024], acc[:, tt, 512:1024], bank(7), ALU.add, ["acc", "ps7"], ["acc"])

    q1 = []
    q2 = []

    def drain(keep1, keep2):
        while len(q1) > keep1:
            a = q1.pop(0)
            emit_WT(*a)
            while q2:
                emit_B2(q2.pop(0))
            emit_out(*a)
            q2.append(a[1])
        while len(q2) > keep2:
            emit_B2(q2.pop(0))

    for blk in range(NB):
        t0 = blk * TB
        DMA("sp", ABb, ABs[t0:t0 + TB].rearrange("t p c -> p t c"), "ldAB", [], ["ABb"])
        DMA("sp", thb, ths[t0:t0 + TB].rearrange("t p c -> p t c"), "ldth", [], ["thb"])
        DMA("sp", hnTb.rearrange("p t a b -> p t (a b)"), hnTs[t0:t0 + TB].rearrange("t p c -> p t c"), "ldhn", [], ["hnTb"])
        S.op("dve", lambda e: e.memset(acc, 0.0), [], ["acc"])
        for eg in range(16):
            sl = eg % 2
            DMA("pool", uS[sl], uT_v[:, :, eg * 1024:(eg + 1) * 1024], "u%d" % sl, [], ["uS%d" % sl])
            DMA("pool", vS[sl], v_v[eg], "v%d" % sl, [], ["vS%d" % sl])
            drain(0, 1)
            for et in range(8):
                bk = et % 2
                for k in range(8):
                    MM(bank(bk)[:, 0:TB * 128].rearrange("p (a b) -> p a b", a=TB), uS[sl][:, k, et * 128:(et + 1) * 128], hnTb[:, :, k, :], k == 0, k == 7,
                       ["hnTb", "uS%d" % sl], ["ps%d" % bk], k == 7)
                ACT(geTa[:, et, :, :], bank(bk)[:, 0:TB * 128].rearrange("p (a b) -> p a b", a=TB), AF.Gelu, ["ps%d" % bk, "geTa"], ["geTa"])
            for tt in range(TB):
                par = step % 2
                step += 1
                pgb = [2, 3] if par == 0 else [4, 5]
                for h in range(8):
                    a_ = ABb[:, tt, (2 * h) * 128 + 8 * eg:(2 * h) * 128 + 8 * eg + 8]
                    b_ = ABb[:, tt, (2 * h + 1) * 128:(2 * h + 2) * 128]
                    gs = 8 * par + h
                    g_ = Gh[gs]
                    gr = "Gh%d" % gs
                    if h < NA:
                        cs = h % 2
                        TT(Cb[cs], bc(a_, [128, 8, 128], 2), bc(b_, [128, 8, 128], 1), ALU.add, ["ABb"], ["Cb%d" % cs])
                        S.op("act", lambda e, cs=cs: e.activation(out=C2[cs], in_=Cb[cs], func=AF.Prelu, alpha=1.0e6),
                             ["Cb%d" % cs], ["C2%d" % cs])
                        ACT(g_, C2[cs].rearrange("p a b -> p (a b)"), AF.Exp, ["C2%d" % cs, "thb"], [gr],
                            bias=thb[:, tt, h:h + 1])
                    else:
                        TT(Eb, bc(a_, [128, 8, 128], 2), bc(b_, [128, 8, 128], 1), ALU.mult, ["ABb"], ["Eb"])
                        E2 = Eb.rearrange("p a b -> p (a b)")
                        STT(g_, E2, thb[:, tt, h:h + 1], E2, ALU.is_ge, ALU.mult, ["Eb", "thb"], [gr])
                for et in range(8):
                    pg = bank(pgb[et // 4], F32, [4, 128])[:, et % 4, :]
                    for h in range(8):
                        MM(pg, Gh[8 * par + h][:, et * 128:(et + 1) * 128], identb, h == 0, h == 7,
                           ["Gh%d" % (8 * par + h), "identb"], ["ps%d" % pgb[et // 4]], (h == 7) and (et % 4 == 3))
                q1.append((par, tt, sl))
                drain(1, 1)
        drain(0, 0)
        for tt in range(TB):
            t = t0 + tt
            DMA("sp", hl, hs[t], "x0", [], ["hl"])
            TT(hl, hl, acc[:, tt, :], ALU.add, ["hl", "acc"], ["hl"])
            rstd_of(hl, "hl", 1024, "st1", sq, st1)
            STT(hl, hl, st1[:, 0:1], gfr, ALU.mult, ALU.mult, ["hl", "st1", "gfr"], ["hl"])
            DMA("sp", out_d[t * 128:(t + 1) * 128, :], hl, "out", ["hl"], [])
    S.barrier()

    return emit()


def host_inputs(x_main, x_pre, P):
    d = dict(P)
    d["xa"] = np.ascontiguousarray(np.concatenate([x_pre, x_main], axis=0))
    return d


def prep_params(norm1_g, w_in, hg_lower_logits, hg_norm_g, gla_w_gate_up, gla_b_gate, gla_norm_g, w_out, norm2_g,
                peer_w_q, peer_sub_keys, peer_u, peer_v, norm_f_g):
    f = np.float32
    cols = np.zeros((128, 16), f)
    cols[:, 0:4] = hg_lower_logits[0].reshape(4, 128).T
    cols[:, 4:8] = hg_lower_logits[1].reshape(4, 128).T
    cols[:, 8:10] = gla_b_gate[0].reshape(2, 128).T
    cols[:, 10] = hg_norm_g[0]
    cols[:, 11] = gla_norm_g[0]
    cst = np.zeros((128, 770), f)
    cst[0:64, 768] = 1.0
    cst[64:128, 769] = 1.0
    cst[:, 0:128] = np.eye(128, dtype=f)
    cst[:, 128:256] = np.triu(np.ones((128, 128), f))
    rp = np.ones((128, 512), f)
    rp[:, 0::128] = 0.0
    cst[:, 256:768] = rp
    rep = lambda g: np.ascontiguousarray(np.broadcast_to(g.reshape(1, -1), (128, g.size))).astype(f)
    kt = np.ascontiguousarray(peer_sub_keys[0].reshape(16, 128, 128).transpose(2, 0, 1).reshape(128, 2048))
    return {
        "w_in": np.ascontiguousarray(w_in[0]),
        "glowT": np.ascontiguousarray(w_in[0][:, 3072:3088].T),
        "w_up": np.ascontiguousarray(gla_w_gate_up[0]),
        "cols": cols,
        "g1r": rep(norm1_g[0]), "g2r": rep(norm2_g[0]), "gfr": rep(norm_f_g),
        "w_out": np.ascontiguousarray(w_out[0]),
        "w_q": np.ascontiguousarray(peer_w_q[0]),
        "kt": kt,
        "uT": np.ascontiguousarray(peer_u[0].T),
        "v": np.ascontiguousarray(peer_v[0]),
        "cst": cst,
    }


def kernel(x, norm1_g, w_in, hg_lower_logits, hg_norm_g, gla_w_gate_up, gla_b_gate, gla_norm_g, w_out, norm2_g,
           peer_w_q, peer_sub_keys, peer_u, peer_v, norm_f_g):
    args = [np.asarray(a, dtype=np.float32) for a in (norm1_g, w_in, hg_lower_logits, hg_norm_g, gla_w_gate_up,
            gla_b_gate, gla_norm_g, w_out, norm2_g, peer_w_q, peer_sub_keys, peer_u, peer_v, norm_f_g)]
    x = np.asarray(x, dtype=np.float32)
    P = prep_params(*args)
    B, T, D = x.shape
    half = T // 2
    in_maps = []
    for c in range(8):
        b, hf = c // 2, c % 2
        xm = x[b, hf * half:(hf + 1) * half]
        xp = x[b, 0:half] if hf == 1 else np.zeros((half, D), np.float32)
        in_maps.append(host_inputs(xm, xp, P))
    nc = build_program(32, 32, 4)
    res = run_bass_kernel_spmd(nc, in_maps, core_ids=list(range(8)))
    out = np.empty((B, T, D), np.float32)
    for c in range(8):
        b, hf = c // 2, c % 2
        out[b, hf * half:(hf + 1) * half] = res.results[c]["out"]
    return out
```

```python
import numpy as np
from contextlib import ExitStack
import concourse.bass as bass
import concourse.mybir as mybir
from concourse.bass_utils import run_bass_kernel_spmd

F32, BF16 = mybir.dt.float32, mybir.dt.bfloat16
AF = mybir.ActivationFunctionType
ALU = mybir.AluOpType
AX = mybir.AxisListType
EPS = 1e-6
STOP = 3
NA = 6
MARGIN = 2.0e-4
LIM = 99
EXP = 0
NEG = -1.0e30


class Sched:
    def __init__(self):
        self.q = {e: [] for e in ("pe", "act", "dve", "pool", "sp")}
        self.cnt = {e: 0 for e in self.q}
        self.dcnt = {}
        self.res = {}
        self.waited = {e: {} for e in self.q}

    def _deps(self, eng, reads, writes):
        deps = []
        for r in reads:
            st = self.res.get(r)
            if st and st[0]:
                deps.append(st[0])
        for w in writes:
            st = self.res.get(w)
            if st:
                if st[0]:
                    deps.append(st[0])
                deps.extend(st[1])
        if eng == "pe":
            deps = [d for d in deps if d[0] != "c_pe"]
        return deps

    def _commit(self, reads, writes, ticket):
        for r in reads:
            self.res.setdefault(r, [None, []])[1].append(ticket)
        for w in writes:
            self.res[w] = [ticket, []]

    def _waits(self, eng, deps):
        wl = self.waited[eng]
        need = {}
        for s, v in deps:
            if wl.get(s, 0) < v:
                need[s] = max(need.get(s, 0), v)
        for s, v in need.items():
            wl[s] = v
            self.q[eng].append(("wait", s, v))

    def op(self, eng, fn, reads=(), writes=(), sig=True):
        self._waits(eng, self._deps(eng, reads, writes))
        if sig:
            self.cnt[eng] += 1
            ticket = ("c_" + eng, self.cnt[eng])
            self.q[eng].append(("op", fn, "c_" + eng))
        else:
            ticket = ("c_" + eng, self.cnt[eng] + 1)
            self.q[eng].append(("op", fn, None))
        self._commit(reads, writes, ticket)

    def dma(self, eng, fn, key, reads=(), writes=()):
        deps = self._deps(eng, reads, writes)
        prev = self.dcnt.get(key, 0)
        if prev:
            deps.append(("d_" + key, prev * 16))
        self._waits(eng, deps)
        self.dcnt[key] = prev + 1
        self.q[eng].append(("dma", fn, "d_" + key))
        self._commit(reads, writes, ("d_" + key, (prev + 1) * 16))

    def barrier(self):
        allsem = [("c_" + e, c) for e, c in self.cnt.items() if c] + \
                 [("d_" + k, c * 16) for k, c in self.dcnt.items()]
        for e in self.q:
            self._waits(e, allsem)
        self.res = {}

    def sem_names(self):
        return ["c_" + e for e in self.cnt] + ["d_" + k for k in self.dcnt]


def build_program(NPRE, NMAIN, TB, debug=False):
    nc = bass.Bass("TRN2", target_bir_lowering=False)
    NT = NPRE + NMAIN
    NB = NMAIN // TB
    D = 1024

    def din(name, shape, dt=F32):
        return nc.dram_tensor(name, list(shape), dt, kind="ExternalInput").ap()

    xa = din("xa", [NT * 128, D])
    w_in = din("w_in", [D, 3600])
    glowT = din("glowT", [16, D])
    w_up = din("w_up", [16, 256])
    cols = din("cols", [128, 16])
    g1r_d = din("g1r", [128, D])
    g2r_d = din("g2r", [128, D])
    gfr_d = din("gfr", [128, D])
    w_out = din("w_out", [D, D])
    w_q = din("w_q", [D, 2048])
    kt_d = din("kt", [128, 2048])
    uT = din("uT", [D, 16384])
    v_d = din("v", [16384, D])
    cst_d = din("cst", [128, 770])
    out_d = nc.dram_tensor("out", [NMAIN * 128, D], F32, kind="ExternalOutput").ap()
    hs = nc.dram_tensor("hs", [NMAIN, 128, D], F32).ap()
    hnTs = nc.dram_tensor("hnTs", [NMAIN, 128, D], BF16).ap()
    ABs = nc.dram_tensor("ABs", [NMAIN, 128, 2048], F32).ap()
    ths = nc.dram_tensor("ths", [NMAIN, 128, 8], F32).ap()
    dbg = {}
    if debug:
        dbg["h"] = nc.dram_tensor("dbg_h", [NMAIN * 128, D], F32, kind="ExternalOutput").ap()

    S = Sched()
    stack = ExitStack()
    NW = 53200
    POOL = stack.enter_context(nc.sbuf_tensor("pool", [128, NW], F32))
    PS = stack.enter_context(nc.psum_tensor("ps", [128, 8, 512], F32))
    off = [0]

    def alloc(n, dt=F32, shape=None):
        nw = n if dt == F32 else (n + 1) // 2
        a = POOL[:, off[0]:off[0] + nw]
        off[0] += nw
        assert off[0] <= NW, ("sbuf overflow", off[0])
        if dt == BF16:
            a = a.bitcast(BF16)
        if shape is not None:
            names = " ".join("abc"[: len(shape)])
            kw = {"abc"[i]: shape[i] for i in range(len(shape) - 1)}
            a = a.rearrange("p (%s) -> p %s" % (names, names), **kw)
        return a

    def bank(k, dt=F32, shape=None):
        a = PS[:, k, :]
        if dt == BF16:
            a = a.bitcast(BF16)
        if shape is not None:
            names = " ".join("abc"[: len(shape)])
            kw = {"abc"[i]: shape[i] for i in range(len(shape) - 1)}
            a = a.rearrange("p (%s) -> p %s" % (names, names), **kw)
        return a

    def MM(out, lhsT, rhs, start, stop, reads, writes, sig):
        S.op("pe", lambda e: e.matmul(out, lhsT=lhsT, rhs=rhs, start=start, stop=stop), reads, writes, sig)

    def ACT(out, in_, func, reads, writes, bias=None, scale=None):
        kw = {}
        if bias is not None:
            kw["bias"] = bias
        if scale is not None:
            kw["scale"] = scale
        S.op("act", lambda e: e.activation(out=out, in_=in_, func=func, **kw), reads, writes)

    def TT(out, in0, in1, op, reads, writes, eng="dve"):
        S.op(eng, lambda e: e.tensor_tensor(out=out, in0=in0, in1=in1, op=op), reads, writes)

    def TS(out, in0, s1, s2, op0, op1, reads, writes):
        if op1 is None:
            S.op("dve", lambda e: e.tensor_scalar(out=out, in0=in0, scalar1=s1, scalar2=None, op0=op0), reads, writes)
        else:
            S.op("dve", lambda e: e.tensor_scalar(out=out, in0=in0, scalar1=s1, scalar2=s2, op0=op0, op1=op1), reads, writes)

    def STT(out, in0, scalar, in1, op0, op1, reads, writes):
        S.op("dve", lambda e: e.scalar_tensor_tensor(out=out, in0=in0, scalar=scalar, in1=in1, op0=op0, op1=op1), reads, writes)

    def CP(eng, out, in_, reads, writes):
        if eng == "act":
            S.op("act", lambda e: e.copy(out=out, in_=in_), reads, writes)
        else:
            S.op(eng, lambda e: e.tensor_copy(out, in_), reads, writes)

    def DMA(eng, out, in_, key, reads, writes):
        S.dma(eng, lambda e: e.dma_start(out=out, in_=in_), key, reads, writes)

    def bc(ap, shape, axis):
        return ap.unsqueeze(axis).to_broadcast(shape)

    def emit():
        sems = {n: stack.enter_context(nc.semaphore(n)) for n in S.sem_names()}
        with stack:
            with nc.Block() as block:
                def replay(name, e):
                    for it in S.q[name]:
                        if it[0] == "wait":
                            e.wait_ge(sems[it[1]], it[2])
                        else:
                            ins = it[1](e)
                            if it[2] is not None:
                                ins.then_inc(sems[it[2]], 16 if it[0] == "dma" else 1)

                @block.tensor
                def _(e):
                    replay("pe", e)

                @block.scalar
                def _(e):
                    replay("act", e)

                @block.vector
                def _(e):
                    replay("dve", e)

                @block.gpsimd
                def _(e):
                    replay("pool", e)

                @block.sync
                def _(e):
                    replay("sp", e)
        return nc

    cst = alloc(770)
    identb = alloc(128, BF16)
    colst = alloc(16)
    small = alloc(16)
    DMA("sp", cst, cst_d[:, :], "c0", [], ["cst"])
    DMA("pool", identb, cst_d[:, 0:128], "c1", [], ["identb"])
    DMA("sp", colst, cols[:, :], "c2", [], ["colst"])
    maskT = cst[:, 128:256]
    rpat = cst[:, 256:768]
    rm = cst[:, 768:770]
    S.op("dve", lambda e: e.memset(small[:, 6:7], 1.0), [], ["small"])
    S.op("dve", lambda e: e.memset(small[:, 7:8], EPS), ["small"], ["small"])
    onec = small[:, 6:7]
    TT(small[:, 8:12], colst[:, 4:8], colst[:, 0:4], ALU.subtract, ["colst", "small"], ["small"])
    ACT(small[:, 0:4], small[:, 8:12], AF.Sigmoid, ["small"], ["small"])
    TS(small[:, 4:6], colst[:, 8:10], -1.0, None, ALU.mult, None, ["colst", "small"], ["small"])
    omlc = small[:, 0:4]
    nbc = small[:, 4:6]
    gnc = colst[:, 10:12]
    base_off = off[0]

    def rstd_of(src, rs, n, tag, sq, st):
        TT(sq[:, 0:n], src, src, ALU.mult, [rs], ["sq"])
        S.op("dve", lambda e: e.reduce_sum(out=st[:, 1:2], in_=sq[:, 0:n], axis=AX.X), ["sq"], [tag])
        TS(st[:, 2:3], st[:, 1:2], 1.0 / n, small[:, 7:8] if False else EPS, ALU.mult, ALU.add, [tag], [tag])
        ACT(st[:, 3:4], st[:, 2:3], AF.Sqrt, [tag], [tag])
        S.op("dve", lambda e: e.reciprocal(out=st[:, 0:1], in_=st[:, 3:4]), [tag], [tag])

    win = alloc(8 * 3600, BF16, [8, 3600])
    wz = alloc(8 * 256, BF16, [8, 256])
    wout = alloc(8 * 1024, BF16, [8, 1024])
    g1r = alloc(1024)
    S32 = alloc(1024, F32, [8, 128])
    Sbf = alloc(1024, BF16, [8, 128])
    glT = alloc(1024)
    wupt = alloc(256)
    xt = [alloc(1024), alloc(1024)]
    sq = alloc(1024)
    st1 = alloc(8)
    st8 = alloc(40, F32, [5, 8])
    xn = alloc(1024, BF16)
    tT = alloc(1024, BF16, [8, 128])
    V = alloc(1024, BF16)
    gsil = alloc(1024)
    nsig = alloc(512, F32, [4, 128])
    kk = alloc(512, F32, [4, 128])
    lf = alloc(768, F32, [6, 128])
    cum = alloc(768, F32, [6, 128])
    ecum = alloc(768, F32, [6, 128])
    encum = alloc(768, F32, [6, 128])
    ez = alloc(256, F32, [2, 128])
    qtilT = alloc(768, BF16, [6, 128])
    ktilT = alloc(768, BF16, [6, 128])
    ktok = alloc(768, BF16, [6, 128])
    eclb = alloc(8)
    sT = alloc(1024, BF16, [8, 128])
    mixed = alloc(1024, BF16)
    qg = alloc(512, BF16, [4, 128])

    w_in_v = w_in.rearrange("(k p) c -> p k c", p=128)
    for k in range(8):
        DMA("pool", win[:, k, :], w_in_v[:, k, :], "w%d" % (k % 4), [], ["win"])
    DMA("pool", wout, w_out.rearrange("(k p) c -> p k c", p=128), "w0", [], ["wout"])
    DMA("sp", g1r, g1r_d[:, :], "c0", [], ["g1r"])
    DMA("sp", glT[0:16, :], glowT[:, :], "c2", [], ["glT"])
    DMA("sp", wupt[0:16, :], w_up[:, :], "c2", [], ["wupt"])
    for k in range(8):
        TS(wout[:, k, :], wout[:, k, :], gnc[:, (k // 4):(k // 4) + 1], None, ALU.mult, None, ["wout", "colst"], ["wout"])
    for k in range(8):
        b = bank(k // 2, F32, [2, 256])
        MM(b[:, k % 2, :], glT[0:16, k * 128:(k + 1) * 128], wupt[0:16, :], True, True,
           ["glT", "wupt"], ["ps%d" % (k // 2)], True)
    for kb in range(4):
        CP("dve", wz[:, 2 * kb:2 * kb + 2, :], bank(kb, F32, [2, 256]), ["ps%d" % kb], ["wz"])
    S.op("dve", lambda e: e.memset(S32, 0.0), [], ["S32"])
    S.op("dve", lambda e: e.memset(Sbf, 0.0), [], ["Sbf"])

    C_HQ, C_HF, C_HI, C_HG, C_GQ, C_GK, C_GV, C_GG = 0, 512, 1024, 1536, 2048, 2304, 2560, 3088

    def fm_block(dst, wsrc, col, wres, pres, last):
        for k in range(8):
            MM(dst, wsrc[:, k, col:col + 128], tT[:, k, :], k == 0, k == 7, [wres, "tT"], [pres], (k == 7) and last)

    def tm_block(bk, col, pres):
        for k in range(8):
            MM(bank(bk), tT[:, k, :], win[:, k, col:col + 512], k == 0, k == 7, ["win", "tT"], [pres], k == 7)

    if STOP == 0:
        S.barrier()
        return emit()
    for c in range(NT):
        main = c >= NPRE
        x_ = xt[c % 2]
        xr = "xt%d" % (c % 2)
        DMA("sp", x_, xa[c * 128:(c + 1) * 128, :], "x%d" % (c % 2), [], [xr])
        rstd_of(x_, xr, 1024, "st1", sq, st1)
        STT(xn, x_, st1[:, 0:1], g1r, ALU.mult, ALU.mult, [xr, "st1", "g1r"], ["xn"])
        for k in range(8):
            S.op("pe", lambda e, k=k: e.transpose(bank(0, BF16, [8, 128])[:, k, :], xn[:, k * 128:(k + 1) * 128], identb),
                 ["xn", "identb"], ["ps0"], k == 7)
        CP("act", tT, bank(0, BF16, [8, 128]), ["ps0"], ["tT"])
        if LIM == 1:
            continue
        tm_block(1, C_HI, "ps1")
        tm_block(2, C_GV, "ps2")
        CP("act", V[:, 0:512], bank(1), ["ps1"], ["V"])
        CP("act", V[:, 512:1024], bank(2), ["ps2"], ["V"])
        if main:
            tm_block(3, C_HG, "ps3")
            tm_block(4, C_GG, "ps4")
            ACT(gsil[:, 0:512], bank(3), AF.Silu, ["ps3"], ["gsil"])
            ACT(gsil[:, 512:1024], bank(4), AF.Silu, ["ps4"], ["gsil"])
        if LIM == 2:
            continue
        b5 = bank(5, F32, [4, 128])
        b6 = bank(6, F32, [4, 128])
        b7 = bank(7, F32, [4, 128])
        b1 = bank(1, F32, [4, 128])
        for b in range(4):
            fm_block(b5[:, b, :], win, C_HF + b * 128, "win", "ps5", b == 3)
        for b in range(2):
            fm_block(b6[:, b, :], win, C_GK + b * 128, "win", "ps6", (b == 1) and not main)
        for b in range(2):
            fm_block(b7[:, b, :], wz, b * 128, "wz", "ps7", b == 1)
        if main:
            for b in range(2):
                fm_block(b6[:, 2 + b, :], win, C_GQ + b * 128, "win", "ps6", b == 1)
            for b in range(4):
                fm_block(b1[:, b, :], win, C_HQ + b * 128, "win", "ps1", b == 3)
        if LIM == 3:
            continue
        ACT(nsig, b5, AF.Sigmoid, ["ps5"], ["nsig"], scale=-1.0)
        TT(kk, nsig, bc(omlc, [128, 4, 128], 2), ALU.mult, ["nsig", "small"], ["kk"])
        ACT(lf[:, 0:4, :], kk, AF.Ln, ["kk", "small"], ["lf"], bias=onec, scale=-1.0)
        for b in range(2):
            ACT(ez[:, b, :], b7[:, b, :], AF.Exp, ["ps7", "small"], ["ez"], bias=nbc[:, b:b + 1], scale=-1.0)
        ACT(lf[:, 4:6, :], ez, AF.Ln, ["ez", "small"], ["lf"], bias=onec, scale=1.0)
        lf2 = lf.rearrange("p a b -> p (a b)")
        cum2 = cum.rearrange("p a b -> p (a b)")
        S.op("dve", lambda e: e.tensor_tensor_scan(out=cum2[:, 0:512], data0=rpat, data1=lf2[:, 0:512], initial=0.0,
                                                   op0=ALU.mult, op1=ALU.add), ["lf", "cst"], ["cum"])
        S.op("dve", lambda e: e.tensor_tensor_scan(out=cum2[:, 512:768], data0=rpat[:, 0:256], data1=lf2[:, 512:768],
                                                   initial=0.0, op0=ALU.mult, op1=ALU.add), ["lf", "cst", "cum"], ["cum"])
        ACT(ecum[:, 0:4, :], cum[:, 0:4, :], AF.Exp, ["cum"], ["ecum"])
        ACT(encum[:, 0:4, :], cum[:, 0:4, :], AF.Exp, ["cum"], ["encum"], scale=-1.0)
        ACT(ecum[:, 4:6, :], cum[:, 4:6, :], AF.Exp, ["cum", "ecum"], ["ecum"], scale=-1.0 / 16.0)
        ACT(encum[:, 4:6, :], cum[:, 4:6, :], AF.Exp, ["cum", "encum"], ["encum"], scale=1.0 / 16.0)
        TT(ktilT[:, 0:4, :], kk, encum[:, 0:4, :], ALU.mult, ["kk", "encum"], ["ktilT"])
        TT(ktilT[:, 4:6, :], b6[:, 0:2, :], encum[:, 4:6, :], ALU.mult, ["ps6", "encum", "ktilT"], ["ktilT"])
        CP("dve", eclb[:, 0:4], ecum[:, 0:4, 127], ["ecum"], ["eclb"])
        CP("dve", eclb[:, 4:8].rearrange("p (a b) -> p a b", a=2), bc(ecum[:, 4:6, 127], [128, 2, 2], 2), ["ecum", "eclb"], ["eclb"])
        if main:
            ACT(nsig, b1, AF.Silu, ["ps1", "kk"], ["nsig"])
            TT(qtilT[:, 0:4, :], nsig, ecum[:, 0:4, :], ALU.mult, ["nsig", "ecum"], ["qtilT"])
            STT(qtilT[:, 4:6, :], b6[:, 2:4, :], 0.125, ecum[:, 4:6, :], ALU.mult, ALU.mult, ["ps6", "ecum", "qtilT"], ["qtilT"])
        if LIM == 4:
            continue
        b2b = bank(2, BF16, [8, 128])
        for b in range(6):
            S.op("pe", lambda e, b=b: e.transpose(b2b[:, b, :], ktilT[:, b, :], identb), ["ktilT", "identb"], ["ps2"], b == 5)
        CP("act", ktok, b2b[:, 0:6, :], ["ps2"], ["ktok"])
        if LIM == 5:
            continue
        if main:
            b3 = bank(3, F32, [4, 128])
            b4 = bank(4, F32, [4, 128])
            for b in range(4):
                MM(b3[:, b, :], ktilT[:, b, :], qtilT[:, b, :], True, True, ["ktilT", "qtilT"], ["ps3"], b == 3)
            for g in range(4):
                TS(qg[:, g, :], qtilT[:, 4 + g // 2, :], rm[:, (g % 2):(g % 2) + 1], None, ALU.mult, None,
                   ["qtilT", "cst", "qg"], ["qg"])
            for g in range(4):
                MM(b4[:, g, :], ktilT[:, 4 + g // 2, :], qg[:, g, :], True, True, ["ktilT", "qg"], ["ps4"], g == 3)
            TT(sT[:, 0:4, :], b3, bc(maskT, [128, 4, 128], 1), ALU.mult, ["ps3", "cst"], ["sT"])
            TT(sT[:, 4:8, :], b4, bc(maskT, [128, 4, 128], 1), ALU.mult, ["ps4", "cst", "sT"], ["sT"])
            if LIM == 55:
                continue
            for hh in range(8):
                ob = (b5 if hh < 4 else b6)[:, hh % 4, :]
                pres = "ps5" if hh < 4 else "ps6"
                MM(ob, sT[:, hh, :], V[:, hh * 128:(hh + 1) * 128], True, False, ["sT", "V"], [pres], False)
                if hh < 4:
                    MM(ob, qtilT[:, hh, :], Sbf[:, hh, :], False, True, ["qtilT", "Sbf"], [pres], hh == 3)
                else:
                    g = hh - 4
                    MM(ob, qg[:, g, :], Sbf[:, hh, :], False, True, ["qg", "Sbf"], [pres], hh == 7)
        if LIM == 6:
            continue
        b0 = bank(0, F32, [4, 128])
        for hh in range(8):
            ub = (b7 if hh < 4 else b0)[:, hh % 4, :]
            pres = "ps7" if hh < 4 else "ps0"
            kb = hh if hh < 4 else 4 + (hh - 4) // 2
            MM(ub, ktok[:, kb, :], V[:, hh * 128:(hh + 1) * 128], True, True, ["ktok", "V"], [pres], hh in (3, 7))
        Us = sq.rearrange("p (a b) -> p a b", a=8)
        TT(Us[:, 0:4, :], b7, bc(eclb[:, 0:4], [128, 4, 128], 2), ALU.mult, ["ps7", "eclb"], ["sq"])
        TT(Us[:, 4:8, :], b0, bc(eclb[:, 4:8], [128, 4, 128], 2), ALU.mult, ["ps0", "eclb", "sq"], ["sq"])
        TT(S32, S32, bc(eclb[:, 0:8], [128, 8, 128], 2), ALU.mult, ["S32", "eclb"], ["S32"])
        TT(S32, S32, Us, ALU.add, ["S32", "sq"], ["S32"])
        CP("act", Sbf, S32, ["S32"], ["Sbf"])
        if LIM == 7:
            continue
        if main:
            sq3 = sq.rearrange("p (a b) -> p a b", a=8)
            ACT(sq3[:, 0:4, :], b5, AF.Square, ["ps5"], ["sq"])
            ACT(sq3[:, 4:8, :], b6, AF.Square, ["ps6", "sq"], ["sq"])
            S.op("dve", lambda e: e.reduce_sum(out=st8[:, 0, :], in_=sq3, axis=AX.X), ["sq"], ["st8"])
            TS(st8[:, 1, :], st8[:, 0, :], 1.0 / 128.0, EPS, ALU.mult, ALU.add, ["st8"], ["st8"])
            ACT(st8[:, 2, :], st8[:, 1, :], AF.Sqrt, ["st8"], ["st8"])
            S.op("dve", lambda e: e.reciprocal(out=st8[:, 3, :], in_=st8[:, 2, :]), ["st8"], ["st8"])
            TT(sq3[:, 0:4, :], b5, bc(st8[:, 3, 0:4], [128, 4, 128], 2), ALU.mult, ["ps5", "st8", "sq"], ["sq"])
            TT(sq3[:, 4:8, :], b6, bc(st8[:, 3, 4:8], [128, 4, 128], 2), ALU.mult, ["ps6", "st8", "sq"], ["sq"])
            TT(mixed, sq, gsil, ALU.mult, ["sq", "gsil"], ["mixed"])
            b1b = bank(1, BF16, [8, 128])
            for k in range(8):
                S.op("pe", lambda e, k=k: e.transpose(b1b[:, k, :], mixed[:, k * 128:(k + 1) * 128], identb),
                     ["mixed", "identb"], ["ps1"], k == 7)
            CP("act", tT, b1b, ["ps1"], ["tT"])
            for hf_ in range(2):
                for k in range(8):
                    MM(bank(2 + hf_), tT[:, k, :], wout[:, k, hf_ * 512:(hf_ + 1) * 512], k == 0, k == 7,
                       ["tT", "wout"], ["ps%d" % (2 + hf_)], k == 7)
            TT(x_[:, 0:512], x_[:, 0:512], bank(2), ALU.add, [xr, "ps2"], [xr])
            TT(x_[:, 512:1024], x_[:, 512:1024], bank(3), ALU.add, [xr, "ps3"], [xr])
            t = c - NPRE
            DMA("sp", hs[t], x_, "hs", [xr], [])
            if debug:
                DMA("sp", dbg["h"][t * 128:(t + 1) * 128, :], x_, "dbgh", [xr], [])

    S.barrier()
    if STOP == 1:
        return emit()
    off[0] = base_off
    wq = alloc(8 * 2048, BF16, [8, 2048])
    kt = alloc(2048, BF16, [16, 128])
    g2r = alloc(1024)
    hl = [alloc(1024), alloc(1024)]
    sq = alloc(1024)
    st1 = alloc(8)
    hn = alloc(1024, BF16)
    hnT = alloc(1024, BF16, [8, 128])
    qT = alloc(2048, BF16, [16, 128])
    ssb = alloc(2048, F32, [16, 128])
    tmp = alloc(128)
    m16 = alloc(256, F32, [16, 16])
    cand = alloc(256)
    ctmp = alloc(256)
    c16 = alloc(128, F32, [8, 16])
    e16 = alloc(128, F32, [8, 16])
    zz = alloc(48, F32, [6, 8])
    w_q_v = w_q.rearrange("(k p) c -> p k c", p=128)
    for k in range(8):
        DMA("pool", wq[:, k, :], w_q_v[:, k, :], "w%d" % (k % 4), [], ["wq"])
    DMA("pool", kt.rearrange("p a b -> p (a b)"), kt_d[:, :], "w0", [], ["kt"])
    DMA("sp", g2r, g2r_d[:, :], "c0", [], ["g2r"])
    for t in range(NMAIN):
        h_ = hl[t % 2]
        hr = "hl%d" % (t % 2)
        DMA("sp", h_, hs[t], "x%d" % (t % 2), [], [hr])
        rstd_of(h_, hr, 1024, "st1", sq, st1)
        STT(hn, h_, st1[:, 0:1], g2r, ALU.mult, ALU.mult, [hr, "st1", "g2r"], ["hn"])
        b0b = bank(0, BF16, [8, 128])
        for k in range(8):
            S.op("pe", lambda e, k=k: e.transpose(b0b[:, k, :], hn[:, k * 128:(k + 1) * 128], identb),
                 ["hn", "identb"], ["ps0"], k == 7)
        CP("act", hnT, b0b, ["ps0"], ["hnT"])
        DMA("sp", hnTs[t], hnT.rearrange("p a b -> p (a b)"), "hnTs", ["hnT"], [])
        for cb in range(16):
            bk = 1 + cb // 4
            dst = bank(bk, F32, [4, 128])[:, cb % 4, :]
            for k in range(8):
                MM(dst, wq[:, k, cb * 128:(cb + 1) * 128], hnT[:, k, :], k == 0, k == 7, ["wq", "hnT"], ["ps%d" % bk],
                   (k == 7) and (cb % 4 == 3))
        for q4 in range(4):
            CP("act", qT[:, 4 * q4:4 * q4 + 4, :], bank(1 + q4, F32, [4, 128]), ["ps%d" % (1 + q4)], ["qT"])
        sbk = [5, 6, 7, 0]
        for cb in range(16):
            bk = sbk[cb // 4]
            MM(bank(bk, F32, [4, 128])[:, cb % 4, :], qT[:, cb, :], kt[:, cb, :], True, True, ["qT", "kt"], ["ps%d" % bk],
               cb % 4 == 3)
        for q4 in range(4):
            CP("act", ssb[:, 4 * q4:4 * q4 + 4, :], bank(sbk[q4], F32, [4, 128]), ["ps%d" % sbk[q4]], ["ssb"])
        for cb in range(16):
            S.op("dve", lambda e, cb=cb: e.max(out=m16[:, cb, 0:8], in_=ssb[:, cb, :]), ["ssb"], ["m16"])
            S.op("dve", lambda e, cb=cb: e.match_replace(out=tmp, in_to_replace=m16[:, cb, 0:8], in_values=ssb[:, cb, :],
                                                         imm_value=NEG), ["ssb", "m16"], ["tmp"])
            S.op("dve", lambda e, cb=cb: e.max(out=m16[:, cb, 8:16], in_=tmp), ["tmp", "m16"], ["m16"])
        for h in range(8):
            cand3 = cand.rearrange("p (a b) -> p a b", a=16)
            TT(cand3, bc(m16[:, 2 * h, :], [128, 16, 16], 2), bc(m16[:, 2 * h + 1, :], [128, 16, 16], 1), ALU.add,
               ["m16"], ["cand"])
            S.op("dve", lambda e, h=h: e.max(out=c16[:, h, 0:8], in_=cand), ["cand"], ["c16"])
            S.op("dve", lambda e, h=h: e.match_replace(out=ctmp, in_to_replace=c16[:, h, 0:8], in_values=cand,
                                                       imm_value=NEG), ["cand", "c16"], ["ctmp"])
            S.op("dve", lambda e, h=h: e.max(out=c16[:, h, 8:16], in_=ctmp), ["ctmp", "c16"], ["c16"])
        TT(e16, c16, bc(c16[:, :, 0], [128, 8, 16], 2), ALU.subtract, ["c16"], ["e16"])
        ACT(e16, e16, AF.Exp, ["e16"], ["e16"])
        S.op("dve", lambda e: e.reduce_sum(out=zz[:, 0, :], in_=e16, axis=AX.X), ["e16"], ["zz"])
        S.op("dve", lambda e: e.reciprocal(out=zz[:, 1, :], in_=zz[:, 0, :]), ["zz"], ["zz"])
        STT(zz[:, 2, :], e16[:, :, 15], 1.0 - MARGIN, zz[:, 1, :], ALU.mult, ALU.mult, ["e16", "zz"], ["zz"])
        ACT(zz[:, 3, :], zz[:, 0, :], AF.Ln, ["zz"], ["zz"])
        TS(zz[:, 4, :], c16[:, :, 15], -MARGIN, None, ALU.add, None, ["c16", "zz"], ["zz"])
        TT(zz[:, 5, :], zz[:, 4, :], c16[:, :, 0], ALU.subtract, ["zz", "c16"], ["zz"])
        TT(zz[:, 5, :], zz[:, 5, :], zz[:, 3, :], ALU.subtract, ["zz"], ["zz"])
        if NA > 0:
            CP("dve", zz[:, 2, 0:NA], zz[:, 5, 0:NA], ["zz"], ["zz"])
        for h in range(NA):
            TS(ssb[:, 2 * h, :], ssb[:, 2 * h, :], zz[:, 4, h:h + 1], None, ALU.subtract, None, ["ssb", "zz"], ["ssb"])
        if NA < 8:
            lo = 2 * NA
            TT(ssb[:, lo:16, :], ssb[:, lo:16, :], bc(m16[:, lo:16, 0], [128, 16 - lo, 128], 2), ALU.subtract,
               ["ssb", "m16"], ["ssb"])
            ACT(ssb[:, lo:16, :], ssb[:, lo:16, :], AF.Exp, ["ssb"], ["ssb"])
            ssb4 = ssb.rearrange("p (h two) n -> p h two n", two=2)
            TT(ssb4[:, NA:8, 0, :], ssb4[:, NA:8, 0, :], bc(zz[:, 1, NA:8], [128, 8 - NA, 128], 2), ALU.mult,
               ["ssb", "zz"], ["ssb"])
        DMA("sp", ABs[t], ssb.rearrange("p a b -> p (a b)"), "ABs", ["ssb"], [])
        DMA("sp", ths[t], zz[:, 2, :], "ths", ["zz"], [])

    S.barrier()
    if STOP == 2:
        return emit()
    off[0] = base_off
    uS = [alloc(8 * 1024, BF16, [8, 1024]) for _ in range(2)]
    vS = [alloc(8 * 1024, BF16, [8, 1024]) for _ in range(2)]
    ABb = alloc(TB * 2048, F32, [TB, 2048])
    thb = alloc(TB * 8, F32, [TB, 8])
    hnTb = alloc(TB * 1024, BF16, [TB, 8, 128])
    acc = alloc(TB * 1024, F32, [TB, 1024])
    Eb = alloc(1024, F32, [8, 128])
    Cb = [alloc(1024, F32, [8, 128]) for _ in range(3)]
    C2 = [alloc(1024, F32, [8, 128]) for _ in range(2)]
    Gh = [alloc(1024, BF16) for _ in range(16)]
    WT = [alloc(1024, BF16, [8, 128]) for _ in range(2)]
    hl = alloc(1024)
    sq = alloc(1024)
    st1 = alloc(8)
    gfr = alloc(1024)
    DMA("sp", gfr, gfr_d[:, :], "c0", [], ["gfr"])
    uT_v = uT.rearrange("(k p) e -> p k e", p=128)
    v_v = v_d.rearrange("(g t p) d -> g p t d", t=8, p=128)
    geTa = alloc(8 * TB * 128, BF16, [8, TB, 128])
    step = 0
    def emit_WT(par, tt, sl):
        pgb = [2, 3] if par == 0 else [4, 5]
        for nb in range(2):
            TT(WT[par][:, 4 * nb:4 * nb + 4, :], bank(pgb[nb], F32, [4, 128]), geTa[:, 4 * nb:4 * nb + 4, tt, :], ALU.mult,
               ["ps%d" % pgb[nb], "geTa", "WT%d" % par], ["WT%d" % par])

    def emit_out(par, tt, sl):
        for hf_ in range(2):
            for et in range(8):
                MM(bank(6 + hf_), WT[par][:, et, :], vS[sl][:, et, hf_ * 512:(hf_ + 1) * 512], et == 0, et == 7,
                   ["WT%d" % par, "vS%d" % sl], ["ps%d" % (6 + hf_)], et == 7)

    def emit_B2(tt):
        TT(acc[:, tt, 0:512], acc[:, tt, 0:512], bank(6), ALU.add, ["acc", "ps6"], ["acc"])
        TT(acc[:, tt, 512:1024], acc[:, tt, 512:1024], bank(7), ALU.add, ["acc", "ps7"], ["acc"])

    ccnt = [0]
    q1 = []
    q2 = []

    def drain(keep1, keep2):
        while len(q1) > keep1:
            a = q1.pop(0)
            emit_WT(*a)
            while q2:
                emit_B2(q2.pop(0))
            emit_out(*a)
            q2.append(a[1])
        while len(q2) > keep2:
            emit_B2(q2.pop(0))

    for blk in range(NB):
        t0 = blk * TB
        DMA("sp", ABb, ABs[t0:t0 + TB].rearrange("t p c -> p t c"), "ldAB", [], ["ABb"])
        DMA("sp", thb, ths[t0:t0 + TB].rearrange("t p c -> p t c"), "ldth", [], ["thb"])
        DMA("sp", hnTb.rearrange("p t a b -> p t (a b)"), hnTs[t0:t0 + TB].rearrange("t p c -> p t c"), "ldhn", [], ["hnTb"])
        S.op("dve", lambda e: e.memset(acc, 0.0), [], ["acc"])
        for eg in range(16):
            sl = eg % 2
            DMA("pool", uS[sl], uT_v[:, :, eg * 1024:(eg + 1) * 1024], "u%d" % sl, [], ["uS%d" % sl])
            DMA("pool", vS[sl], v_v[eg], "v%d" % sl, [], ["vS%d" % sl])
            drain(0, 1)
            for et in range(8):
                bk = et % 2
                for k in range(8):
                    MM(bank(bk)[:, 0:TB * 128].rearrange("p (a b) -> p a b", a=TB), uS[sl][:, k, et * 128:(et + 1) * 128], hnTb[:, :, k, :], k == 0, k == 7,
                       ["hnTb", "uS%d" % sl], ["ps%d" % bk], k == 7)
                ACT(geTa[:, et, :, :], bank(bk)[:, 0:TB * 128].rearrange("p (a b) -> p a b", a=TB), AF.Gelu, ["ps%d" % bk, "geTa"], ["geTa"])
            for tt in range(TB):
                par = step % 2
                step += 1
                pgb = [2, 3] if par == 0 else [4, 5]
                a_heads = list(range(NA))
                d_heads = list(range(NA, 8))
                order = []
                while a_heads or d_heads:
                    for _ in range(3):
                        if a_heads:
                            order.append(a_heads.pop(0))
                    if d_heads:
                        order.append(d_heads.pop(0))
                for h in order:
                    a_ = ABb[:, tt, (2 * h) * 128 + 8 * eg:(2 * h) * 128 + 8 * eg + 8]
                    b_ = ABb[:, tt, (2 * h + 1) * 128:(2 * h + 2) * 128]
                    gs = 8 * par + h
                    g_ = Gh[gs]
                    gr = "Gh%d" % gs
                    if h < NA:
                        cs = ccnt[0] % 3
                        c2 = ccnt[0] % 2
                        ccnt[0] += 1
                        TT(Cb[cs], bc(a_, [128, 8, 128], 2), bc(b_, [128, 8, 128], 1), ALU.add, ["ABb"], ["Cb%d" % cs])
                        S.op("act", lambda e, cs=cs, c2=c2: e.activation(out=C2[c2], in_=Cb[cs], func=AF.Prelu, alpha=1.0e6),
                             ["Cb%d" % cs], ["C2%d" % c2])
                        ACT(g_, C2[c2].rearrange("p a b -> p (a b)"), AF.Exp, ["C2%d" % c2, "thb"], [gr],
                            bias=thb[:, tt, h:h + 1])
                    else:
                        TT(Eb, bc(a_, [128, 8, 128], 2), bc(b_, [128, 8, 128], 1), ALU.mult, ["ABb"], ["Eb"])
                        E2 = Eb.rearrange("p a b -> p (a b)")
                        STT(g_, E2, thb[:, tt, h:h + 1], E2, ALU.is_ge, ALU.mult, ["Eb", "thb"], [gr])
                for et in range(8):
                    pg = bank(pgb[et // 4], F32, [4, 128])[:, et % 4, :]
                    for h in range(8):
                        MM(pg, Gh[8 * par + h][:, et * 128:(et + 1) * 128], identb, h == 0, h == 7,
                           ["Gh%d" % (8 * par + h), "identb"], ["ps%d" % pgb[et // 4]], (h == 7) and (et % 4 == 3))
                q1.append((par, tt, sl))
                drain(1, 1)
        drain(0, 0)
        for tt in range(TB):
            t = t0 + tt
            DMA("sp", hl, hs[t], "x0", [], ["hl"])
            TT(hl, hl, acc[:, tt, :], ALU.add, ["hl", "acc"], ["hl"])
            rstd_of(hl, "hl", 1024, "st1", sq, st1)
            STT(hl, hl, st1[:, 0:1], gfr, ALU.mult, ALU.mult, ["hl", "st1", "gfr"], ["hl"])
            DMA("sp", out_d[t * 128:(t + 1) * 128, :], hl, "out", ["hl"], [])
    S.barrier()

    return emit()


def host_inputs(x_main, x_pre, P):
    d = dict(P)
    d["xa"] = np.ascontiguousarray(np.concatenate([x_pre, x_main], axis=0))
    return d


def prep_params(norm1_g, w_in, hg_lower_logits, hg_norm_g, gla_w_gate_up, gla_b_gate, gla_norm_g, w_out, norm2_g,
                peer_w_q, peer_sub_keys, peer_u, peer_v, norm_f_g):
    f = np.float32
    cols = np.zeros((128, 16), f)
    cols[:, 0:4] = hg_lower_logits[0].reshape(4, 128).T
    cols[:, 4:8] = hg_lower_logits[1].reshape(4, 128).T
    cols[:, 8:10] = gla_b_gate[0].reshape(2, 128).T
    cols[:, 10] = hg_norm_g[0]
    cols[:, 11] = gla_norm_g[0]
    cst = np.zeros((128, 770), f)
    cst[0:64, 768] = 1.0
    cst[64:128, 769] = 1.0
    cst[:, 0:128] = np.eye(128, dtype=f)
    cst[:, 128:256] = np.triu(np.ones((128, 128), f))
    rp = np.ones((128, 512), f)
    rp[:, 0::128] = 0.0
    cst[:, 256:768] = rp
    rep = lambda g: np.ascontiguousarray(np.broadcast_to(g.reshape(1, -1), (128, g.size))).astype(f)
    kt = np.ascontiguousarray(peer_sub_keys[0].reshape(16, 128, 128).transpose(2, 0, 1).reshape(128, 2048))
    return {
        "w_in": np.ascontiguousarray(w_in[0]),
        "glowT": np.ascontiguousarray(w_in[0][:, 3072:3088].T),
        "w_up": np.ascontiguousarray(gla_w_gate_up[0]),
        "cols": cols,
        "g1r": rep(norm1_g[0]), "g2r": rep(norm2_g[0]), "gfr": rep(norm_f_g),
        "w_out": np.ascontiguousarray(w_out[0]),
        "w_q": np.ascontiguousarray(peer_w_q[0]),
        "kt": kt,
        "uT": np.ascontiguousarray(peer_u[0].T),
        "v": np.ascontiguousarray(peer_v[0]),
        "cst": cst,
    }


def kernel(x, norm1_g, w_in, hg_lower_logits, hg_norm_g, gla_w_gate_up, gla_b_gate, gla_norm_g, w_out, norm2_g,
           peer_w_q, peer_sub_keys, peer_u, peer_v, norm_f_g):
    args = [np.asarray(a, dtype=np.float32) for a in (norm1_g, w_in, hg_lower_logits, hg_norm_g, gla_w_gate_up,
            gla_b_gate, gla_norm_g, w_out, norm2_g, peer_w_q, peer_sub_keys, peer_u, peer_v, norm_f_g)]
    x = np.asarray(x, dtype=np.float32)
    P = prep_params(*args)
    B, T, D = x.shape
    half = T // 2
    in_maps = []
    for c in range(8):
        b, hf = c // 2, c % 2
        xm = x[b, hf * half:(hf + 1) * half]
        xp = x[b, 0:half] if hf == 1 else np.zeros((half, D), np.float32)
        in_maps.append(host_inputs(xm, xp, P))
    nc = build_program(32, 32, 4)
    res = run_bass_kernel_spmd(nc, in_maps, core_ids=list(range(8)))
    out = np.empty((B, T, D), np.float32)
    for c in range(8):
        b, hf = c // 2, c % 2
        out[b, hf * half:(hf + 1) * half] = res.results[c]["out"]
    return out
```

```python
import numpy as np
from contextlib import ExitStack
import concourse.bass as bass
import concourse.mybir as mybir
from concourse.bass_utils import run_bass_kernel_spmd

F32, BF16 = mybir.dt.float32, mybir.dt.bfloat16
AF = mybir.ActivationFunctionType
ALU = mybir.AluOpType
AX = mybir.AxisListType
EPS = 1e-6
STOP = 3
NA = 6
MARGIN = 2.0e-4
LIM = 99
EXP = 0
NEG = -1.0e30


class Sched:
    def __init__(self):
        self.q = {e: [] for e in ("pe", "act", "dve", "pool", "sp")}
        self.cnt = {e: 0 for e in self.q}
        self.dcnt = {}
        self.res = {}
        self.waited = {e: {} for e in self.q}

    def _deps(self, eng, reads, writes):
        deps = []
        for r in reads:
            st = self.res.get(r)
            if st and st[0]:
                deps.append(st[0])
        for w in writes:
            st = self.res.get(w)
            if st:
                if st[0]:
                    deps.append(st[0])
                deps.extend(st[1])
        if eng == "pe":
            deps = [d for d in deps if d[0] != "c_pe"]
        return deps

    def _commit(self, reads, writes, ticket):
        for r in reads:
            self.res.setdefault(r, [None, []])[1].append(ticket)
        for w in writes:
            self.res[w] = [ticket, []]

    def _waits(self, eng, deps):
        wl = self.waited[eng]
        need = {}
        for s, v in deps:
            if wl.get(s, 0) < v:
                need[s] = max(need.get(s, 0), v)
        for s, v in need.items():
            wl[s] = v
            self.q[eng].append(("wait", s, v))

    def op(self, eng, fn, reads=(), writes=(), sig=True):
        self._waits(eng, self._deps(eng, reads, writes))
        if sig:
            self.cnt[eng] += 1
            ticket = ("c_" + eng, self.cnt[eng])
            self.q[eng].append(("op", fn, "c_" + eng))
        else:
            ticket = ("c_" + eng, self.cnt[eng] + 1)
            self.q[eng].append(("op", fn, None))
        self._commit(reads, writes, ticket)

    def dma(self, eng, fn, key, reads=(), writes=()):
        deps = self._deps(eng, reads, writes)
        prev = self.dcnt.get(key, 0)
        if prev:
            deps.append(("d_" + key, prev * 16))
        self._waits(eng, deps)
        self.dcnt[key] = prev + 1
        self.q[eng].append(("dma", fn, "d_" + key))
        self._commit(reads, writes, ("d_" + key, (prev + 1) * 16))

    def barrier(self):
        allsem = [("c_" + e, c) for e, c in self.cnt.items() if c] + \
                 [("d_" + k, c * 16) for k, c in self.dcnt.items()]
        for e in self.q:
            self._waits(e, allsem)
        self.res = {}

    def sem_names(self):
        return ["c_" + e for e in self.cnt] + ["d_" + k for k in self.dcnt]


def build_program(NPRE, NMAIN, TB, debug=False):
    nc = bass.Bass("TRN2", target_bir_lowering=False)
    NT = NPRE + NMAIN
    NB = NMAIN // TB
    D = 1024

    def din(name, shape, dt=F32):
        return nc.dram_tensor(name, list(shape), dt, kind="ExternalInput").ap()

    xa = din("xa", [NT * 128, D])
    w_in = din("w_in", [D, 3600])
    glowT = din("glowT", [16, D])
    w_up = din("w_up", [16, 256])
    cols = din("cols", [128, 16])
    g1r_d = din("g1r", [128, D])
    g2r_d = din("g2r", [128, D])
    gfr_d = din("gfr", [128, D])
    w_out = din("w_out", [D, D])
    w_q = din("w_q", [D, 2048])
    kt_d = din("kt", [128, 2048])
    uT = din("uT", [D, 16384])
    v_d = din("v", [16384, D])
    cst_d = din("cst", [128, 770])
    out_d = nc.dram_tensor("out", [NMAIN * 128, D], F32, kind="ExternalOutput").ap()
    hs = nc.dram_tensor("hs", [NMAIN, 128, D], F32).ap()
    hnTs = nc.dram_tensor("hnTs", [NMAIN, 128, D], BF16).ap()
    ABs = nc.dram_tensor("ABs", [NMAIN, 128, 2048], F32).ap()
    ths = nc.dram_tensor("ths", [NMAIN, 128, 8], F32).ap()
    dbg = {}
    if debug:
        dbg["h"] = nc.dram_tensor("dbg_h", [NMAIN * 128, D], F32, kind="ExternalOutput").ap()

    S = Sched()
    stack = ExitStack()
    NW = 53200
    POOL = stack.enter_context(nc.sbuf_tensor("pool", [128, NW], F32))
    PS = stack.enter_context(nc.psum_tensor("ps", [128, 8, 512], F32))
    off = [0]

    def alloc(n, dt=F32, shape=None):
        nw = n if dt == F32 else (n + 1) // 2
        a = POOL[:, off[0]:off[0] + nw]
        off[0] += nw
        assert off[0] <= NW, ("sbuf overflow", off[0])
        if dt == BF16:
            a = a.bitcast(BF16)
        if shape is not None:
            names = " ".join("abc"[: len(shape)])
            kw = {"abc"[i]: shape[i] for i in range(len(shape) - 1)}
            a = a.rearrange("p (%s) -> p %s" % (names, names), **kw)
        return a

    def bank(k, dt=F32, shape=None):
        a = PS[:, k, :]
        if dt == BF16:
            a = a.bitcast(BF16)
        if shape is not None:
            names = " ".join("abc"[: len(shape)])
            kw = {"abc"[i]: shape[i] for i in range(len(shape) - 1)}
            a = a.rearrange("p (%s) -> p %s" % (names, names), **kw)
        return a

    def MM(out, lhsT, rhs, start, stop, reads, writes, sig):
        S.op("pe", lambda e: e.matmul(out, lhsT=lhsT, rhs=rhs, start=start, stop=stop), reads, writes, sig)

    def ACT(out, in_, func, reads, writes, bias=None, scale=None):
        kw = {}
        if bias is not None:
            kw["bias"] = bias
        if scale is not None:
            kw["scale"] = scale
        S.op("act", lambda e: e.activation(out=out, in_=in_, func=func, **kw), reads, writes)

    def TT(out, in0, in1, op, reads, writes, eng="dve"):
        S.op(eng, lambda e: e.tensor_tensor(out=out, in0=in0, in1=in1, op=op), reads, writes)

    def TS(out, in0, s1, s2, op0, op1, reads, writes):
        if op1 is None:
            S.op("dve", lambda e: e.tensor_scalar(out=out, in0=in0, scalar1=s1, scalar2=None, op0=op0), reads, writes)
        else:
            S.op("dve", lambda e: e.tensor_scalar(out=out, in0=in0, scalar1=s1, scalar2=s2, op0=op0, op1=op1), reads, writes)

    def STT(out, in0, scalar, in1, op0, op1, reads, writes):
        S.op("dve", lambda e: e.scalar_tensor_tensor(out=out, in0=in0, scalar=scalar, in1=in1, op0=op0, op1=op1), reads, writes)

    def CP(eng, out, in_, reads, writes):
        if eng == "act":
            S.op("act", lambda e: e.copy(out=out, in_=in_), reads, writes)
        else:
            S.op(eng, lambda e: e.tensor_copy(out, in_), reads, writes)

    def DMA(eng, out, in_, key, reads, writes):
        S.dma(eng, lambda e: e.dma_start(out=out, in_=in_), key, reads, writes)

    def bc(ap, shape, axis):
        return ap.unsqueeze(axis).to_broadcast(shape)

    def emit():
        sems = {n: stack.enter_context(nc.semaphore(n)) for n in S.sem_names()}
        with stack:
            with nc.Block() as block:
                def replay(name, e):
                    for it in S.q[name]:
                        if it[0] == "wait":
                            e.wait_ge(sems[it[1]], it[2])
                        else:
                            ins = it[1](e)
                            if it[2] is not None:
                                ins.then_inc(sems[it[2]], 16 if it[0] == "dma" else 1)

                @block.tensor
                def _(e):
                    replay("pe", e)

                @block.scalar
                def _(e):
                    replay("act", e)

                @block.vector
                def _(e):
                    replay("dve", e)

                @block.gpsimd
                def _(e):
                    replay("pool", e)

                @block.sync
                def _(e):
                    replay("sp", e)
        return nc

    cst = alloc(770)
    identb = alloc(128, BF16)
    colst = alloc(16)
    small = alloc(16)
    DMA("sp", cst, cst_d[:, :], "c0", [], ["cst"])
    DMA("pool", identb, cst_d[:, 0:128], "c1", [], ["identb"])
    DMA("sp", colst, cols[:, :], "c2", [], ["colst"])
    maskT = cst[:, 128:256]
    rpat = cst[:, 256:768]
    rm = cst[:, 768:770]
    S.op("dve", lambda e: e.memset(small[:, 6:7], 1.0), [], ["small"])
    S.op("dve", lambda e: e.memset(small[:, 7:8], EPS), ["small"], ["small"])
    onec = small[:, 6:7]
    TT(small[:, 8:12], colst[:, 4:8], colst[:, 0:4], ALU.subtract, ["colst", "small"], ["small"])
    ACT(small[:, 0:4], small[:, 8:12], AF.Sigmoid, ["small"], ["small"])
    TS(small[:, 4:6], colst[:, 8:10], -1.0, None, ALU.mult, None, ["colst", "small"], ["small"])
    omlc = small[:, 0:4]
    nbc = small[:, 4:6]
    gnc = colst[:, 10:12]
    base_off = off[0]

    def rstd_of(src, rs, n, tag, sq, st):
        TT(sq[:, 0:n], src, src, ALU.mult, [rs], ["sq"])
        S.op("dve", lambda e: e.reduce_sum(out=st[:, 1:2], in_=sq[:, 0:n], axis=AX.X), ["sq"], [tag])
        TS(st[:, 2:3], st[:, 1:2], 1.0 / n, small[:, 7:8] if False else EPS, ALU.mult, ALU.add, [tag], [tag])
        ACT(st[:, 3:4], st[:, 2:3], AF.Sqrt, [tag], [tag])
        S.op("dve", lambda e: e.reciprocal(out=st[:, 0:1], in_=st[:, 3:4]), [tag], [tag])

    win = alloc(8 * 3600, BF16, [8, 3600])
    wz = alloc(8 * 256, BF16, [8, 256])
    wout = alloc(8 * 1024, BF16, [8, 1024])
    g1r = alloc(1024)
    S32 = alloc(1024, F32, [8, 128])
    Sbf = alloc(1024, BF16, [8, 128])
    glT = alloc(1024)
    wupt = alloc(256)
    xt = [alloc(1024), alloc(1024)]
    sq = alloc(1024)
    st1 = alloc(8)
    st8 = alloc(40, F32, [5, 8])
    xn = alloc(1024, BF16)
    tT = alloc(1024, BF16, [8, 128])
    V = alloc(1024, BF16)
    gsil = alloc(1024)
    nsig = alloc(512, F32, [4, 128])
    kk = alloc(512, F32, [4, 128])
    lf = alloc(768, F32, [6, 128])
    cum = alloc(768, F32, [6, 128])
    ecum = alloc(768, F32, [6, 128])
    encum = alloc(768, F32, [6, 128])
    ez = alloc(256, F32, [2, 128])
    qtilT = alloc(768, BF16, [6, 128])
    ktilT = alloc(768, BF16, [6, 128])
    ktok = alloc(768, BF16, [6, 128])
    eclb = alloc(8)
    sT = alloc(1024, BF16, [8, 128])
    mixed = alloc(1024, BF16)
    qg = alloc(512, BF16, [4, 128])

    w_in_v = w_in.rearrange("(k p) c -> p k c", p=128)
    for k in range(8):
        DMA("pool", win[:, k, :], w_in_v[:, k, :], "w%d" % (k % 4), [], ["win"])
    DMA("pool", wout, w_out.rearrange("(k p) c -> p k c", p=128), "w0", [], ["wout"])
    DMA("sp", g1r, g1r_d[:, :], "c0", [], ["g1r"])
    DMA("sp", glT[0:16, :], glowT[:, :], "c2", [], ["glT"])
    DMA("sp", wupt[0:16, :], w_up[:, :], "c2", [], ["wupt"])
    for k in range(8):
        TS(wout[:, k, :], wout[:, k, :], gnc[:, (k // 4):(k // 4) + 1], None, ALU.mult, None, ["wout", "colst"], ["wout"])
    for k in range(8):
        b = bank(k // 2, F32, [2, 256])
        MM(b[:, k % 2, :], glT[0:16, k * 128:(k + 1) * 128], wupt[0:16, :], True, True,
           ["glT", "wupt"], ["ps%d" % (k // 2)], True)
    for kb in range(4):
        CP("dve", wz[:, 2 * kb:2 * kb + 2, :], bank(kb, F32, [2, 256]), ["ps%d" % kb], ["wz"])
    S.op("dve", lambda e: e.memset(S32, 0.0), [], ["S32"])
    S.op("dve", lambda e: e.memset(Sbf, 0.0), [], ["Sbf"])

    C_HQ, C_HF, C_HI, C_HG, C_GQ, C_GK, C_GV, C_GG = 0, 512, 1024, 1536, 2048, 2304, 2560, 3088

    def fm_block(dst, wsrc, col, wres, pres, last):
        for k in range(8):
            MM(dst, wsrc[:, k, col:col + 128], tT[:, k, :], k == 0, k == 7, [wres, "tT"], [pres], (k == 7) and last)

    def tm_block(bk, col, pres):
        for k in range(8):
            MM(bank(bk), tT[:, k, :], win[:, k, col:col + 512], k == 0, k == 7, ["win", "tT"], [pres], k == 7)

    if STOP == 0:
        S.barrier()
        return emit()
    for c in range(NT):
        main = c >= NPRE
        x_ = xt[c % 2]
        xr = "xt%d" % (c % 2)
        DMA("sp", x_, xa[c * 128:(c + 1) * 128, :], "x%d" % (c % 2), [], [xr])
        rstd_of(x_, xr, 1024, "st1", sq, st1)
        STT(xn, x_, st1[:, 0:1], g1r, ALU.mult, ALU.mult, [xr, "st1", "g1r"], ["xn"])
        for k in range(8):
            S.op("pe", lambda e, k=k: e.transpose(bank(0, BF16, [8, 128])[:, k, :], xn[:, k * 128:(k + 1) * 128], identb),
                 ["xn", "identb"], ["ps0"], k == 7)
        CP("act", tT, bank(0, BF16, [8, 128]), ["ps0"], ["tT"])
        if LIM == 1:
            continue
        tm_block(1, C_HI, "ps1")
        tm_block(2, C_GV, "ps2")
        CP("act", V[:, 0:512], bank(1), ["ps1"], ["V"])
        CP("act", V[:, 512:1024], bank(2), ["ps2"], ["V"])
        if main:
            tm_block(3, C_HG, "ps3")
            tm_block(4, C_GG, "ps4")
            ACT(gsil[:, 0:512], bank(3), AF.Silu, ["ps3"], ["gsil"])
            ACT(gsil[:, 512:1024], bank(4), AF.Silu, ["ps4"], ["gsil"])
        if LIM == 2:
            continue
        b5 = bank(5, F32, [4, 128])
        b6 = bank(6, F32, [4, 128])
        b7 = bank(7, F32, [4, 128])
        b1 = bank(1, F32, [4, 128])
        for b in range(4):
            fm_block(b5[:, b, :], win, C_HF + b * 128, "win", "ps5", b == 3)
        for b in range(2):
            fm_block(b6[:, b, :], win, C_GK + b * 128, "win", "ps6", (b == 1) and not main)
        for b in range(2):
            fm_block(b7[:, b, :], wz, b * 128, "wz", "ps7", b == 1)
        if main:
            for b in range(2):
                fm_block(b6[:, 2 + b, :], win, C_GQ + b * 128, "win", "ps6", b == 1)
            for b in range(4):
                fm_block(b1[:, b, :], win, C_HQ + b * 128, "win", "ps1", b == 3)
        if LIM == 3:
            continue
        ACT(nsig, b5, AF.Sigmoid, ["ps5"], ["nsig"], scale=-1.0)
        TT(kk, nsig, bc(omlc, [128, 4, 128], 2), ALU.mult, ["nsig", "small"], ["kk"])
        ACT(lf[:, 0:4, :], kk, AF.Ln, ["kk", "small"], ["lf"], bias=onec, scale=-1.0)
        for b in range(2):
            ACT(ez[:, b, :], b7[:, b, :], AF.Exp, ["ps7", "small"], ["ez"], bias=nbc[:, b:b + 1], scale=-1.0)
        ACT(lf[:, 4:6, :], ez, AF.Ln, ["ez", "small"], ["lf"], bias=onec, scale=1.0)
        lf2 = lf.rearrange("p a b -> p (a b)")
        cum2 = cum.rearrange("p a b -> p (a b)")
        S.op("dve", lambda e: e.tensor_tensor_scan(out=cum2[:, 0:512], data0=rpat, data1=lf2[:, 0:512], initial=0.0,
                                                   op0=ALU.mult, op1=ALU.add), ["lf", "cst"], ["cum"])
        S.op("dve", lambda e: e.tensor_tensor_scan(out=cum2[:, 512:768], data0=rpat[:, 0:256], data1=lf2[:, 512:768],
                                                   initial=0.0, op0=ALU.mult, op1=ALU.add), ["lf", "cst", "cum"], ["cum"])
        ACT(ecum[:, 0:4, :], cum[:, 0:4, :], AF.Exp, ["cum"], ["ecum"])
        ACT(encum[:, 0:4, :], cum[:, 0:4, :], AF.Exp, ["cum"], ["encum"], scale=-1.0)
        ACT(ecum[:, 4:6, :], cum[:, 4:6, :], AF.Exp, ["cum", "ecum"], ["ecum"], scale=-1.0 / 16.0)
        ACT(encum[:, 4:6, :], cum[:, 4:6, :], AF.Exp, ["cum", "encum"], ["encum"], scale=1.0 / 16.0)
        TT(ktilT[:, 0:4, :], kk, encum[:, 0:4, :], ALU.mult, ["kk", "encum"], ["ktilT"])
        TT(ktilT[:, 4:6, :], b6[:, 0:2, :], encum[:, 4:6, :], ALU.mult, ["ps6", "encum", "ktilT"], ["ktilT"])
        CP("dve", eclb[:, 0:4], ecum[:, 0:4, 127], ["ecum"], ["eclb"])
        CP("dve", eclb[:, 4:8].rearrange("p (a b) -> p a b", a=2), bc(ecum[:, 4:6, 127], [128, 2, 2], 2), ["ecum", "eclb"], ["eclb"])
        if main:
            ACT(nsig, b1, AF.Silu, ["ps1", "kk"], ["nsig"])
            TT(qtilT[:, 0:4, :], nsig, ecum[:, 0:4, :], ALU.mult, ["nsig", "ecum"], ["qtilT"])
            STT(qtilT[:, 4:6, :], b6[:, 2:4, :], 0.125, ecum[:, 4:6, :], ALU.mult, ALU.mult, ["ps6", "ecum", "qtilT"], ["qtilT"])
        if LIM == 4:
            continue
        b2b = bank(2, BF16, [8, 128])
        for b in range(6):
            S.op("pe", lambda e, b=b: e.transpose(b2b[:, b, :], ktilT[:, b, :], identb), ["ktilT", "identb"], ["ps2"], b == 5)
        CP("act", ktok, b2b[:, 0:6, :], ["ps2"], ["ktok"])
        if LIM == 5:
            continue
        if main:
            b3 = bank(3, F32, [4, 128])
            b4 = bank(4, F32, [4, 128])
            for b in range(4):
                MM(b3[:, b, :], ktilT[:, b, :], qtilT[:, b, :], True, True, ["ktilT", "qtilT"], ["ps3"], b == 3)
            for g in range(4):
                TS(qg[:, g, :], qtilT[:, 4 + g // 2, :], rm[:, (g % 2):(g % 2) + 1], None, ALU.mult, None,
                   ["qtilT", "cst", "qg"], ["qg"])
            for g in range(4):
                MM(b4[:, g, :], ktilT[:, 4 + g // 2, :], qg[:, g, :], True, True, ["ktilT", "qg"], ["ps4"], g == 3)
            TT(sT[:, 0:4, :], b3, bc(maskT, [128, 4, 128], 1), ALU.mult, ["ps3", "cst"], ["sT"])
            TT(sT[:, 4:8, :], b4, bc(maskT, [128, 4, 128], 1), ALU.mult, ["ps4", "cst", "sT"], ["sT"])
            if LIM == 55:
                continue
            for hh in range(8):
                ob = (b5 if hh < 4 else b6)[:, hh % 4, :]
                pres = "ps5" if hh < 4 else "ps6"
                MM(ob, sT[:, hh, :], V[:, hh * 128:(hh + 1) * 128], True, False, ["sT", "V"], [pres], False)
                if hh < 4:
                    MM(ob, qtilT[:, hh, :], Sbf[:, hh, :], False, True, ["qtilT", "Sbf"], [pres], hh == 3)
                else:
                    g = hh - 4
                    MM(ob, qg[:, g, :], Sbf[:, hh, :], False, True, ["qg", "Sbf"], [pres], hh == 7)
        if LIM == 6:
            continue
        b0 = bank(0, F32, [4, 128])
        for hh in range(8):
            ub = (b7 if hh < 4 else b0)[:, hh % 4, :]
            pres = "ps7" if hh < 4 else "ps0"
            kb = hh if hh < 4 else 4 + (hh - 4) // 2
            MM(ub, ktok[:, kb, :], V[:, hh * 128:(hh + 1) * 128], True, True, ["ktok", "V"], [pres], hh in (3, 7))
        Us = sq.rearrange("p (a b) -> p a b", a=8)
        TT(Us[:, 0:4, :], b7, bc(eclb[:, 0:4], [128, 4, 128], 2), ALU.mult, ["ps7", "eclb"], ["sq"])
        TT(Us[:, 4:8, :], b0, bc(eclb[:, 4:8], [128, 4, 128], 2), ALU.mult, ["ps0", "eclb", "sq"], ["sq"])
        TT(S32, S32, bc(eclb[:, 0:8], [128, 8, 128], 2), ALU.mult, ["S32", "eclb"], ["S32"])
        TT(S32, S32, Us, ALU.add, ["S32", "sq"], ["S32"])
        CP("act", Sbf, S32, ["S32"], ["Sbf"])
        if LIM == 7:
            continue
        if main:
            sq3 = sq.rearrange("p (a b) -> p a b", a=8)
            ACT(sq3[:, 0:4, :], b5, AF.Square, ["ps5"], ["sq"])
            ACT(sq3[:, 4:8, :], b6, AF.Square, ["ps6", "sq"], ["sq"])
            S.op("dve", lambda e: e.reduce_sum(out=st8[:, 0, :], in_=sq3, axis=AX.X), ["sq"], ["st8"])
            TS(st8[:, 1, :], st8[:, 0, :], 1.0 / 128.0, EPS, ALU.mult, ALU.add, ["st8"], ["st8"])
            ACT(st8[:, 2, :], st8[:, 1, :], AF.Sqrt, ["st8"], ["st8"])
            S.op("dve", lambda e: e.reciprocal(out=st8[:, 3, :], in_=st8[:, 2, :]), ["st8"], ["st8"])
            TT(sq3[:, 0:4, :], b5, bc(st8[:, 3, 0:4], [128, 4, 128], 2), ALU.mult, ["ps5", "st8", "sq"], ["sq"])
            TT(sq3[:, 4:8, :], b6, bc(st8[:, 3, 4:8], [128, 4, 128], 2), ALU.mult, ["ps6", "st8", "sq"], ["sq"])
            TT(mixed, sq, gsil, ALU.mult, ["sq", "gsil"], ["mixed"])
            b1b = bank(1, BF16, [8, 128])
            for k in range(8):
                S.op("pe", lambda e, k=k: e.transpose(b1b[:, k, :], mixed[:, k * 128:(k + 1) * 128], identb),
                     ["mixed", "identb"], ["ps1"], k == 7)
            CP("act", tT, b1b, ["ps1"], ["tT"])
            for hf_ in range(2):
                for k in range(8):
                    MM(bank(2 + hf_), tT[:, k, :], wout[:, k, hf_ * 512:(hf_ + 1) * 512], k == 0, k == 7,
                       ["tT", "wout"], ["ps%d" % (2 + hf_)], k == 7)
            TT(x_[:, 0:512], x_[:, 0:512], bank(2), ALU.add, [xr, "ps2"], [xr])
            TT(x_[:, 512:1024], x_[:, 512:1024], bank(3), ALU.add, [xr, "ps3"], [xr])
            t = c - NPRE
            DMA("sp", hs[t], x_, "hs", [xr], [])
            if debug:
                DMA("sp", dbg["h"][t * 128:(t + 1) * 128, :], x_, "dbgh", [xr], [])

    S.barrier()
    if STOP == 1:
        return emit()
    off[0] = base_off
    wq = alloc(8 * 2048, BF16, [8, 2048])
    kt = alloc(2048, BF16, [16, 128])
    g2r = alloc(1024)
    hl = [alloc(1024), alloc(1024)]
    sq = alloc(1024)
    st1 = alloc(8)
    hn = alloc(1024, BF16)
    hnT = alloc(1024, BF16, [8, 128])
    qT = alloc(2048, BF16, [16, 128])
    ssb = alloc(2048, F32, [16, 128])
    tmpA = alloc(2048, F32, [16, 128])
    m16 = alloc(256, F32, [16, 16])
    candA = alloc(2048, F32, [8, 256])
    ctmpA = alloc(2048, F32, [8, 256])
    c16 = alloc(128, F32, [8, 16])
    e16 = alloc(128, F32, [8, 16])
    zz = alloc(48, F32, [6, 8])
    w_q_v = w_q.rearrange("(k p) c -> p k c", p=128)
    for k in range(8):
        DMA("pool", wq[:, k, :], w_q_v[:, k, :], "w%d" % (k % 4), [], ["wq"])
    DMA("pool", kt.rearrange("p a b -> p (a b)"), kt_d[:, :], "w0", [], ["kt"])
    DMA("sp", g2r, g2r_d[:, :], "c0", [], ["g2r"])
    for t in range(NMAIN):
        h_ = hl[t % 2]
        hr = "hl%d" % (t % 2)
        DMA("sp", h_, hs[t], "x%d" % (t % 2), [], [hr])
        rstd_of(h_, hr, 1024, "st1", sq, st1)
        STT(hn, h_, st1[:, 0:1], g2r, ALU.mult, ALU.mult, [hr, "st1", "g2r"], ["hn"])
        b0b = bank(0, BF16, [8, 128])
        for k in range(8):
            S.op("pe", lambda e, k=k: e.transpose(b0b[:, k, :], hn[:, k * 128:(k + 1) * 128], identb),
                 ["hn", "identb"], ["ps0"], k == 7)
        CP("act", hnT, b0b, ["ps0"], ["hnT"])
        DMA("sp", hnTs[t], hnT.rearrange("p a b -> p (a b)"), "hnTs", ["hnT"], [])
        for cb in range(16):
            bk = 1 + cb // 4
            dst = bank(bk, F32, [4, 128])[:, cb % 4, :]
            for k in range(8):
                MM(dst, wq[:, k, cb * 128:(cb + 1) * 128], hnT[:, k, :], k == 0, k == 7, ["wq", "hnT"], ["ps%d" % bk],
                   (k == 7) and (cb % 4 == 3))
        for q4 in range(4):
            CP("act", qT[:, 4 * q4:4 * q4 + 4, :], bank(1 + q4, F32, [4, 128]), ["ps%d" % (1 + q4)], ["qT"])
        sbk = [5, 6, 7, 0]
        for cb in range(16):
            bk = sbk[cb // 4]
            MM(bank(bk, F32, [4, 128])[:, cb % 4, :], qT[:, cb, :], kt[:, cb, :], True, True, ["qT", "kt"], ["ps%d" % bk],
               cb % 4 == 3)
        for q4 in range(4):
            CP("act", ssb[:, 4 * q4:4 * q4 + 4, :], bank(sbk[q4], F32, [4, 128]), ["ps%d" % sbk[q4]], ["ssb"])
        for cb in range(16):
            S.op("dve", lambda e, cb=cb: e.max(out=m16[:, cb, 0:8], in_=ssb[:, cb, :]), ["ssb"], ["m16a%d" % cb])
        for cb in range(16):
            S.op("dve", lambda e, cb=cb: e.match_replace(out=tmpA[:, cb, :], in_to_replace=m16[:, cb, 0:8],
                                                         in_values=ssb[:, cb, :], imm_value=NEG),
                 ["ssb", "m16a%d" % cb], ["tmp%d" % cb])
        for cb in range(16):
            S.op("dve", lambda e, cb=cb: e.max(out=m16[:, cb, 8:16], in_=tmpA[:, cb, :]), ["tmp%d" % cb], ["m16b%d" % cb])
        for h in range(8):
            TT(candA[:, h, :].rearrange("p (a b) -> p a b", a=16), bc(m16[:, 2 * h, :], [128, 16, 16], 2),
               bc(m16[:, 2 * h + 1, :], [128, 16, 16], 1), ALU.add,
               ["m16a%d" % (2 * h), "m16b%d" % (2 * h), "m16a%d" % (2 * h + 1), "m16b%d" % (2 * h + 1)], ["cand%d" % h])
        for h in range(8):
            S.op("dve", lambda e, h=h: e.max(out=c16[:, h, 0:8], in_=candA[:, h, :]), ["cand%d" % h], ["c16a%d" % h])
        for h in range(8):
            S.op("dve", lambda e, h=h: e.match_replace(out=ctmpA[:, h, :], in_to_replace=c16[:, h, 0:8],
                                                       in_values=candA[:, h, :], imm_value=NEG),
                 ["cand%d" % h, "c16a%d" % h], ["ctmp%d" % h])
        for h in range(8):
            S.op("dve", lambda e, h=h: e.max(out=c16[:, h, 8:16], in_=ctmpA[:, h, :]), ["ctmp%d" % h], ["c16b%d" % h])
        C16R = ["c16a%d" % h for h in range(8)] + ["c16b%d" % h for h in range(8)]
        M16R = ["m16a%d" % cb for cb in range(16)]
        TT(e16, c16, bc(c16[:, :, 0], [128, 8, 16], 2), ALU.subtract, C16R, ["e16"])
        ACT(e16, e16, AF.Exp, ["e16"], ["e16"])
        S.op("dve", lambda e: e.reduce_sum(out=zz[:, 0, :], in_=e16, axis=AX.X), ["e16"], ["zz"])
        S.op("dve", lambda e: e.reciprocal(out=zz[:, 1, :], in_=zz[:, 0, :]), ["zz"], ["zz"])
        STT(zz[:, 2, :], e16[:, :, 15], 1.0 - MARGIN, zz[:, 1, :], ALU.mult, ALU.mult, ["e16", "zz"], ["zz"])
        ACT(zz[:, 3, :], zz[:, 0, :], AF.Ln, ["zz"], ["zz"])
        TS(zz[:, 4, :], c16[:, :, 15], -MARGIN, None, ALU.add, None, C16R + ["zz"], ["zz"])
        TT(zz[:, 5, :], zz[:, 4, :], c16[:, :, 0], ALU.subtract, C16R + ["zz"], ["zz"])
        TT(zz[:, 5, :], zz[:, 5, :], zz[:, 3, :], ALU.subtract, ["zz"], ["zz"])
        if NA > 0:
            CP("dve", zz[:, 2, 0:NA], zz[:, 5, 0:NA], ["zz"], ["zz"])
        for h in range(NA):
            TS(ssb[:, 2 * h, :], ssb[:, 2 * h, :], zz[:, 4, h:h + 1], None, ALU.subtract, None, ["ssb", "zz"], ["ssb"])
        if NA < 8:
            lo = 2 * NA
            TT(ssb[:, lo:16, :], ssb[:, lo:16, :], bc(m16[:, lo:16, 0], [128, 16 - lo, 128], 2), ALU.subtract,
               ["ssb"] + M16R, ["ssb"])
            ACT(ssb[:, lo:16, :], ssb[:, lo:16, :], AF.Exp, ["ssb"], ["ssb"])
            ssb4 = ssb.rearrange("p (h two) n -> p h two n", two=2)
            TT(ssb4[:, NA:8, 0, :], ssb4[:, NA:8, 0, :], bc(zz[:, 1, NA:8], [128, 8 - NA, 128], 2), ALU.mult,
               ["ssb", "zz"], ["ssb"])
        DMA("sp", ABs[t], ssb.rearrange("p a b -> p (a b)"), "ABs", ["ssb"], [])
        DMA("sp", ths[t], zz[:, 2, :], "ths", ["zz"], [])

    S.barrier()
    if STOP == 2:
        return emit()
    off[0] = base_off
    uS = [alloc(8 * 1024, BF16, [8, 1024]) for _ in range(2)]
    vS = [alloc(8 * 1024, BF16, [8, 1024]) for _ in range(2)]
    ABb = alloc(TB * 2048, F32, [TB, 2048])
    thb = alloc(TB * 8, F32, [TB, 8])
    hnTb = alloc(TB * 1024, BF16, [TB, 8, 128])
    acc = alloc(TB * 1024, F32, [TB, 1024])
    Eb = alloc(1024, F32, [8, 128])
    Cb = [alloc(1024, F32, [8, 128]) for _ in range(3)]
    C2 = [alloc(1024, F32, [8, 128]) for _ in range(2)]
    Gh = [alloc(1024, BF16) for _ in range(16)]
    WT = [alloc(1024, BF16, [8, 128]) for _ in range(2)]
    hl = alloc(1024)
    sq = alloc(1024)
    st1 = alloc(8)
    gfr = alloc(1024)
    DMA("sp", gfr, gfr_d[:, :], "c0", [], ["gfr"])
    uT_v = uT.rearrange("(k p) e -> p k e", p=128)
    v_v = v_d.rearrange("(g t p) d -> g p t d", t=8, p=128)
    geTa = alloc(8 * TB * 128, BF16, [8, TB, 128])
    step = 0
    def emit_WT(par, tt, sl):
        pgb = [2, 3] if par == 0 else [4, 5]
        for nb in range(2):
            TT(WT[par][:, 4 * nb:4 * nb + 4, :], bank(pgb[nb], F32, [4, 128]), geTa[:, 4 * nb:4 * nb + 4, tt, :], ALU.mult,
               ["ps%d" % pgb[nb], "geTa", "WT%d" % par], ["WT%d" % par])

    def emit_out(par, tt, sl):
        for hf_ in range(2):
            for et in range(8):
                MM(bank(6 + hf_), WT[par][:, et, :], vS[sl][:, et, hf_ * 512:(hf_ + 1) * 512], et == 0, et == 7,
                   ["WT%d" % par, "vS%d" % sl], ["ps%d" % (6 + hf_)], et == 7)

    def emit_B2(tt):
        TT(acc[:, tt, 0:512], acc[:, tt, 0:512], bank(6), ALU.add, ["acc", "ps6"], ["acc"])
        TT(acc[:, tt, 512:1024], acc[:, tt, 512:1024], bank(7), ALU.add, ["acc", "ps7"], ["acc"])

    ccnt = [0]
    q1 = []
    q2 = []

    def drain(keep1, keep2):
        while len(q1) > keep1:
            a = q1.pop(0)
            emit_WT(*a)
            while q2:
                emit_B2(q2.pop(0))
            emit_out(*a)
            q2.append(a[1])
        while len(q2) > keep2:
            emit_B2(q2.pop(0))

    for blk in range(NB):
        t0 = blk * TB
        DMA("sp", ABb, ABs[t0:t0 + TB].rearrange("t p c -> p t c"), "ldAB", [], ["ABb"])
        DMA("sp", thb, ths[t0:t0 + TB].rearrange("t p c -> p t c"), "ldth", [], ["thb"])
        DMA("sp", hnTb.rearrange("p t a b -> p t (a b)"), hnTs[t0:t0 + TB].rearrange("t p c -> p t c"), "ldhn", [], ["hnTb"])
        S.op("dve", lambda e: e.memset(acc, 0.0), [], ["acc"])
        for eg in range(16):
            sl = eg % 2
            DMA("pool", uS[sl], uT_v[:, :, eg * 1024:(eg + 1) * 1024], "u%d" % sl, [], ["uS%d" % sl])
            DMA("pool", vS[sl], v_v[eg], "v%d" % sl, [], ["vS%d" % sl])
            drain(0, 1)
            for et in range(8):
                bk = et % 2
                for k in range(8):
                    MM(bank(bk)[:, 0:TB * 128].rearrange("p (a b) -> p a b", a=TB), uS[sl][:, k, et * 128:(et + 1) * 128], hnTb[:, :, k, :], k == 0, k == 7,
                       ["hnTb", "uS%d" % sl], ["ps%d" % bk], k == 7)
                ACT(geTa[:, et, :, :], bank(bk)[:, 0:TB * 128].rearrange("p (a b) -> p a b", a=TB), AF.Gelu, ["ps%d" % bk, "geTa"], ["geTa"])
            for tt in range(TB):
                par = step % 2
                step += 1
                pgb = [2, 3] if par == 0 else [4, 5]
                a_heads = list(range(NA))
                d_heads = list(range(NA, 8))
                order = []
                while a_heads or d_heads:
                    for _ in range(3):
                        if a_heads:
                            order.append(a_heads.pop(0))
                    if d_heads:
                        order.append(d_heads.pop(0))
                for h in order:
                    a_ = ABb[:, tt, (2 * h) * 128 + 8 * eg:(2 * h) * 128 + 8 * eg + 8]
                    b_ = ABb[:, tt, (2 * h + 1) * 128:(2 * h + 2) * 128]
                    gs = 8 * par + h
                    g_ = Gh[gs]
                    gr = "Gh%d" % gs
                    if h < NA:
                        cs = ccnt[0] % 3
                        c2 = ccnt[0] % 2
                        ccnt[0] += 1
                        TT(Cb[cs], bc(a_, [128, 8, 128], 2), bc(b_, [128, 8, 128], 1), ALU.add, ["ABb"], ["Cb%d" % cs])
                        S.op("act", lambda e, cs=cs, c2=c2: e.activation(out=C2[c2], in_=Cb[cs], func=AF.Prelu, alpha=1.0e6),
                             ["Cb%d" % cs], ["C2%d" % c2])
                        ACT(g_, C2[c2].rearrange("p a b -> p (a b)"), AF.Exp, ["C2%d" % c2, "thb"], [gr],
                            bias=thb[:, tt, h:h + 1])
                    else:
                        TT(Eb, bc(a_, [128, 8, 128], 2), bc(b_, [128, 8, 128], 1), ALU.mult, ["ABb"], ["Eb"])
                        E2 = Eb.rearrange("p a b -> p (a b)")
                        STT(g_, E2, thb[:, tt, h:h + 1], E2, ALU.is_ge, ALU.mult, ["Eb", "thb"], [gr])
                for et in range(8):
                    pg = bank(pgb[et // 4], F32, [4, 128])[:, et % 4, :]
                    for h in range(8):
                        MM(pg, Gh[8 * par + h][:, et * 128:(et + 1) * 128], identb, h == 0, h == 7,
                           ["Gh%d" % (8 * par + h), "identb"], ["ps%d" % pgb[et // 4]], (h == 7) and (et % 4 == 3))
                q1.append((par, tt, sl))
                drain(1, 1)
        drain(0, 0)
        for tt in range(TB):
            t = t0 + tt
            DMA("sp", hl, hs[t], "x0", [], ["hl"])
            TT(hl, hl, acc[:, tt, :], ALU.add, ["hl", "acc"], ["hl"])
            rstd_of(hl, "hl", 1024, "st1", sq, st1)
            STT(hl, hl, st1[:, 0:1], gfr, ALU.mult, ALU.mult, ["hl", "st1", "gfr"], ["hl"])
            DMA("sp", out_d[t * 128:(t + 1) * 128, :], hl, "out", ["hl"], [])
    S.barrier()

    return emit()


def host_inputs(x_main, x_pre, P):
    d = dict(P)
    d["xa"] = np.ascontiguousarray(np.concatenate([x_pre, x_main], axis=0))
    return d


def prep_params(norm1_g, w_in, hg_lower_logits, hg_norm_g, gla_w_gate_up, gla_b_gate, gla_norm_g, w_out, norm2_g,
                peer_w_q, peer_sub_keys, peer_u, peer_v, norm_f_g):
    f = np.float32
    cols = np.zeros((128, 16), f)
    cols[:, 0:4] = hg_lower_logits[0].reshape(4, 128).T
    cols[:, 4:8] = hg_lower_logits[1].reshape(4, 128).T
    cols[:, 8:10] = gla_b_gate[0].reshape(2, 128).T
    cols[:, 10] = hg_norm_g[0]
    cols[:, 11] = gla_norm_g[0]
    cst = np.zeros((128, 770), f)
    cst[0:64, 768] = 1.0
    cst[64:128, 769] = 1.0
    cst[:, 0:128] = np.eye(128, dtype=f)
    cst[:, 128:256] = np.triu(np.ones((128, 128), f))
    rp = np.ones((128, 512), f)
    rp[:, 0::128] = 0.0
    cst[:, 256:768] = rp
    rep = lambda g: np.ascontiguousarray(np.broadcast_to(g.reshape(1, -1), (128, g.size))).astype(f)
    kt = np.ascontiguousarray(peer_sub_keys[0].reshape(16, 128, 128).transpose(2, 0, 1).reshape(128, 2048))
    return {
        "w_in": np.ascontiguousarray(w_in[0]),
        "glowT": np.ascontiguousarray(w_in[0][:, 3072:3088].T),
        "w_up": np.ascontiguousarray(gla_w_gate_up[0]),
        "cols": cols,
        "g1r": rep(norm1_g[0]), "g2r": rep(norm2_g[0]), "gfr": rep(norm_f_g),
        "w_out": np.ascontiguousarray(w_out[0]),
        "w_q": np.ascontiguousarray(peer_w_q[0]),
        "kt": kt,
        "uT": np.ascontiguousarray(peer_u[0].T),
        "v": np.ascontiguousarray(peer_v[0]),
        "cst": cst,
    }


def kernel(x, norm1_g, w_in, hg_lower_logits, hg_norm_g, gla_w_gate_up, gla_b_gate, gla_norm_g, w_out, norm2_g,
           peer_w_q, peer_sub_keys, peer_u, peer_v, norm_f_g):
    args = [np.asarray(a, dtype=np.float32) for a in (norm1_g, w_in, hg_lower_logits, hg_norm_g, gla_w_gate_up,
            gla_b_gate, gla_norm_g, w_out, norm2_g, peer_w_q, peer_sub_keys, peer_u, peer_v, norm_f_g)]
    x = np.asarray(x, dtype=np.float32)
    P = prep_params(*args)
    B, T, D = x.shape
    half = T // 2
    in_maps = []
    for c in range(8):
        b, hf = c // 2, c % 2
        xm = x[b, hf * half:(hf + 1) * half]
        xp = x[b, 0:half] if hf == 1 else np.zeros((half, D), np.float32)
        in_maps.append(host_inputs(xm, xp, P))
    nc = build_program(32, 32, 4)
    res = run_bass_kernel_spmd(nc, in_maps, core_ids=list(range(8)))
    out = np.empty((B, T, D), np.float32)
    for c in range(8):
        b, hf = c // 2, c % 2
        out[b, hf * half:(hf + 1) * half] = res.results[c]["out"]
    return out
```

```python
import numpy as np
from contextlib import ExitStack
import concourse.bass as bass
import concourse.mybir as mybir
from concourse.bass_utils import run_bass_kernel_spmd

F32, BF16 = mybir.dt.float32, mybir.dt.bfloat16
AF = mybir.ActivationFunctionType
ALU = mybir.AluOpType
AX = mybir.AxisListType
EPS = 1e-6
STOP = 3
NA = 6
MARGIN = 2.0e-4
LIM = 99
EXP = 0
NEG = -1.0e30


class Sched:
    def __init__(self):
        self.q = {e: [] for e in ("pe", "act", "dve", "pool", "sp")}
        self.cnt = {e: 0 for e in self.q}
        self.dcnt = {}
        self.res = {}
        self.waited = {e: {} for e in self.q}

    def _deps(self, eng, reads, writes):
        deps = []
        for r in reads:
            st = self.res.get(r)
            if st and st[0]:
                deps.append(st[0])
        for w in writes:
            st = self.res.get(w)
            if st:
                if st[0]:
                    deps.append(st[0])
                deps.extend(st[1])
        if eng == "pe":
            deps = [d for d in deps if d[0] != "c_pe"]
        return deps

    def _commit(self, reads, writes, ticket):
        for r in reads:
            self.res.setdefault(r, [None, []])[1].append(ticket)
        for w in writes:
            self.res[w] = [ticket, []]

    def _waits(self, eng, deps):
        wl = self.waited[eng]
        need = {}
        for s, v in deps:
            if wl.get(s, 0) < v:
                need[s] = max(need.get(s, 0), v)
        for s, v in need.items():
            wl[s] = v
            self.q[eng].append(("wait", s, v))

    def op(self, eng, fn, reads=(), writes=(), sig=True):
        self._waits(eng, self._deps(eng, reads, writes))
        if sig:
            self.cnt[eng] += 1
            ticket = ("c_" + eng, self.cnt[eng])
            self.q[eng].append(("op", fn, "c_" + eng))
        else:
            ticket = ("c_" + eng, self.cnt[eng] + 1)
            self.q[eng].append(("op", fn, None))
        self._commit(reads, writes, ticket)

    def dma(self, eng, fn, key, reads=(), writes=()):
        deps = self._deps(eng, reads, writes)
        prev = self.dcnt.get(key, 0)
        if prev:
            deps.append(("d_" + key, prev * 16))
        self._waits(eng, deps)
        self.dcnt[key] = prev + 1
        self.q[eng].append(("dma", fn, "d_" + key))
        self._commit(reads, writes, ("d_" + key, (prev + 1) * 16))

    def barrier(self):
        allsem = [("c_" + e, c) for e, c in self.cnt.items() if c] + \
                 [("d_" + k, c * 16) for k, c in self.dcnt.items()]
        for e in self.q:
            self._waits(e, allsem)
        self.res = {}

    def sem_names(self):
        return ["c_" + e for e in self.cnt] + ["d_" + k for k in self.dcnt]


def build_program(NPRE, NMAIN, TB, debug=False):
    nc = bass.Bass("TRN2", target_bir_lowering=False)
    NT = NPRE + NMAIN
    NB = NMAIN // TB
    D = 1024

    def din(name, shape, dt=F32):
        return nc.dram_tensor(name, list(shape), dt, kind="ExternalInput").ap()

    xa = din("xa", [NT * 128, D])
    w_in = din("w_in", [D, 3600])
    glowT = din("glowT", [16, D])
    w_up = din("w_up", [16, 256])
    cols = din("cols", [128, 16])
    g1r_d = din("g1r", [128, D])
    g2r_d = din("g2r", [128, D])
    gfr_d = din("gfr", [128, D])
    w_out = din("w_out", [D, D])
    w_q = din("w_q", [D, 2048])
    kt_d = din("kt", [128, 2048])
    uT = din("uT", [D, 16384])
    v_d = din("v", [16384, D])
    cst_d = din("cst", [128, 770])
    out_d = nc.dram_tensor("out", [NMAIN * 128, D], F32, kind="ExternalOutput").ap()
    hs = nc.dram_tensor("hs", [NMAIN, 128, D], F32).ap()
    hnTs = nc.dram_tensor("hnTs", [NMAIN, 128, D], BF16).ap()
    ABs = nc.dram_tensor("ABs", [NMAIN, 128, 2048], F32).ap()
    ths = nc.dram_tensor("ths", [NMAIN, 128, 8], F32).ap()
    dbg = {}
    if debug:
        dbg["h"] = nc.dram_tensor("dbg_h", [NMAIN * 128, D], F32, kind="ExternalOutput").ap()

    S = Sched()
    stack = ExitStack()
    NW = 53200
    POOL = stack.enter_context(nc.sbuf_tensor("pool", [128, NW], F32))
    PS = stack.enter_context(nc.psum_tensor("ps", [128, 8, 512], F32))
    off = [0]

    def alloc(n, dt=F32, shape=None):
        nw = n if dt == F32 else (n + 1) // 2
        a = POOL[:, off[0]:off[0] + nw]
        off[0] += nw
        assert off[0] <= NW, ("sbuf overflow", off[0])
        if dt == BF16:
            a = a.bitcast(BF16)
        if shape is not None:
            names = " ".join("abc"[: len(shape)])
            kw = {"abc"[i]: shape[i] for i in range(len(shape) - 1)}
            a = a.rearrange("p (%s) -> p %s" % (names, names), **kw)
        return a

    def bank(k, dt=F32, shape=None):
        a = PS[:, k, :]
        if dt == BF16:
            a = a.bitcast(BF16)
        if shape is not None:
            names = " ".join("abc"[: len(shape)])
            kw = {"abc"[i]: shape[i] for i in range(len(shape) - 1)}
            a = a.rearrange("p (%s) -> p %s" % (names, names), **kw)
        return a

    def MM(out, lhsT, rhs, start, stop, reads, writes, sig):
        S.op("pe", lambda e: e.matmul(out, lhsT=lhsT, rhs=rhs, start=start, stop=stop), reads, writes, sig)

    def ACT(out, in_, func, reads, writes, bias=None, scale=None):
        kw = {}
        if bias is not None:
            kw["bias"] = bias
        if scale is not None:
            kw["scale"] = scale
        S.op("act", lambda e: e.activation(out=out, in_=in_, func=func, **kw), reads, writes)

    def TT(out, in0, in1, op, reads, writes, eng="dve"):
        S.op(eng, lambda e: e.tensor_tensor(out=out, in0=in0, in1=in1, op=op), reads, writes)

    def TS(out, in0, s1, s2, op0, op1, reads, writes):
        if op1 is None:
            S.op("dve", lambda e: e.tensor_scalar(out=out, in0=in0, scalar1=s1, scalar2=None, op0=op0), reads, writes)
        else:
            S.op("dve", lambda e: e.tensor_scalar(out=out, in0=in0, scalar1=s1, scalar2=s2, op0=op0, op1=op1), reads, writes)

    def STT(out, in0, scalar, in1, op0, op1, reads, writes):
        S.op("dve", lambda e: e.scalar_tensor_tensor(out=out, in0=in0, scalar=scalar, in1=in1, op0=op0, op1=op1), reads, writes)

    def CP(eng, out, in_, reads, writes):
        if eng == "act":
            S.op("act", lambda e: e.copy(out=out, in_=in_), reads, writes)
        else:
            S.op(eng, lambda e: e.tensor_copy(out, in_), reads, writes)

    def DMA(eng, out, in_, key, reads, writes):
        S.dma(eng, lambda e: e.dma_start(out=out, in_=in_), key, reads, writes)

    def bc(ap, shape, axis):
        return ap.unsqueeze(axis).to_broadcast(shape)

    def emit():
        sems = {n: stack.enter_context(nc.semaphore(n)) for n in S.sem_names()}
        with stack:
            with nc.Block() as block:
                def replay(name, e):
                    for it in S.q[name]:
                        if it[0] == "wait":
                            e.wait_ge(sems[it[1]], it[2])
                        else:
                            ins = it[1](e)
                            if it[2] is not None:
                                ins.then_inc(sems[it[2]], 16 if it[0] == "dma" else 1)

                @block.tensor
                def _(e):
                    replay("pe", e)

                @block.scalar
                def _(e):
                    replay("act", e)

                @block.vector
                def _(e):
                    replay("dve", e)

                @block.gpsimd
                def _(e):
                    replay("pool", e)

                @block.sync
                def _(e):
                    replay("sp", e)
        return nc

    cst = alloc(770)
    identb = alloc(128, BF16)
    colst = alloc(16)
    small = alloc(16)
    DMA("sp", cst, cst_d[:, :], "c0", [], ["cst"])
    DMA("pool", identb, cst_d[:, 0:128], "c1", [], ["identb"])
    DMA("sp", colst, cols[:, :], "c2", [], ["colst"])
    maskT = cst[:, 128:256]
    rpat = cst[:, 256:768]
    rm = cst[:, 768:770]
    S.op("dve", lambda e: e.memset(small[:, 6:7], 1.0), [], ["small"])
    S.op("dve", lambda e: e.memset(small[:, 7:8], EPS), ["small"], ["small"])
    onec = small[:, 6:7]
    TT(small[:, 8:12], colst[:, 4:8], colst[:, 0:4], ALU.subtract, ["colst", "small"], ["small"])
    ACT(small[:, 0:4], small[:, 8:12], AF.Sigmoid, ["small"], ["small"])
    TS(small[:, 4:6], colst[:, 8:10], -1.0, None, ALU.mult, None, ["colst", "small"], ["small"])
    omlc = small[:, 0:4]
    nbc = small[:, 4:6]
    gnc = colst[:, 10:12]
    base_off = off[0]

    def rstd_of(src, rs, n, tag, sq, st):
        TT(sq[:, 0:n], src, src, ALU.mult, [rs], ["sq"])
        S.op("dve", lambda e: e.reduce_sum(out=st[:, 1:2], in_=sq[:, 0:n], axis=AX.X), ["sq"], [tag])
        TS(st[:, 2:3], st[:, 1:2], 1.0 / n, small[:, 7:8] if False else EPS, ALU.mult, ALU.add, [tag], [tag])
        ACT(st[:, 3:4], st[:, 2:3], AF.Sqrt, [tag], [tag])
        S.op("dve", lambda e: e.reciprocal(out=st[:, 0:1], in_=st[:, 3:4]), [tag], [tag])

    win = alloc(8 * 3600, BF16, [8, 3600])
    wz = alloc(8 * 256, BF16, [8, 256])
    wout = alloc(8 * 1024, BF16, [8, 1024])
    g1r = alloc(1024)
    S32 = alloc(1024, F32, [8, 128])
    Sbf = alloc(1024, BF16, [8, 128])
    glT = alloc(1024)
    wupt = alloc(256)
    xt = [alloc(1024), alloc(1024)]
    sq = alloc(1024)
    st1 = alloc(8)
    st8 = alloc(40, F32, [5, 8])
    xn = alloc(1024, BF16)
    tT = alloc(1024, BF16, [8, 128])
    V = alloc(1024, BF16)
    gsil = alloc(1024)
    nsig = alloc(512, F32, [4, 128])
    kk = alloc(512, F32, [4, 128])
    lf = alloc(768, F32, [6, 128])
    cum = alloc(768, F32, [6, 128])
    ecum = alloc(768, F32, [6, 128])
    encum = alloc(768, F32, [6, 128])
    ez = alloc(256, F32, [2, 128])
    qtilT = alloc(768, BF16, [6, 128])
    ktilT = alloc(768, BF16, [6, 128])
    ktok = alloc(768, BF16, [6, 128])
    eclb = alloc(8)
    sT = alloc(1024, BF16, [8, 128])
    mixed = alloc(1024, BF16)
    qg = alloc(512, BF16, [4, 128])

    w_in_v = w_in.rearrange("(k p) c -> p k c", p=128)
    for k in range(8):
        DMA("pool", win[:, k, :], w_in_v[:, k, :], "w%d" % (k % 4), [], ["win"])
    DMA("pool", wout, w_out.rearrange("(k p) c -> p k c", p=128), "w0", [], ["wout"])
    DMA("sp", g1r, g1r_d[:, :], "c0", [], ["g1r"])
    DMA("sp", glT[0:16, :], glowT[:, :], "c2", [], ["glT"])
    DMA("sp", wupt[0:16, :], w_up[:, :], "c2", [], ["wupt"])
    for k in range(8):
        TS(wout[:, k, :], wout[:, k, :], gnc[:, (k // 4):(k // 4) + 1], None, ALU.mult, None, ["wout", "colst"], ["wout"])
    for k in range(8):
        b = bank(k // 2, F32, [2, 256])
        MM(b[:, k % 2, :], glT[0:16, k * 128:(k + 1) * 128], wupt[0:16, :], True, True,
           ["glT", "wupt"], ["ps%d" % (k // 2)], True)
    for kb in range(4):
        CP("dve", wz[:, 2 * kb:2 * kb + 2, :], bank(kb, F32, [2, 256]), ["ps%d" % kb], ["wz"])
    S.op("dve", lambda e: e.memset(S32, 0.0), [], ["S32"])
    S.op("dve", lambda e: e.memset(Sbf, 0.0), [], ["Sbf"])

    C_HQ, C_HF, C_HI, C_HG, C_GQ, C_GK, C_GV, C_GG = 0, 512, 1024, 1536, 2048, 2304, 2560, 3088

    def fm_block(dst, wsrc, col, wres, pres, last):
        for k in range(8):
            MM(dst, wsrc[:, k, col:col + 128], tT[:, k, :], k == 0, k == 7, [wres, "tT"], [pres], (k == 7) and last)

    def tm_block(bk, col, pres):
        for k in range(8):
            MM(bank(bk), tT[:, k, :], win[:, k, col:col + 512], k == 0, k == 7, ["win", "tT"], [pres], k == 7)

    if STOP == 0:
        S.barrier()
        return emit()
    for c in range(NT):
        main = c >= NPRE
        x_ = xt[c % 2]
        xr = "xt%d" % (c % 2)
        DMA("sp", x_, xa[c * 128:(c + 1) * 128, :], "x%d" % (c % 2), [], [xr])
        rstd_of(x_, xr, 1024, "st1", sq, st1)
        STT(xn, x_, st1[:, 0:1], g1r, ALU.mult, ALU.mult, [xr, "st1", "g1r"], ["xn"])
        for k in range(8):
            S.op("pe", lambda e, k=k: e.transpose(bank(0, BF16, [8, 128])[:, k, :], xn[:, k * 128:(k + 1) * 128], identb),
                 ["xn", "identb"], ["ps0"], k == 7)
        CP("act", tT, bank(0, BF16, [8, 128]), ["ps0"], ["tT"])
        if LIM == 1:
            continue
        tm_block(1, C_HI, "ps1")
        tm_block(2, C_GV, "ps2")
        CP("act", V[:, 0:512], bank(1), ["ps1"], ["V"])
        CP("act", V[:, 512:1024], bank(2), ["ps2"], ["V"])
        if main:
            tm_block(3, C_HG, "ps3")
            tm_block(4, C_GG, "ps4")
            ACT(gsil[:, 0:512], bank(3), AF.Silu, ["ps3"], ["gsil"])
            ACT(gsil[:, 512:1024], bank(4), AF.Silu, ["ps4"], ["gsil"])
        if LIM == 2:
            continue
        b5 = bank(5, F32, [4, 128])
        b6 = bank(6, F32, [4, 128])
        b7 = bank(7, F32, [4, 128])
        b1 = bank(1, F32, [4, 128])
        for b in range(4):
            fm_block(b5[:, b, :], win, C_HF + b * 128, "win", "ps5", b == 3)
        for b in range(2):
            fm_block(b6[:, b, :], win, C_GK + b * 128, "win", "ps6", (b == 1) and not main)
        for b in range(2):
            fm_block(b7[:, b, :], wz, b * 128, "wz", "ps7", b == 1)
        if main:
            for b in range(2):
                fm_block(b6[:, 2 + b, :], win, C_GQ + b * 128, "win", "ps6", b == 1)
            for b in range(4):
                fm_block(b1[:, b, :], win, C_HQ + b * 128, "win", "ps1", b == 3)
        if LIM == 3:
            continue
        ACT(nsig, b5, AF.Sigmoid, ["ps5"], ["nsig"], scale=-1.0)
        TT(kk, nsig, bc(omlc, [128, 4, 128], 2), ALU.mult, ["nsig", "small"], ["kk"])
        ACT(lf[:, 0:4, :], kk, AF.Ln, ["kk", "small"], ["lf"], bias=onec, scale=-1.0)
        for b in range(2):
            ACT(ez[:, b, :], b7[:, b, :], AF.Exp, ["ps7", "small"], ["ez"], bias=nbc[:, b:b + 1], scale=-1.0)
        ACT(lf[:, 4:6, :], ez, AF.Ln, ["ez", "small"], ["lf"], bias=onec, scale=1.0)
        lf2 = lf.rearrange("p a b -> p (a b)")
        cum2 = cum.rearrange("p a b -> p (a b)")
        S.op("dve", lambda e: e.tensor_tensor_scan(out=cum2[:, 0:512], data0=rpat, data1=lf2[:, 0:512], initial=0.0,
                                                   op0=ALU.mult, op1=ALU.add), ["lf", "cst"], ["cum"])
        S.op("dve", lambda e: e.tensor_tensor_scan(out=cum2[:, 512:768], data0=rpat[:, 0:256], data1=lf2[:, 512:768],
                                                   initial=0.0, op0=ALU.mult, op1=ALU.add), ["lf", "cst", "cum"], ["cum"])
        ACT(ecum[:, 0:4, :], cum[:, 0:4, :], AF.Exp, ["cum"], ["ecum"])
        ACT(encum[:, 0:4, :], cum[:, 0:4, :], AF.Exp, ["cum"], ["encum"], scale=-1.0)
        ACT(ecum[:, 4:6, :], cum[:, 4:6, :], AF.Exp, ["cum", "ecum"], ["ecum"], scale=-1.0 / 16.0)
        ACT(encum[:, 4:6, :], cum[:, 4:6, :], AF.Exp, ["cum", "encum"], ["encum"], scale=1.0 / 16.0)
        TT(ktilT[:, 0:4, :], kk, encum[:, 0:4, :], ALU.mult, ["kk", "encum"], ["ktilT"])
        TT(ktilT[:, 4:6, :], b6[:, 0:2, :], encum[:, 4:6, :], ALU.mult, ["ps6", "encum", "ktilT"], ["ktilT"])
        CP("dve", eclb[:, 0:4], ecum[:, 0:4, 127], ["ecum"], ["eclb"])
        CP("dve", eclb[:, 4:8].rearrange("p (a b) -> p a b", a=2), bc(ecum[:, 4:6, 127], [128, 2, 2], 2), ["ecum", "eclb"], ["eclb"])
        if main:
            ACT(nsig, b1, AF.Silu, ["ps1", "kk"], ["nsig"])
            TT(qtilT[:, 0:4, :], nsig, ecum[:, 0:4, :], ALU.mult, ["nsig", "ecum"], ["qtilT"])
            STT(qtilT[:, 4:6, :], b6[:, 2:4, :], 0.125, ecum[:, 4:6, :], ALU.mult, ALU.mult, ["ps6", "ecum", "qtilT"], ["qtilT"])
        if LIM == 4:
            continue
        b2b = bank(2, BF16, [8, 128])
        for b in range(6):
            S.op("pe", lambda e, b=b: e.transpose(b2b[:, b, :], ktilT[:, b, :], identb), ["ktilT", "identb"], ["ps2"], b == 5)
        CP("act", ktok, b2b[:, 0:6, :], ["ps2"], ["ktok"])
        if LIM == 5:
            continue
        if main:
            b3 = bank(3, F32, [4, 128])
            b4 = bank(4, F32, [4, 128])
            for b in range(4):
                MM(b3[:, b, :], ktilT[:, b, :], qtilT[:, b, :], True, True, ["ktilT", "qtilT"], ["ps3"], b == 3)
            for g in range(4):
                TS(qg[:, g, :], qtilT[:, 4 + g // 2, :], rm[:, (g % 2):(g % 2) + 1], None, ALU.mult, None,
                   ["qtilT", "cst", "qg"], ["qg"])
            for g in range(4):
                MM(b4[:, g, :], ktilT[:, 4 + g // 2, :], qg[:, g, :], True, True, ["ktilT", "qg"], ["ps4"], g == 3)
            TT(sT[:, 0:4, :], b3, bc(maskT, [128, 4, 128], 1), ALU.mult, ["ps3", "cst"], ["sT"])
            TT(sT[:, 4:8, :], b4, bc(maskT, [128, 4, 128], 1), ALU.mult, ["ps4", "cst", "sT"], ["sT"])
            if LIM == 55:
                continue
            for hh in range(8):
                ob = (b5 if hh < 4 else b6)[:, hh % 4, :]
                pres = "ps5" if hh < 4 else "ps6"
                MM(ob, sT[:, hh, :], V[:, hh * 128:(hh + 1) * 128], True, False, ["sT", "V"], [pres], False)
                if hh < 4:
                    MM(ob, qtilT[:, hh, :], Sbf[:, hh, :], False, True, ["qtilT", "Sbf"], [pres], hh == 3)
                else:
                    g = hh - 4
                    MM(ob, qg[:, g, :], Sbf[:, hh, :], False, True, ["qg", "Sbf"], [pres], hh == 7)
        if LIM == 6:
            continue
        b0 = bank(0, F32, [4, 128])
        for hh in range(8):
            ub = (b7 if hh < 4 else b0)[:, hh % 4, :]
            pres = "ps7" if hh < 4 else "ps0"
            kb = hh if hh < 4 else 4 + (hh - 4) // 2
            MM(ub, ktok[:, kb, :], V[:, hh * 128:(hh + 1) * 128], True, True, ["ktok", "V"], [pres], hh in (3, 7))
        Us = sq.rearrange("p (a b) -> p a b", a=8)
        TT(Us[:, 0:4, :], b7, bc(eclb[:, 0:4], [128, 4, 128], 2), ALU.mult, ["ps7", "eclb"], ["sq"])
        TT(Us[:, 4:8, :], b0, bc(eclb[:, 4:8], [128, 4, 128], 2), ALU.mult, ["ps0", "eclb", "sq"], ["sq"])
        TT(S32, S32, bc(eclb[:, 0:8], [128, 8, 128], 2), ALU.mult, ["S32", "eclb"], ["S32"])
        TT(S32, S32, Us, ALU.add, ["S32", "sq"], ["S32"])
        CP("act", Sbf, S32, ["S32"], ["Sbf"])
        if LIM == 7:
            continue
        if main:
            sq3 = sq.rearrange("p (a b) -> p a b", a=8)
            ACT(sq3[:, 0:4, :], b5, AF.Square, ["ps5"], ["sq"])
            ACT(sq3[:, 4:8, :], b6, AF.Square, ["ps6", "sq"], ["sq"])
            S.op("dve", lambda e: e.reduce_sum(out=st8[:, 0, :], in_=sq3, axis=AX.X), ["sq"], ["st8"])
            TS(st8[:, 1, :], st8[:, 0, :], 1.0 / 128.0, EPS, ALU.mult, ALU.add, ["st8"], ["st8"])
            ACT(st8[:, 2, :], st8[:, 1, :], AF.Sqrt, ["st8"], ["st8"])
            S.op("dve", lambda e: e.reciprocal(out=st8[:, 3, :], in_=st8[:, 2, :]), ["st8"], ["st8"])
            TT(sq3[:, 0:4, :], b5, bc(st8[:, 3, 0:4], [128, 4, 128], 2), ALU.mult, ["ps5", "st8", "sq"], ["sq"])
            TT(sq3[:, 4:8, :], b6, bc(st8[:, 3, 4:8], [128, 4, 128], 2), ALU.mult, ["ps6", "st8", "sq"], ["sq"])
            TT(mixed, sq, gsil, ALU.mult, ["sq", "gsil"], ["mixed"])
            b1b = bank(1, BF16, [8, 128])
            for k in range(8):
                S.op("pe", lambda e, k=k: e.transpose(b1b[:, k, :], mixed[:, k * 128:(k + 1) * 128], identb),
                     ["mixed", "identb"], ["ps1"], k == 7)
            CP("act", tT, b1b, ["ps1"], ["tT"])
            for hf_ in range(2):
                for k in range(8):
                    MM(bank(2 + hf_), tT[:, k, :], wout[:, k, hf_ * 512:(hf_ + 1) * 512], k == 0, k == 7,
                       ["tT", "wout"], ["ps%d" % (2 + hf_)], k == 7)
            TT(x_[:, 0:512], x_[:, 0:512], bank(2), ALU.add, [xr, "ps2"], [xr])
            TT(x_[:, 512:1024], x_[:, 512:1024], bank(3), ALU.add, [xr, "ps3"], [xr])
            t = c - NPRE
            DMA("sp", hs[t], x_, "hs", [xr], [])
            if debug:
                DMA("sp", dbg["h"][t * 128:(t + 1) * 128, :], x_, "dbgh", [xr], [])

    S.barrier()
    if STOP == 1:
        return emit()
    off[0] = base_off
    wq = alloc(8 * 2048, BF16, [8, 2048])
    kt = alloc(2048, BF16, [16, 128])
    g2r = alloc(1024)
    hl = [alloc(1024), alloc(1024)]
    sq = alloc(1024)
    st1 = alloc(8)
    hn = alloc(1024, BF16)
    hnT = alloc(1024, BF16, [8, 128])
    qT = alloc(2048, BF16, [16, 128])
    ssb = alloc(2048, F32, [16, 128])
    tmpA = alloc(2048, F32, [16, 128])
    m16 = alloc(256, F32, [16, 16])
    candA = alloc(2048, F32, [8, 256])
    ctmpA = alloc(2048, F32, [8, 256])
    c16 = alloc(128, F32, [8, 16])
    e16 = alloc(128, F32, [8, 16])
    zz = alloc(48, F32, [6, 8])
    w_q_v = w_q.rearrange("(k p) c -> p k c", p=128)
    for k in range(8):
        DMA("pool", wq[:, k, :], w_q_v[:, k, :], "w%d" % (k % 4), [], ["wq"])
    DMA("pool", kt.rearrange("p a b -> p (a b)"), kt_d[:, :], "w0", [], ["kt"])
    DMA("sp", g2r, g2r_d[:, :], "c0", [], ["g2r"])
    for t in range(NMAIN):
        h_ = hl[t % 2]
        hr = "hl%d" % (t % 2)
        DMA("sp", h_, hs[t], "x%d" % (t % 2), [], [hr])
        rstd_of(h_, hr, 1024, "st1", sq, st1)
        STT(hn, h_, st1[:, 0:1], g2r, ALU.mult, ALU.mult, [hr, "st1", "g2r"], ["hn"])
        b0b = bank(0, BF16, [8, 128])
        for k in range(8):
            S.op("pe", lambda e, k=k: e.transpose(b0b[:, k, :], hn[:, k * 128:(k + 1) * 128], identb),
                 ["hn", "identb"], ["ps0"], k == 7)
        CP("act", hnT, b0b, ["ps0"], ["hnT"])
        DMA("sp", hnTs[t], hnT.rearrange("p a b -> p (a b)"), "hnTs", ["hnT"], [])
        for cb in range(16):
            bk = 1 + cb // 4
            dst = bank(bk, F32, [4, 128])[:, cb % 4, :]
            for k in range(8):
                MM(dst, wq[:, k, cb * 128:(cb + 1) * 128], hnT[:, k, :], k == 0, k == 7, ["wq", "hnT"], ["ps%d" % bk],
                   (k == 7) and (cb % 4 == 3))
        for q4 in range(4):
            CP("act", qT[:, 4 * q4:4 * q4 + 4, :], bank(1 + q4, F32, [4, 128]), ["ps%d" % (1 + q4)], ["qT"])
        sbk = [5, 6, 7, 0]
        for cb in range(16):
            bk = sbk[cb // 4]
            MM(bank(bk, F32, [4, 128])[:, cb % 4, :], qT[:, cb, :], kt[:, cb, :], True, True, ["qT", "kt"], ["ps%d" % bk],
               cb % 4 == 3)
        for q4 in range(4):
            CP("act", ssb[:, 4 * q4:4 * q4 + 4, :], bank(sbk[q4], F32, [4, 128]), ["ps%d" % sbk[q4]], ["ssb"])
        for cb in range(16):
            S.op("dve", lambda e, cb=cb: e.max(out=m16[:, cb, 0:8], in_=ssb[:, cb, :]), ["ssb"], ["m16a%d" % cb])
        for cb in range(16):
            S.op("dve", lambda e, cb=cb: e.match_replace(out=tmpA[:, cb, :], in_to_replace=m16[:, cb, 0:8],
                                                         in_values=ssb[:, cb, :], imm_value=NEG),
                 ["ssb", "m16a%d" % cb], ["tmp%d" % cb])
        for cb in range(16):
            S.op("dve", lambda e, cb=cb: e.max(out=m16[:, cb, 8:16], in_=tmpA[:, cb, :]), ["tmp%d" % cb], ["m16b%d" % cb])
        for h in range(8):
            TT(candA[:, h, :].rearrange("p (a b) -> p a b", a=16), bc(m16[:, 2 * h, :], [128, 16, 16], 2),
               bc(m16[:, 2 * h + 1, :], [128, 16, 16], 1), ALU.add,
               ["m16a%d" % (2 * h), "m16b%d" % (2 * h), "m16a%d" % (2 * h + 1), "m16b%d" % (2 * h + 1)], ["cand%d" % h])
        for h in range(8):
            S.op("dve", lambda e, h=h: e.max(out=c16[:, h, 0:8], in_=candA[:, h, :]), ["cand%d" % h], ["c16a%d" % h])
        for h in range(8):
            S.op("dve", lambda e, h=h: e.match_replace(out=ctmpA[:, h, :], in_to_replace=c16[:, h, 0:8],
                                                       in_values=candA[:, h, :], imm_value=NEG),
                 ["cand%d" % h, "c16a%d" % h], ["ctmp%d" % h])
        for h in range(8):
            S.op("dve", lambda e, h=h: e.max(out=c16[:, h, 8:16], in_=ctmpA[:, h, :]), ["ctmp%d" % h], ["c16b%d" % h])
        C16R = ["c16a%d" % h for h in range(8)] + ["c16b%d" % h for h in range(8)]
        M16R = ["m16a%d" % cb for cb in range(16)]
        TT(e16, c16, bc(c16[:, :, 0], [128, 8, 16], 2), ALU.subtract, C16R, ["e16"])
        ACT(e16, e16, AF.Exp, ["e16"], ["e16"])
        S.op("dve", lambda e: e.reduce_sum(out=zz[:, 0, :], in_=e16, axis=AX.X), ["e16"], ["zz"])
        S.op("dve", lambda e: e.reciprocal(out=zz[:, 1, :], in_=zz[:, 0, :]), ["zz"], ["zz"])
        STT(zz[:, 2, :], e16[:, :, 15], 1.0 - MARGIN, zz[:, 1, :], ALU.mult, ALU.mult, ["e16", "zz"], ["zz"])
        ACT(zz[:, 3, :], zz[:, 0, :], AF.Ln, ["zz"], ["zz"])
        TS(zz[:, 4, :], c16[:, :, 15], -MARGIN, None, ALU.add, None, C16R + ["zz"], ["zz"])
        TT(zz[:, 5, :], zz[:, 4, :], c16[:, :, 0], ALU.subtract, C16R + ["zz"], ["zz"])
        TT(zz[:, 5, :], zz[:, 5, :], zz[:, 3, :], ALU.subtract, ["zz"], ["zz"])
        if NA > 0:
            CP("dve", zz[:, 2, 0:NA], zz[:, 5, 0:NA], ["zz"], ["zz"])
        for h in range(NA):
            TS(ssb[:, 2 * h, :], ssb[:, 2 * h, :], zz[:, 4, h:h + 1], None, ALU.subtract, None, ["ssb", "zz"], ["ssb"])
        if NA < 8:
            lo = 2 * NA
            TT(ssb[:, lo:16, :], ssb[:, lo:16, :], bc(m16[:, lo:16, 0], [128, 16 - lo, 128], 2), ALU.subtract,
               ["ssb"] + M16R, ["ssb"])
            ACT(ssb[:, lo:16, :], ssb[:, lo:16, :], AF.Exp, ["ssb"], ["ssb"])
            ssb4 = ssb.rearrange("p (h two) n -> p h two n", two=2)
            TT(ssb4[:, NA:8, 0, :], ssb4[:, NA:8, 0, :], bc(zz[:, 1, NA:8], [128, 8 - NA, 128], 2), ALU.mult,
               ["ssb", "zz"], ["ssb"])
        DMA("sp", ABs[t], ssb.rearrange("p a b -> p (a b)"), "ABs", ["ssb"], [])
        DMA("sp", ths[t], zz[:, 2, :], "ths", ["zz"], [])

    S.barrier()
    if STOP == 2:
        return emit()
    off[0] = base_off
    uS = [alloc(8 * 1024, BF16, [8, 1024]) for _ in range(2)]
    vS = [alloc(8 * 1024, BF16, [8, 1024]) for _ in range(2)]
    ABb = alloc(TB * 2048, F32, [TB, 2048])
    thb = alloc(TB * 8, F32, [TB, 8])
    hnTb = alloc(TB * 1024, BF16, [TB, 8, 128])
    acc = alloc(TB * 1024, F32, [TB, 1024])
    Eb = alloc(1024, F32, [8, 128])
    Cb = [alloc(1024, F32, [8, 128]) for _ in range(3)]
    C2 = [alloc(1024, F32, [8, 128]) for _ in range(2)]
    Gh = [alloc(1024, BF16) for _ in range(16)]
    WT = [alloc(1024, BF16, [8, 128]) for _ in range(2)]
    hl = alloc(1024)
    sq = Eb.rearrange("p a b -> p (a b)")
    st1 = alloc(8)
    gfr = alloc(1024)
    DMA("sp", gfr, gfr_d[:, :], "c0", [], ["gfr"])
    uT_v = uT.rearrange("(k p) e -> p k e", p=128)
    v_v = v_d.rearrange("(g t p) d -> g p t d", t=8, p=128)
    geTa = [alloc(8 * TB * 128, BF16, [8, TB, 128]) for _ in range(2)]
    step = 0
    def emit_WT(par, tt, sl):
        gT = geTa[sl]
        pgb = [2, 3] if par == 0 else [4, 5]
        for nb in range(2):
            TT(WT[par][:, 4 * nb:4 * nb + 4, :], bank(pgb[nb], F32, [4, 128]), gT[:, 4 * nb:4 * nb + 4, tt, :], ALU.mult,
               ["ps%d" % pgb[nb], "geTa%d" % sl, "WT%d" % par], ["WT%d" % par])

    def emit_out(par, tt, sl):
        for hf_ in range(2):
            for et in range(8):
                MM(bank(6 + hf_), WT[par][:, et, :], vS[sl][:, et, hf_ * 512:(hf_ + 1) * 512], et == 0, et == 7,
                   ["WT%d" % par, "vS%d" % sl], ["ps%d" % (6 + hf_)], et == 7)

    def emit_B2(tt):
        TT(acc[:, tt, 0:512], acc[:, tt, 0:512], bank(6), ALU.add, ["acc", "ps6"], ["acc"])
        TT(acc[:, tt, 512:1024], acc[:, tt, 512:1024], bank(7), ALU.add, ["acc", "ps7"], ["acc"])

    ccnt = [0]
    q1 = []
    q2 = []

    def drain(keep1, keep2):
        while len(q1) > keep1:
            a = q1.pop(0)
            emit_WT(*a)
            while q2:
                emit_B2(q2.pop(0))
            emit_out(*a)
            q2.append(a[1])
        while len(q2) > keep2:
            emit_B2(q2.pop(0))

    for blk in range(NB):
        t0 = blk * TB
        DMA("sp", ABb, ABs[t0:t0 + TB].rearrange("t p c -> p t c"), "ldAB", [], ["ABb"])
        DMA("sp", thb, ths[t0:t0 + TB].rearrange("t p c -> p t c"), "ldth", [], ["thb"])
        DMA("sp", hnTb.rearrange("p t a b -> p t (a b)"), hnTs[t0:t0 + TB].rearrange("t p c -> p t c"), "ldhn", [], ["hnTb"])
        S.op("dve", lambda e: e.memset(acc, 0.0), [], ["acc"])
        def load_eg(eg):
            sl = eg % 2
            DMA("pool", uS[sl], uT_v[:, :, eg * 1024:(eg + 1) * 1024], "u%d" % sl, [], ["uS%d" % sl])
            DMA("pool", vS[sl], v_v[eg], "v%d" % sl, [], ["vS%d" % sl])

        def actT_eg(eg):
            sl = eg % 2
            for et in range(8):
                bk = et % 2
                for k in range(8):
                    MM(bank(bk)[:, 0:TB * 128].rearrange("p (a b) -> p a b", a=TB), uS[sl][:, k, et * 128:(et + 1) * 128],
                       hnTb[:, :, k, :], k == 0, k == 7, ["hnTb", "uS%d" % sl], ["ps%d" % bk], k == 7)
                ACT(geTa[sl][:, et, :, :], bank(bk)[:, 0:TB * 128].rearrange("p (a b) -> p a b", a=TB), AF.Gelu,
                    ["ps%d" % bk, "geTa%d" % sl], ["geTa%d" % sl])

        drain(0, 0)
        load_eg(0)
        actT_eg(0)
        for eg in range(16):
            sl = eg % 2
            for tt in range(TB):
                par = step % 2
                step += 1
                pgb = [2, 3] if par == 0 else [4, 5]
                a_heads = list(range(NA))
                d_heads = list(range(NA, 8))
                order = []
                while a_heads or d_heads:
                    for _ in range(3):
                        if a_heads:
                            order.append(a_heads.pop(0))
                    if d_heads:
                        order.append(d_heads.pop(0))
                for h in order:
                    a_ = ABb[:, tt, (2 * h) * 128 + 8 * eg:(2 * h) * 128 + 8 * eg + 8]
                    b_ = ABb[:, tt, (2 * h + 1) * 128:(2 * h + 2) * 128]
                    gs = 8 * par + h
                    g_ = Gh[gs]
                    gr = "Gh%d" % gs
                    if h < NA:
                        cs = ccnt[0] % 3
                        c2 = ccnt[0] % 2
                        ccnt[0] += 1
                        TT(Cb[cs], bc(a_, [128, 8, 128], 2), bc(b_, [128, 8, 128], 1), ALU.add, ["ABb"], ["Cb%d" % cs])
                        S.op("act", lambda e, cs=cs, c2=c2: e.activation(out=C2[c2], in_=Cb[cs], func=AF.Prelu, alpha=1.0e6),
                             ["Cb%d" % cs], ["C2%d" % c2])
                        ACT(g_, C2[c2].rearrange("p a b -> p (a b)"), AF.Exp, ["C2%d" % c2, "thb"], [gr],
                            bias=thb[:, tt, h:h + 1])
                    else:
                        TT(Eb, bc(a_, [128, 8, 128], 2), bc(b_, [128, 8, 128], 1), ALU.mult, ["ABb"], ["sq"])
                        E2 = Eb.rearrange("p a b -> p (a b)")
                        STT(g_, E2, thb[:, tt, h:h + 1], E2, ALU.is_ge, ALU.mult, ["sq", "thb"], [gr])
                for et in range(8):
                    pg = bank(pgb[et // 4], F32, [4, 128])[:, et % 4, :]
                    for h in range(8):
                        MM(pg, Gh[8 * par + h][:, et * 128:(et + 1) * 128], identb, h == 0, h == 7,
                           ["Gh%d" % (8 * par + h), "identb"], ["ps%d" % pgb[et // 4]], (h == 7) and (et % 4 == 3))
                q1.append((par, tt, sl))
                drain(1, 1)
                if tt == 0 and eg + 1 < 16:
                    load_eg(eg + 1)
                if tt == min(1, TB - 1) and eg + 1 < 16:
                    actT_eg(eg + 1)
        drain(0, 0)
        for tt in range(TB):
            t = t0 + tt
            DMA("sp", hl, hs[t], "x0", [], ["hl"])
            TT(hl, hl, acc[:, tt, :], ALU.add, ["hl", "acc"], ["hl"])
            rstd_of(hl, "hl", 1024, "st1", sq, st1)
            STT(hl, hl, st1[:, 0:1], gfr, ALU.mult, ALU.mult, ["hl", "st1", "gfr"], ["hl"])
            DMA("sp", out_d[t * 128:(t + 1) * 128, :], hl, "out", ["hl"], [])
    S.barrier()

    return emit()


def host_inputs(x_main, x_pre, P):
    d = dict(P)
    d["xa"] = np.ascontiguousarray(np.concatenate([x_pre, x_main], axis=0))
    return d


def prep_params(norm1_g, w_in, hg_lower_logits, hg_norm_g, gla_w_gate_up, gla_b_gate, gla_norm_g, w_out, norm2_g,
                peer_w_q, peer_sub_keys, peer_u, peer_v, norm_f_g):
    f = np.float32
    cols = np.zeros((128, 16), f)
    cols[:, 0:4] = hg_lower_logits[0].reshape(4, 128).T
    cols[:, 4:8] = hg_lower_logits[1].reshape(4, 128).T
    cols[:, 8:10] = gla_b_gate[0].reshape(2, 128).T
    cols[:, 10] = hg_norm_g[0]
    cols[:, 11] = gla_norm_g[0]
    cst = np.zeros((128, 770), f)
    cst[0:64, 768] = 1.0
    cst[64:128, 769] = 1.0
    cst[:, 0:128] = np.eye(128, dtype=f)
    cst[:, 128:256] = np.triu(np.ones((128, 128), f))
    rp = np.ones((128, 512), f)
    rp[:, 0::128] = 0.0
    cst[:, 256:768] = rp
    rep = lambda g: np.ascontiguousarray(np.broadcast_to(g.reshape(1, -1), (128, g.size))).astype(f)
    kt = np.ascontiguousarray(peer_sub_keys[0].reshape(16, 128, 128).transpose(2, 0, 1).reshape(128, 2048))
    return {
        "w_in": np.ascontiguousarray(w_in[0]),
        "glowT": np.ascontiguousarray(w_in[0][:, 3072:3088].T),
        "w_up": np.ascontiguousarray(gla_w_gate_up[0]),
        "cols": cols,
        "g1r": rep(norm1_g[0]), "g2r": rep(norm2_g[0]), "gfr": rep(norm_f_g),
        "w_out": np.ascontiguousarray(w_out[0]),
        "w_q": np.ascontiguousarray(peer_w_q[0]),
        "kt": kt,
        "uT": np.ascontiguousarray(peer_u[0].T),
        "v": np.ascontiguousarray(peer_v[0]),
        "cst": cst,
    }


def kernel(x, norm1_g, w_in, hg_lower_logits, hg_norm_g, gla_w_gate_up, gla_b_gate, gla_norm_g, w_out, norm2_g,
           peer_w_q, peer_sub_keys, peer_u, peer_v, norm_f_g):
    args = [np.asarray(a, dtype=np.float32) for a in (norm1_g, w_in, hg_lower_logits, hg_norm_g, gla_w_gate_up,
            gla_b_gate, gla_norm_g, w_out, norm2_g, peer_w_q, peer_sub_keys, peer_u, peer_v, norm_f_g)]
    x = np.asarray(x, dtype=np.float32)
    P = prep_params(*args)
    B, T, D = x.shape
    half = T // 2
    in_maps = []
    for c in range(8):
        b, hf = c // 2, c % 2
        xm = x[b, hf * half:(hf + 1) * half]
        xp = x[b, 0:half] if hf == 1 else np.zeros((half, D), np.float32)
        in_maps.append(host_inputs(xm, xp, P))
    nc = build_program(32, 32, 4)
    res = run_bass_kernel_spmd(nc, in_maps, core_ids=list(range(8)))
    out = np.empty((B, T, D), np.float32)
    for c in range(8):
        b, hf = c // 2, c % 2
        out[b, hf * half:(hf + 1) * half] = res.results[c]["out"]
    return out
```

```python
import numpy as np
from contextlib import ExitStack
import concourse.bass as bass
import concourse.mybir as mybir
from concourse.bass_utils import run_bass_kernel_spmd

F32, BF16 = mybir.dt.float32, mybir.dt.bfloat16
AF = mybir.ActivationFunctionType
ALU = mybir.AluOpType
AX = mybir.AxisListType
EPS = 1e-6
STOP = 3
NA = 6
MARGIN = 2.0e-4
LIM = 99
EXP = 0
NEG = -1.0e30


class Sched:
    def __init__(self):
        self.q = {e: [] for e in ("pe", "act", "dve", "pool", "sp")}
        self.cnt = {e: 0 for e in self.q}
        self.dcnt = {}
        self.res = {}
        self.waited = {e: {} for e in self.q}

    def _deps(self, eng, reads, writes):
        deps = []
        for r in reads:
            st = self.res.get(r)
            if st and st[0]:
                deps.append(st[0])
        for w in writes:
            st = self.res.get(w)
            if st:
                if st[0]:
                    deps.append(st[0])
                deps.extend(st[1])
        if eng == "pe":
            deps = [d for d in deps if d[0] != "c_pe"]
        return deps

    def _commit(self, reads, writes, ticket):
        for r in reads:
            self.res.setdefault(r, [None, []])[1].append(ticket)
        for w in writes:
            self.res[w] = [ticket, []]

    def _waits(self, eng, deps):
        wl = self.waited[eng]
        need = {}
        for s, v in deps:
            if wl.get(s, 0) < v:
                need[s] = max(need.get(s, 0), v)
        for s, v in need.items():
            wl[s] = v
            self.q[eng].append(("wait", s, v))

    def op(self, eng, fn, reads=(), writes=(), sig=True):
        self._waits(eng, self._deps(eng, reads, writes))
        if sig:
            self.cnt[eng] += 1
            ticket = ("c_" + eng, self.cnt[eng])
            self.q[eng].append(("op", fn, "c_" + eng))
        else:
            ticket = ("c_" + eng, self.cnt[eng] + 1)
            self.q[eng].append(("op", fn, None))
        self._commit(reads, writes, ticket)

    def dma(self, eng, fn, key, reads=(), writes=()):
        deps = self._deps(eng, reads, writes)
        prev = self.dcnt.get(key, 0)
        if prev:
            deps.append(("d_" + key, prev * 16))
        self._waits(eng, deps)
        self.dcnt[key] = prev + 1
        self.q[eng].append(("dma", fn, "d_" + key))
        self._commit(reads, writes, ("d_" + key, (prev + 1) * 16))

    def barrier(self):
        allsem = [("c_" + e, c) for e, c in self.cnt.items() if c] + \
                 [("d_" + k, c * 16) for k, c in self.dcnt.items()]
        for e in self.q:
            self._waits(e, allsem)
        self.res = {}

    def sem_names(self):
        return ["c_" + e for e in self.cnt] + ["d_" + k for k in self.dcnt]


def build_program(NPRE, NMAIN, TB, debug=False):
    nc = bass.Bass("TRN2", target_bir_lowering=False)
    NT = NPRE + NMAIN
    NB = NMAIN // TB
    D = 1024

    def din(name, shape, dt=F32):
        return nc.dram_tensor(name, list(shape), dt, kind="ExternalInput").ap()

    xa = din("xa", [NT * 128, D])
    w_in = din("w_in", [D, 3600])
    glowT = din("glowT", [16, D])
    w_up = din("w_up", [16, 256])
    cols = din("cols", [128, 16])
    g1r_d = din("g1r", [128, D])
    g2r_d = din("g2r", [128, D])
    gfr_d = din("gfr", [128, D])
    w_out = din("w_out", [D, D])
    w_q = din("w_q", [D, 2048])
    kt_d = din("kt", [128, 2048])
    uT = din("uT", [D, 16384])
    v_d = din("v", [16384, D])
    cst_d = din("cst", [128, 770])
    out_d = nc.dram_tensor("out", [NMAIN * 128, D], F32, kind="ExternalOutput").ap()
    hs = nc.dram_tensor("hs", [NMAIN, 128, D], F32).ap()
    hnTs = nc.dram_tensor("hnTs", [NMAIN, 128, D], BF16).ap()
    ABs = nc.dram_tensor("ABs", [NMAIN, 128, 2048], F32).ap()
    ths = nc.dram_tensor("ths", [NMAIN, 128, 8], F32).ap()
    dbg = {}
    if debug:
        dbg["h"] = nc.dram_tensor("dbg_h", [NMAIN * 128, D], F32, kind="ExternalOutput").ap()

    S = Sched()
    stack = ExitStack()
    NW = 53200
    POOL = stack.enter_context(nc.sbuf_tensor("pool", [128, NW], F32))
    PS = stack.enter_context(nc.psum_tensor("ps", [128, 8, 512], F32))
    off = [0]

    def alloc(n, dt=F32, shape=None):
        nw = n if dt == F32 else (n + 1) // 2
        a = POOL[:, off[0]:off[0] + nw]
        off[0] += nw
        assert off[0] <= NW, ("sbuf overflow", off[0])
        if dt == BF16:
            a = a.bitcast(BF16)
        if shape is not None:
            names = " ".join("abc"[: len(shape)])
            kw = {"abc"[i]: shape[i] for i in range(len(shape) - 1)}
            a = a.rearrange("p (%s) -> p %s" % (names, names), **kw)
        return a

    def bank(k, dt=F32, shape=None):
        a = PS[:, k, :]
        if dt == BF16:
            a = a.bitcast(BF16)
        if shape is not None:
            names = " ".join("abc"[: len(shape)])
            kw = {"abc"[i]: shape[i] for i in range(len(shape) - 1)}
            a = a.rearrange("p (%s) -> p %s" % (names, names), **kw)
        return a

    def MM(out, lhsT, rhs, start, stop, reads, writes, sig):
        S.op("pe", lambda e: e.matmul(out, lhsT=lhsT, rhs=rhs, start=start, stop=stop), reads, writes, sig)

    def ACT(out, in_, func, reads, writes, bias=None, scale=None):
        kw = {}
        if bias is not None:
            kw["bias"] = bias
        if scale is not None:
            kw["scale"] = scale
        S.op("act", lambda e: e.activation(out=out, in_=in_, func=func, **kw), reads, writes)

    def TT(out, in0, in1, op, reads, writes, eng="dve"):
        S.op(eng, lambda e: e.tensor_tensor(out=out, in0=in0, in1=in1, op=op), reads, writes)

    def TS(out, in0, s1, s2, op0, op1, reads, writes):
        if op1 is None:
            S.op("dve", lambda e: e.tensor_scalar(out=out, in0=in0, scalar1=s1, scalar2=None, op0=op0), reads, writes)
        else:
            S.op("dve", lambda e: e.tensor_scalar(out=out, in0=in0, scalar1=s1, scalar2=s2, op0=op0, op1=op1), reads, writes)

    def STT(out, in0, scalar, in1, op0, op1, reads, writes):
        S.op("dve", lambda e: e.scalar_tensor_tensor(out=out, in0=in0, scalar=scalar, in1=in1, op0=op0, op1=op1), reads, writes)

    def CP(eng, out, in_, reads, writes):
        if eng == "act":
            S.op("act", lambda e: e.copy(out=out, in_=in_), reads, writes)
        else:
            S.op(eng, lambda e: e.tensor_copy(out, in_), reads, writes)

    def DMA(eng, out, in_, key, reads, writes):
        S.dma(eng, lambda e: e.dma_start(out=out, in_=in_), key, reads, writes)

    def bc(ap, shape, axis):
        return ap.unsqueeze(axis).to_broadcast(shape)

    def emit():
        sems = {n: stack.enter_context(nc.semaphore(n)) for n in S.sem_names()}
        with stack:
            with nc.Block() as block:
                def replay(name, e):
                    for it in S.q[name]:
                        if it[0] == "wait":
                            e.wait_ge(sems[it[1]], it[2])
                        else:
                            ins = it[1](e)
                            if it[2] is not None:
                                ins.then_inc(sems[it[2]], 16 if it[0] == "dma" else 1)

                @block.tensor
                def _(e):
                    replay("pe", e)

                @block.scalar
                def _(e):
                    replay("act", e)

                @block.vector
                def _(e):
                    replay("dve", e)

                @block.gpsimd
                def _(e):
                    replay("pool", e)

                @block.sync
                def _(e):
                    replay("sp", e)
        return nc

    cst = alloc(770)
    identb = alloc(128, BF16)
    colst = alloc(16)
    small = alloc(16)
    DMA("sp", cst, cst_d[:, :], "c0", [], ["cst"])
    DMA("pool", identb, cst_d[:, 0:128], "c1", [], ["identb"])
    DMA("sp", colst, cols[:, :], "c2", [], ["colst"])
    maskT = cst[:, 128:256]
    rpat = cst[:, 256:768]
    rm = cst[:, 768:770]
    S.op("dve", lambda e: e.memset(small[:, 6:7], 1.0), [], ["small"])
    S.op("dve", lambda e: e.memset(small[:, 7:8], EPS), ["small"], ["small"])
    onec = small[:, 6:7]
    TT(small[:, 8:12], colst[:, 4:8], colst[:, 0:4], ALU.subtract, ["colst", "small"], ["small"])
    ACT(small[:, 0:4], small[:, 8:12], AF.Sigmoid, ["small"], ["small"])
    TS(small[:, 4:6], colst[:, 8:10], -1.0, None, ALU.mult, None, ["colst", "small"], ["small"])
    omlc = small[:, 0:4]
    nbc = small[:, 4:6]
    gnc = colst[:, 10:12]
    base_off = off[0]

    def rstd_of(src, rs, n, tag, sq, st):
        TT(sq[:, 0:n], src, src, ALU.mult, [rs], ["sq"])
        S.op("dve", lambda e: e.reduce_sum(out=st[:, 1:2], in_=sq[:, 0:n], axis=AX.X), ["sq"], [tag])
        TS(st[:, 2:3], st[:, 1:2], 1.0 / n, small[:, 7:8] if False else EPS, ALU.mult, ALU.add, [tag], [tag])
        ACT(st[:, 3:4], st[:, 2:3], AF.Sqrt, [tag], [tag])
        S.op("dve", lambda e: e.reciprocal(out=st[:, 0:1], in_=st[:, 3:4]), [tag], [tag])

    win = alloc(8 * 3600, BF16, [8, 3600])
    wz = alloc(8 * 256, BF16, [8, 256])
    wout = alloc(8 * 1024, BF16, [8, 1024])
    g1r = alloc(1024)
    S32 = alloc(1024, F32, [8, 128])
    Sbf = alloc(1024, BF16, [8, 128])
    glT = alloc(1024)
    wupt = alloc(256)
    xt = [alloc(1024), alloc(1024)]
    sq = alloc(1024)
    st1 = alloc(8)
    st8 = alloc(40, F32, [5, 8])
    xn = alloc(1024, BF16)
    tT = alloc(1024, BF16, [8, 128])
    V = alloc(1024, BF16)
    gsil = alloc(1024)
    nsig = alloc(512, F32, [4, 128])
    kk = alloc(512, F32, [4, 128])
    lf = alloc(768, F32, [6, 128])
    cum = alloc(768, F32, [6, 128])
    ecum = alloc(768, F32, [6, 128])
    encum = alloc(768, F32, [6, 128])
    ez = alloc(256, F32, [2, 128])
    qtilT = alloc(768, BF16, [6, 128])
    ktilT = alloc(768, BF16, [6, 128])
    ktok = alloc(768, BF16, [6, 128])
    eclb = alloc(8)
    sT = alloc(1024, BF16, [8, 128])
    mixed = alloc(1024, BF16)
    qg = alloc(512, BF16, [4, 128])

    w_in_v = w_in.rearrange("(k p) c -> p k c", p=128)
    for k in range(8):
        DMA("pool", win[:, k, :], w_in_v[:, k, :], "w%d" % (k % 4), [], ["win"])
    DMA("pool", wout, w_out.rearrange("(k p) c -> p k c", p=128), "w0", [], ["wout"])
    DMA("sp", g1r, g1r_d[:, :], "c0", [], ["g1r"])
    DMA("sp", glT[0:16, :], glowT[:, :], "c2", [], ["glT"])
    DMA("sp", wupt[0:16, :], w_up[:, :], "c2", [], ["wupt"])
    for k in range(8):
        TS(wout[:, k, :], wout[:, k, :], gnc[:, (k // 4):(k // 4) + 1], None, ALU.mult, None, ["wout", "colst"], ["wout"])
    for k in range(8):
        b = bank(k // 2, F32, [2, 256])
        MM(b[:, k % 2, :], glT[0:16, k * 128:(k + 1) * 128], wupt[0:16, :], True, True,
           ["glT", "wupt"], ["ps%d" % (k // 2)], True)
    for kb in range(4):
        CP("dve", wz[:, 2 * kb:2 * kb + 2, :], bank(kb, F32, [2, 256]), ["ps%d" % kb], ["wz"])
    S.op("dve", lambda e: e.memset(S32, 0.0), [], ["S32"])
    S.op("dve", lambda e: e.memset(Sbf, 0.0), [], ["Sbf"])

    C_HQ, C_HF, C_HI, C_HG, C_GQ, C_GK, C_GV, C_GG = 0, 512, 1024, 1536, 2048, 2304, 2560, 3088

    def fm_block(dst, wsrc, col, wres, pres, last):
        for k in range(8):
            MM(dst, wsrc[:, k, col:col + 128], tT[:, k, :], k == 0, k == 7, [wres, "tT"], [pres], (k == 7) and last)

    def tm_block(bk, col, pres):
        for k in range(8):
            MM(bank(bk), tT[:, k, :], win[:, k, col:col + 512], k == 0, k == 7, ["win", "tT"], [pres], k == 7)

    if STOP == 0:
        S.barrier()
        return emit()
    for c in range(NT):
        main = c >= NPRE
        x_ = xt[c % 2]
        xr = "xt%d" % (c % 2)
        DMA("sp", x_, xa[c * 128:(c + 1) * 128, :], "x%d" % (c % 2), [], [xr])
        rstd_of(x_, xr, 1024, "st1", sq, st1)
        STT(xn, x_, st1[:, 0:1], g1r, ALU.mult, ALU.mult, [xr, "st1", "g1r"], ["xn"])
        for k in range(8):
            S.op("pe", lambda e, k=k: e.transpose(bank(0, BF16, [8, 128])[:, k, :], xn[:, k * 128:(k + 1) * 128], identb),
                 ["xn", "identb"], ["ps0"], k == 7)
        CP("act", tT, bank(0, BF16, [8, 128]), ["ps0"], ["tT"])
        if LIM == 1:
            continue
        tm_block(1, C_HI, "ps1")
        tm_block(2, C_GV, "ps2")
        CP("act", V[:, 0:512], bank(1), ["ps1"], ["V"])
        CP("act", V[:, 512:1024], bank(2), ["ps2"], ["V"])
        if main:
            tm_block(3, C_HG, "ps3")
            tm_block(4, C_GG, "ps4")
            ACT(gsil[:, 0:512], bank(3), AF.Silu, ["ps3"], ["gsil"])
            ACT(gsil[:, 512:1024], bank(4), AF.Silu, ["ps4"], ["gsil"])
        if LIM == 2:
            continue
        b5 = bank(5, F32, [4, 128])
        b6 = bank(6, F32, [4, 128])
        b7 = bank(7, F32, [4, 128])
        b1 = bank(1, F32, [4, 128])
        for b in range(4):
            fm_block(b5[:, b, :], win, C_HF + b * 128, "win", "ps5", b == 3)
        for b in range(2):
            fm_block(b6[:, b, :], win, C_GK + b * 128, "win", "ps6", (b == 1) and not main)
        for b in range(2):
            fm_block(b7[:, b, :], wz, b * 128, "wz", "ps7", b == 1)
        if main:
            for b in range(2):
                fm_block(b6[:, 2 + b, :], win, C_GQ + b * 128, "win", "ps6", b == 1)
            for b in range(4):
                fm_block(b1[:, b, :], win, C_HQ + b * 128, "win", "ps1", b == 3)
        if LIM == 3:
            continue
        ACT(nsig, b5, AF.Sigmoid, ["ps5"], ["nsig"], scale=-1.0)
        TT(kk, nsig, bc(omlc, [128, 4, 128], 2), ALU.mult, ["nsig", "small"], ["kk"])
        ACT(lf[:, 0:4, :], kk, AF.Ln, ["kk", "small"], ["lf"], bias=onec, scale=-1.0)
        for b in range(2):
            ACT(ez[:, b, :], b7[:, b, :], AF.Exp, ["ps7", "small"], ["ez"], bias=nbc[:, b:b + 1], scale=-1.0)
        ACT(lf[:, 4:6, :], ez, AF.Ln, ["ez", "small"], ["lf"], bias=onec, scale=1.0)
        lf2 = lf.rearrange("p a b -> p (a b)")
        cum2 = cum.rearrange("p a b -> p (a b)")
        S.op("dve", lambda e: e.tensor_tensor_scan(out=cum2[:, 0:512], data0=rpat, data1=lf2[:, 0:512], initial=0.0,
                                                   op0=ALU.mult, op1=ALU.add), ["lf", "cst"], ["cum"])
        S.op("dve", lambda e: e.tensor_tensor_scan(out=cum2[:, 512:768], data0=rpat[:, 0:256], data1=lf2[:, 512:768],
                                                   initial=0.0, op0=ALU.mult, op1=ALU.add), ["lf", "cst", "cum"], ["cum"])
        ACT(ecum[:, 0:4, :], cum[:, 0:4, :], AF.Exp, ["cum"], ["ecum"])
        ACT(encum[:, 0:4, :], cum[:, 0:4, :], AF.Exp, ["cum"], ["encum"], scale=-1.0)
        ACT(ecum[:, 4:6, :], cum[:, 4:6, :], AF.Exp, ["cum", "ecum"], ["ecum"], scale=-1.0 / 16.0)
        ACT(encum[:, 4:6, :], cum[:, 4:6, :], AF.Exp, ["cum", "encum"], ["encum"], scale=1.0 / 16.0)
        TT(ktilT[:, 0:4, :], kk, encum[:, 0:4, :], ALU.mult, ["kk", "encum"], ["ktilT"])
        TT(ktilT[:, 4:6, :], b6[:, 0:2, :], encum[:, 4:6, :], ALU.mult, ["ps6", "encum", "ktilT"], ["ktilT"])
        CP("dve", eclb[:, 0:4], ecum[:, 0:4, 127], ["ecum"], ["eclb"])
        CP("dve", eclb[:, 4:8].rearrange("p (a b) -> p a b", a=2), bc(ecum[:, 4:6, 127], [128, 2, 2], 2), ["ecum", "eclb"], ["eclb"])
        if main:
            ACT(nsig, b1, AF.Silu, ["ps1", "kk"], ["nsig"])
            TT(qtilT[:, 0:4, :], nsig, ecum[:, 0:4, :], ALU.mult, ["nsig", "ecum"], ["qtilT"])
            STT(qtilT[:, 4:6, :], b6[:, 2:4, :], 0.125, ecum[:, 4:6, :], ALU.mult, ALU.mult, ["ps6", "ecum", "qtilT"], ["qtilT"])
        if LIM == 4:
            continue
        b2b = bank(2, BF16, [8, 128])
        for b in range(6):
            S.op("pe", lambda e, b=b: e.transpose(b2b[:, b, :], ktilT[:, b, :], identb), ["ktilT", "identb"], ["ps2"], b == 5)
        CP("act", ktok, b2b[:, 0:6, :], ["ps2"], ["ktok"])
        if LIM == 5:
            continue
        if main:
            b3 = bank(3, F32, [4, 128])
            b4 = bank(4, F32, [4, 128])
            for b in range(4):
                MM(b3[:, b, :], ktilT[:, b, :], qtilT[:, b, :], True, True, ["ktilT", "qtilT"], ["ps3"], b == 3)
            for g in range(4):
                TS(qg[:, g, :], qtilT[:, 4 + g // 2, :], rm[:, (g % 2):(g % 2) + 1], None, ALU.mult, None,
                   ["qtilT", "cst", "qg"], ["qg"])
            for g in range(4):
                MM(b4[:, g, :], ktilT[:, 4 + g // 2, :], qg[:, g, :], True, True, ["ktilT", "qg"], ["ps4"], g == 3)
            TT(sT[:, 0:4, :], b3, bc(maskT, [128, 4, 128], 1), ALU.mult, ["ps3", "cst"], ["sT"])
            TT(sT[:, 4:8, :], b4, bc(maskT, [128, 4, 128], 1), ALU.mult, ["ps4", "cst", "sT"], ["sT"])
            if LIM == 55:
                continue
            for hh in range(8):
                ob = (b5 if hh < 4 else b6)[:, hh % 4, :]
                pres = "ps5" if hh < 4 else "ps6"
                MM(ob, sT[:, hh, :], V[:, hh * 128:(hh + 1) * 128], True, False, ["sT", "V"], [pres], False)
                if hh < 4:
                    MM(ob, qtilT[:, hh, :], Sbf[:, hh, :], False, True, ["qtilT", "Sbf"], [pres], hh == 3)
                else:
                    g = hh - 4
                    MM(ob, qg[:, g, :], Sbf[:, hh, :], False, True, ["qg", "Sbf"], [pres], hh == 7)
        if LIM == 6:
            continue
        b0 = bank(0, F32, [4, 128])
        for hh in range(8):
            ub = (b7 if hh < 4 else b0)[:, hh % 4, :]
            pres = "ps7" if hh < 4 else "ps0"
            kb = hh if hh < 4 else 4 + (hh - 4) // 2
            MM(ub, ktok[:, kb, :], V[:, hh * 128:(hh + 1) * 128], True, True, ["ktok", "V"], [pres], hh in (3, 7))
        Us = sq.rearrange("p (a b) -> p a b", a=8)
        TT(Us[:, 0:4, :], b7, bc(eclb[:, 0:4], [128, 4, 128], 2), ALU.mult, ["ps7", "eclb"], ["sq"])
        TT(Us[:, 4:8, :], b0, bc(eclb[:, 4:8], [128, 4, 128], 2), ALU.mult, ["ps0", "eclb", "sq"], ["sq"])
        TT(S32, S32, bc(eclb[:, 0:8], [128, 8, 128], 2), ALU.mult, ["S32", "eclb"], ["S32"])
        TT(S32, S32, Us, ALU.add, ["S32", "sq"], ["S32"])
        CP("act", Sbf, S32, ["S32"], ["Sbf"])
        if LIM == 7:
            continue
        if main:
            sq3 = sq.rearrange("p (a b) -> p a b", a=8)
            ACT(sq3[:, 0:4, :], b5, AF.Square, ["ps5"], ["sq"])
            ACT(sq3[:, 4:8, :], b6, AF.Square, ["ps6", "sq"], ["sq"])
            S.op("dve", lambda e: e.reduce_sum(out=st8[:, 0, :], in_=sq3, axis=AX.X), ["sq"], ["st8"])
            TS(st8[:, 1, :], st8[:, 0, :], 1.0 / 128.0, EPS, ALU.mult, ALU.add, ["st8"], ["st8"])
            ACT(st8[:, 2, :], st8[:, 1, :], AF.Sqrt, ["st8"], ["st8"])
            S.op("dve", lambda e: e.reciprocal(out=st8[:, 3, :], in_=st8[:, 2, :]), ["st8"], ["st8"])
            TT(sq3[:, 0:4, :], b5, bc(st8[:, 3, 0:4], [128, 4, 128], 2), ALU.mult, ["ps5", "st8", "sq"], ["sq"])
            TT(sq3[:, 4:8, :], b6, bc(st8[:, 3, 4:8], [128, 4, 128], 2), ALU.mult, ["ps6", "st8", "sq"], ["sq"])
            TT(mixed, sq, gsil, ALU.mult, ["sq", "gsil"], ["mixed"])
            b1b = bank(1, BF16, [8, 128])
            for k in range(8):
                S.op("pe", lambda e, k=k: e.transpose(b1b[:, k, :], mixed[:, k * 128:(k + 1) * 128], identb),
                     ["mixed", "identb"], ["ps1"], k == 7)
            CP("act", tT, b1b, ["ps1"], ["tT"])
            for hf_ in range(2):
                for k in range(8):
                    MM(bank(2 + hf_), tT[:, k, :], wout[:, k, hf_ * 512:(hf_ + 1) * 512], k == 0, k == 7,
                       ["tT", "wout"], ["ps%d" % (2 + hf_)], k == 7)
            TT(x_[:, 0:512], x_[:, 0:512], bank(2), ALU.add, [xr, "ps2"], [xr])
            TT(x_[:, 512:1024], x_[:, 512:1024], bank(3), ALU.add, [xr, "ps3"], [xr])
            t = c - NPRE
            DMA("sp", hs[t], x_, "hs", [xr], [])
            if debug:
                DMA("sp", dbg["h"][t * 128:(t + 1) * 128, :], x_, "dbgh", [xr], [])

    S.barrier()
    if STOP == 1:
        return emit()
    off[0] = base_off
    wq = alloc(8 * 2048, BF16, [8, 2048])
    kt = alloc(2048, BF16, [16, 128])
    g2r = alloc(1024)
    hl = [alloc(1024), alloc(1024)]
    sq = alloc(1024)
    st1 = alloc(8)
    hn = alloc(1024, BF16)
    hnT = alloc(1024, BF16, [8, 128])
    qT = alloc(2048, BF16, [16, 128])
    ssb = alloc(2048, F32, [16, 128])
    tmpA = alloc(2048, F32, [16, 128])
    m16 = alloc(256, F32, [16, 16])
    candA = alloc(2048, F32, [8, 256])
    ctmpA = alloc(2048, F32, [8, 256])
    c16 = alloc(128, F32, [8, 16])
    e16 = alloc(128, F32, [8, 16])
    zz = alloc(48, F32, [6, 8])
    w_q_v = w_q.rearrange("(k p) c -> p k c", p=128)
    for k in range(8):
        DMA("pool", wq[:, k, :], w_q_v[:, k, :], "w%d" % (k % 4), [], ["wq"])
    DMA("pool", kt.rearrange("p a b -> p (a b)"), kt_d[:, :], "w0", [], ["kt"])
    DMA("sp", g2r, g2r_d[:, :], "c0", [], ["g2r"])
    for t in range(NMAIN):
        h_ = hl[t % 2]
        hr = "hl%d" % (t % 2)
        DMA("sp", h_, hs[t], "x%d" % (t % 2), [], [hr])
        rstd_of(h_, hr, 1024, "st1", sq, st1)
        STT(hn, h_, st1[:, 0:1], g2r, ALU.mult, ALU.mult, [hr, "st1", "g2r"], ["hn"])
        b0b = bank(0, BF16, [8, 128])
        for k in range(8):
            S.op("pe", lambda e, k=k: e.transpose(b0b[:, k, :], hn[:, k * 128:(k + 1) * 128], identb),
                 ["hn", "identb"], ["ps0"], k == 7)
        CP("act", hnT, b0b, ["ps0"], ["hnT"])
        DMA("sp", hnTs[t], hnT.rearrange("p a b -> p (a b)"), "hnTs", ["hnT"], [])
        for cb in range(16):
            bk = 1 + cb // 4
            dst = bank(bk, F32, [4, 128])[:, cb % 4, :]
            for k in range(8):
                MM(dst, wq[:, k, cb * 128:(cb + 1) * 128], hnT[:, k, :], k == 0, k == 7, ["wq", "hnT"], ["ps%d" % bk],
                   (k == 7) and (cb % 4 == 3))
        for q4 in range(4):
            CP("act", qT[:, 4 * q4:4 * q4 + 4, :], bank(1 + q4, F32, [4, 128]), ["ps%d" % (1 + q4)], ["qT"])
        sbk = [5, 6, 7, 0]
        for cb in range(16):
            bk = sbk[cb // 4]
            MM(bank(bk, F32, [4, 128])[:, cb % 4, :], qT[:, cb, :], kt[:, cb, :], True, True, ["qT", "kt"], ["ps%d" % bk],
               cb % 4 == 3)
        for q4 in range(4):
            CP("act", ssb[:, 4 * q4:4 * q4 + 4, :], bank(sbk[q4], F32, [4, 128]), ["ps%d" % sbk[q4]], ["ssb"])
        for cb in range(16):
            S.op("dve", lambda e, cb=cb: e.max(out=m16[:, cb, 0:8], in_=ssb[:, cb, :]), ["ssb"], ["m16a%d" % cb])
        for cb in range(16):
            S.op("dve", lambda e, cb=cb: e.match_replace(out=tmpA[:, cb, :], in_to_replace=m16[:, cb, 0:8],
                                                         in_values=ssb[:, cb, :], imm_value=NEG),
                 ["ssb", "m16a%d" % cb], ["tmp%d" % cb])
        for cb in range(16):
            S.op("dve", lambda e, cb=cb: e.max(out=m16[:, cb, 8:16], in_=tmpA[:, cb, :]), ["tmp%d" % cb], ["m16b%d" % cb])
        for h in range(8):
            TT(candA[:, h, :].rearrange("p (a b) -> p a b", a=16), bc(m16[:, 2 * h, :], [128, 16, 16], 2),
               bc(m16[:, 2 * h + 1, :], [128, 16, 16], 1), ALU.add,
               ["m16a%d" % (2 * h), "m16b%d" % (2 * h), "m16a%d" % (2 * h + 1), "m16b%d" % (2 * h + 1)], ["cand%d" % h])
        for h in range(8):
            S.op("dve", lambda e, h=h: e.max(out=c16[:, h, 0:8], in_=candA[:, h, :]), ["cand%d" % h], ["c16a%d" % h])
        for h in range(8):
            S.op("dve", lambda e, h=h: e.match_replace(out=ctmpA[:, h, :], in_to_replace=c16[:, h, 0:8],
                                                       in_values=candA[:, h, :], imm_value=NEG),
                 ["cand%d" % h, "c16a%d" % h], ["ctmp%d" % h])
        for h in range(8):
            S.op("dve", lambda e, h=h: e.max(out=c16[:, h, 8:16], in_=ctmpA[:, h, :]), ["ctmp%d" % h], ["c16b%d" % h])
        C16R = ["c16a%d" % h for h in range(8)] + ["c16b%d" % h for h in range(8)]
        M16R = ["m16a%d" % cb for cb in range(16)]
        TT(e16, c16, bc(c16[:, :, 0], [128, 8, 16], 2), ALU.subtract, C16R, ["e16"])
        ACT(e16, e16, AF.Exp, ["e16"], ["e16"])
        S.op("dve", lambda e: e.reduce_sum(out=zz[:, 0, :], in_=e16, axis=AX.X), ["e16"], ["zz"])
        S.op("dve", lambda e: e.reciprocal(out=zz[:, 1, :], in_=zz[:, 0, :]), ["zz"], ["zz"])
        STT(zz[:, 2, :], e16[:, :, 15], 1.0 - MARGIN, zz[:, 1, :], ALU.mult, ALU.mult, ["e16", "zz"], ["zz"])
        ACT(zz[:, 3, :], zz[:, 0, :], AF.Ln, ["zz"], ["zz"])
        TS(zz[:, 4, :], c16[:, :, 15], -MARGIN, None, ALU.add, None, C16R + ["zz"], ["zz"])
        TT(zz[:, 5, :], zz[:, 4, :], c16[:, :, 0], ALU.subtract, C16R + ["zz"], ["zz"])
        TT(zz[:, 5, :], zz[:, 5, :], zz[:, 3, :], ALU.subtract, ["zz"], ["zz"])
        if NA > 0:
            CP("dve", zz[:, 2, 0:NA], zz[:, 5, 0:NA], ["zz"], ["zz"])
        for h in range(NA):
            TS(ssb[:, 2 * h, :], ssb[:, 2 * h, :], zz[:, 4, h:h + 1], None, ALU.subtract, None, ["ssb", "zz"], ["ssb"])
        if NA < 8:
            lo = 2 * NA
            TT(ssb[:, lo:16, :], ssb[:, lo:16, :], bc(m16[:, lo:16, 0], [128, 16 - lo, 128], 2), ALU.subtract,
               ["ssb"] + M16R, ["ssb"])
            ACT(ssb[:, lo:16, :], ssb[:, lo:16, :], AF.Exp, ["ssb"], ["ssb"])
            ssb4 = ssb.rearrange("p (h two) n -> p h two n", two=2)
            TT(ssb4[:, NA:8, 0, :], ssb4[:, NA:8, 0, :], bc(zz[:, 1, NA:8], [128, 8 - NA, 128], 2), ALU.mult,
               ["ssb", "zz"], ["ssb"])
        DMA("sp", ABs[t], ssb.rearrange("p a b -> p (a b)"), "ABs", ["ssb"], [])
        DMA("sp", ths[t], zz[:, 2, :], "ths", ["zz"], [])

    S.barrier()
    if STOP == 2:
        return emit()
    off[0] = base_off
    uS = [alloc(8 * 1024, BF16, [8, 1024]) for _ in range(2)]
    vS = [alloc(8 * 1024, BF16, [8, 1024]) for _ in range(2)]
    ABb = alloc(TB * 2048, F32, [TB, 2048])
    thb = alloc(TB * 8, F32, [TB, 8])
    hnTb = alloc(TB * 1024, BF16, [TB, 8, 128])
    acc = alloc(TB * 1024, F32, [TB, 1024])
    Eb = alloc(1024, F32, [8, 128])
    Cb = [alloc(1024, F32, [8, 128]) for _ in range(3)]
    C2 = [alloc(1024, F32, [8, 128]) for _ in range(2)]
    Gh = [alloc(1024, BF16) for _ in range(16)]
    WT = [alloc(1024, BF16, [8, 128]) for _ in range(2)]
    hl = alloc(1024)
    sq = Eb.rearrange("p a b -> p (a b)")
    st1 = alloc(8)
    gfr = alloc(1024)
    DMA("sp", gfr, gfr_d[:, :], "c0", [], ["gfr"])
    uT_v = uT.rearrange("(k p) e -> p k e", p=128)
    v_v = v_d.rearrange("(g t p) d -> g p t d", t=8, p=128)
    geTa = [alloc(8 * TB * 128, BF16, [8, TB, 128]) for _ in range(2)]
    step = 0
    def emit_WT(par, tt, sl):
        gT = geTa[sl]
        pgb = [2, 3] if par == 0 else [4, 5]
        for nb in range(2):
            TT(WT[par][:, 4 * nb:4 * nb + 4, :], bank(pgb[nb], F32, [4, 128]), gT[:, 4 * nb:4 * nb + 4, tt, :], ALU.mult,
               ["ps%d" % pgb[nb], "geTa%d" % sl, "WT%d" % par], ["WT%d" % par])

    def emit_out(par, tt, sl):
        for hf_ in range(2):
            for et in range(8):
                MM(bank(6 + hf_), WT[par][:, et, :], vS[sl][:, et, hf_ * 512:(hf_ + 1) * 512], et == 0, et == 7,
                   ["WT%d" % par, "vS%d" % sl], ["ps%d" % (6 + hf_)], et == 7)

    def emit_B2(tt):
        TT(acc[:, tt, 0:512], acc[:, tt, 0:512], bank(6), ALU.add, ["acc", "ps6"], ["acc"])
        TT(acc[:, tt, 512:1024], acc[:, tt, 512:1024], bank(7), ALU.add, ["acc", "ps7"], ["acc"])

    ccnt = [0]
    q1 = []
    q2 = []

    def drain(keep1, keep2):
        while len(q1) > keep1:
            a = q1.pop(0)
            emit_WT(*a)
            while q2:
                emit_B2(q2.pop(0))
            emit_out(*a)
            q2.append(a[1])
        while len(q2) > keep2:
            emit_B2(q2.pop(0))

    for blk in range(NB):
        t0 = blk * TB
        DMA("sp", ABb, ABs[t0:t0 + TB].rearrange("t p c -> p t c"), "ldAB", [], ["ABb"])
        DMA("sp", thb, ths[t0:t0 + TB].rearrange("t p c -> p t c"), "ldth", [], ["thb"])
        DMA("sp", hnTb.rearrange("p t a b -> p t (a b)"), hnTs[t0:t0 + TB].rearrange("t p c -> p t c"), "ldhn", [], ["hnTb"])
        S.op("dve", lambda e: e.memset(acc, 0.0), [], ["acc"])
        def load_eg(eg):
            sl = eg % 2
            DMA("pool", uS[sl], uT_v[:, :, eg * 1024:(eg + 1) * 1024], "u%d" % sl, [], ["uS%d" % sl])
            DMA("pool", vS[sl], v_v[eg], "v%d" % sl, [], ["vS%d" % sl])

        def actT_eg(eg):
            sl = eg % 2
            for et in range(8):
                bk = et % 2
                for k in range(8):
                    MM(bank(bk)[:, 0:TB * 128].rearrange("p (a b) -> p a b", a=TB), uS[sl][:, k, et * 128:(et + 1) * 128],
                       hnTb[:, :, k, :], k == 0, k == 7, ["hnTb", "uS%d" % sl], ["ps%d" % bk], k == 7)
                ACT(geTa[sl][:, et, :, :], bank(bk)[:, 0:TB * 128].rearrange("p (a b) -> p a b", a=TB), AF.Gelu,
                    ["ps%d" % bk, "geTa%d" % sl], ["geTa%d" % sl])

        drain(0, 0)
        load_eg(0)
        actT_eg(0)
        for eg in range(16):
            sl = eg % 2
            for tt in range(TB):
                par = step % 2
                step += 1
                pgb = [2, 3] if par == 0 else [4, 5]
                a_heads = list(range(NA))
                d_heads = list(range(NA, 8))
                order = []
                while a_heads or d_heads:
                    for _ in range(3):
                        if a_heads:
                            order.append(a_heads.pop(0))
                    if d_heads:
                        order.append(d_heads.pop(0))
                for h in order:
                    a_ = ABb[:, tt, (2 * h) * 128 + 8 * eg:(2 * h) * 128 + 8 * eg + 8]
                    b_ = ABb[:, tt, (2 * h + 1) * 128:(2 * h + 2) * 128]
                    gs = 8 * par + h
                    g_ = Gh[gs]
                    gr = "Gh%d" % gs
                    if h < NA:
                        cs = ccnt[0] % 3
                        c2 = ccnt[0] % 2
                        ccnt[0] += 1
                        TT(Cb[cs], bc(a_, [128, 8, 128], 2), bc(b_, [128, 8, 128], 1), ALU.add, ["ABb"], ["Cb%d" % cs])
                        S.op("act", lambda e, cs=cs, c2=c2: e.activation(out=C2[c2], in_=Cb[cs], func=AF.Prelu, alpha=1.0e6),
                             ["Cb%d" % cs], ["C2%d" % c2])
                        ACT(g_, C2[c2].rearrange("p a b -> p (a b)"), AF.Exp, ["C2%d" % c2, "thb"], [gr],
                            bias=thb[:, tt, h:h + 1])
                    else:
                        TT(Eb, bc(a_, [128, 8, 128], 2), bc(b_, [128, 8, 128], 1), ALU.mult, ["ABb"], ["sq"])
                        E2 = Eb.rearrange("p a b -> p (a b)")
                        STT(g_, E2, thb[:, tt, h:h + 1], E2, ALU.is_ge, ALU.mult, ["sq", "thb"], [gr])
                for et in range(8):
                    pg = bank(pgb[et // 4], F32, [4, 128])[:, et % 4, :]
                    for h in range(8):
                        MM(pg, Gh[8 * par + h][:, et * 128:(et + 1) * 128], identb, h == 0, h == 7,
                           ["Gh%d" % (8 * par + h), "identb"], ["ps%d" % pgb[et // 4]], (h == 7) and (et % 4 == 3))
                q1.append((par, tt, sl))
                drain(1, 1)
                if tt == 0 and eg + 1 < 16:
                    load_eg(eg + 1)
                if tt == TB - 1 and eg + 1 < 16:
                    actT_eg(eg + 1)
        drain(0, 0)
        for tt in range(TB):
            t = t0 + tt
            DMA("sp", hl, hs[t], "x0", [], ["hl"])
            TT(hl, hl, acc[:, tt, :], ALU.add, ["hl", "acc"], ["hl"])
            rstd_of(hl, "hl", 1024, "st1", sq, st1)
            STT(hl, hl, st1[:, 0:1], gfr, ALU.mult, ALU.mult, ["hl", "st1", "gfr"], ["hl"])
            DMA("sp", out_d[t * 128:(t + 1) * 128, :], hl, "out", ["hl"], [])
    S.barrier()

    return emit()


def host_inputs(x_main, x_pre, P):
    d = dict(P)
    d["xa"] = np.ascontiguousarray(np.concatenate([x_pre, x_main], axis=0))
    return d


def prep_params(norm1_g, w_in, hg_lower_logits, hg_norm_g, gla_w_gate_up, gla_b_gate, gla_norm_g, w_out, norm2_g,
                peer_w_q, peer_sub_keys, peer_u, peer_v, norm_f_g):
    f = np.float32
    cols = np.zeros((128, 16), f)
    cols[:, 0:4] = hg_lower_logits[0].reshape(4, 128).T
    cols[:, 4:8] = hg_lower_logits[1].reshape(4, 128).T
    cols[:, 8:10] = gla_b_gate[0].reshape(2, 128).T
    cols[:, 10] = hg_norm_g[0]
    cols[:, 11] = gla_norm_g[0]
    cst = np.zeros((128, 770), f)
    cst[0:64, 768] = 1.0
    cst[64:128, 769] = 1.0
    cst[:, 0:128] = np.eye(128, dtype=f)
    cst[:, 128:256] = np.triu(np.ones((128, 128), f))
    rp = np.ones((128, 512), f)
    rp[:, 0::128] = 0.0
    cst[:, 256:768] = rp
    rep = lambda g: np.ascontiguousarray(np.broadcast_to(g.reshape(1, -1), (128, g.size))).astype(f)
    kt = np.ascontiguousarray(peer_sub_keys[0].reshape(16, 128, 128).transpose(2, 0, 1).reshape(128, 2048))
    return {
        "w_in": np.ascontiguousarray(w_in[0]),
        "glowT": np.ascontiguousarray(w_in[0][:, 3072:3088].T),
        "w_up": np.ascontiguousarray(gla_w_gate_up[0]),
        "cols": cols,
        "g1r": rep(norm1_g[0]), "g2r": rep(norm2_g[0]), "gfr": rep(norm_f_g),
        "w_out": np.ascontiguousarray(w_out[0]),
        "w_q": np.ascontiguousarray(peer_w_q[0]),
        "kt": kt,
        "uT": np.ascontiguousarray(peer_u[0].T),
        "v": np.ascontiguousarray(peer_v[0]),
        "cst": cst,
    }


def kernel(x, norm1_g, w_in, hg_lower_logits, hg_norm_g, gla_w_gate_up, gla_b_gate, gla_norm_g, w_out, norm2_g,
           peer_w_q, peer_sub_keys, peer_u, peer_v, norm_f_g):
    args = [np.asarray(a, dtype=np.float32) for a in (norm1_g, w_in, hg_lower_logits, hg_norm_g, gla_w_gate_up,
            gla_b_gate, gla_norm_g, w_out, norm2_g, peer_w_q, peer_sub_keys, peer_u, peer_v, norm_f_g)]
    x = np.asarray(x, dtype=np.float32)
    P = prep_params(*args)
    B, T, D = x.shape
    half = T // 2
    in_maps = []
    for c in range(8):
        b, hf = c // 2, c % 2
        xm = x[b, hf * half:(hf + 1) * half]
        xp = x[b, 0:half] if hf == 1 else np.zeros((half, D), np.float32)
        in_maps.append(host_inputs(xm, xp, P))
    nc = build_program(32, 32, 4)
    res = run_bass_kernel_spmd(nc, in_maps, core_ids=list(range(8)))
    out = np.empty((B, T, D), np.float32)
    for c in range(8):
        b, hf = c // 2, c % 2
        out[b, hf * half:(hf + 1) * half] = res.results[c]["out"]
    return out
```

```python
import numpy as np
from contextlib import ExitStack
import concourse.bass as bass
import concourse.mybir as mybir
from concourse.bass_utils import run_bass_kernel_spmd

F32, BF16 = mybir.dt.float32, mybir.dt.bfloat16
AF = mybir.ActivationFunctionType
ALU = mybir.AluOpType
AX = mybir.AxisListType
EPS = 1e-6
STOP = 3
NA = 6
MARGIN = 2.0e-4
LIM = 99
EXP = 0
NEG = -1.0e30


class Sched:
    def __init__(self):
        self.q = {e: [] for e in ("pe", "act", "dve", "pool", "sp")}
        self.cnt = {e: 0 for e in self.q}
        self.dcnt = {}
        self.res = {}
        self.waited = {e: {} for e in self.q}

    def _deps(self, eng, reads, writes):
        deps = []
        for r in reads:
            st = self.res.get(r)
            if st and st[0]:
                deps.append(st[0])
        for w in writes:
            st = self.res.get(w)
            if st:
                if st[0]:
                    deps.append(st[0])
                deps.extend(st[1])
        if eng == "pe":
            deps = [d for d in deps if d[0] != "c_pe"]
        return deps

    def _commit(self, reads, writes, ticket):
        for r in reads:
            self.res.setdefault(r, [None, []])[1].append(ticket)
        for w in writes:
            self.res[w] = [ticket, []]

    def _waits(self, eng, deps):
        wl = self.waited[eng]
        need = {}
        for s, v in deps:
            if wl.get(s, 0) < v:
                need[s] = max(need.get(s, 0), v)
        for s, v in need.items():
            wl[s] = v
            self.q[eng].append(("wait", s, v))

    def op(self, eng, fn, reads=(), writes=(), sig=True):
        self._waits(eng, self._deps(eng, reads, writes))
        if sig:
            self.cnt[eng] += 1
            ticket = ("c_" + eng, self.cnt[eng])
            self.q[eng].append(("op", fn, "c_" + eng))
        else:
            ticket = ("c_" + eng, self.cnt[eng] + 1)
            self.q[eng].append(("op", fn, None))
        self._commit(reads, writes, ticket)

    def dma(self, eng, fn, key, reads=(), writes=()):
        deps = self._deps(eng, reads, writes)
        prev = self.dcnt.get(key, 0)
        if prev:
            deps.append(("d_" + key, prev * 16))
        self._waits(eng, deps)
        self.dcnt[key] = prev + 1
        self.q[eng].append(("dma", fn, "d_" + key))
        self._commit(reads, writes, ("d_" + key, (prev + 1) * 16))

    def barrier(self):
        allsem = [("c_" + e, c) for e, c in self.cnt.items() if c] + \
                 [("d_" + k, c * 16) for k, c in self.dcnt.items()]
        for e in self.q:
            self._waits(e, allsem)
        self.res = {}

    def sem_names(self):
        return ["c_" + e for e in self.cnt] + ["d_" + k for k in self.dcnt]


def build_program(NPRE, NMAIN, TB, debug=False):
    nc = bass.Bass("TRN2", target_bir_lowering=False)
    NT = NPRE + NMAIN
    NB = NMAIN // TB
    D = 1024

    def din(name, shape, dt=F32):
        return nc.dram_tensor(name, list(shape), dt, kind="ExternalInput").ap()

    xa = din("xa", [NT * 128, D])
    w_in = din("w_in", [D, 3600])
    glowT = din("glowT", [16, D])
    w_up = din("w_up", [16, 256])
    cols = din("cols", [128, 16])
    g1r_d = din("g1r", [128, D])
    g2r_d = din("g2r", [128, D])
    gfr_d = din("gfr", [128, D])
    w_out = din("w_out", [D, D])
    w_q = din("w_q", [D, 2048])
    kt_d = din("kt", [128, 2048])
    uT = din("uT", [D, 16384])
    v_d = din("v", [16384, D])
    cst_d = din("cst", [128, 770])
    out_d = nc.dram_tensor("out", [NMAIN * 128, D], F32, kind="ExternalOutput").ap()
    hs = nc.dram_tensor("hs", [NMAIN, 128, D], F32).ap()
    hnTs = nc.dram_tensor("hnTs", [NMAIN, 128, D], BF16).ap()
    ABs = nc.dram_tensor("ABs", [NMAIN, 128, 2048], F32).ap()
    ths = nc.dram_tensor("ths", [NMAIN, 128, 8], F32).ap()
    dbg = {}
    if debug:
        dbg["h"] = nc.dram_tensor("dbg_h", [NMAIN * 128, D], F32, kind="ExternalOutput").ap()

    S = Sched()
    stack = ExitStack()
    NW = 53200
    POOL = stack.enter_context(nc.sbuf_tensor("pool", [128, NW], F32))
    PS = stack.enter_context(nc.psum_tensor("ps", [128, 8, 512], F32))
    off = [0]

    def alloc(n, dt=F32, shape=None):
        nw = n if dt == F32 else (n + 1) // 2
        a = POOL[:, off[0]:off[0] + nw]
        off[0] += nw
        assert off[0] <= NW, ("sbuf overflow", off[0])
        if dt == BF16:
            a = a.bitcast(BF16)
        if shape is not None:
            names = " ".join("abc"[: len(shape)])
            kw = {"abc"[i]: shape[i] for i in range(len(shape) - 1)}
            a = a.rearrange("p (%s) -> p %s" % (names, names), **kw)
        return a

    def bank(k, dt=F32, shape=None):
        a = PS[:, k, :]
        if dt == BF16:
            a = a.bitcast(BF16)
        if shape is not None:
            names = " ".join("abc"[: len(shape)])
            kw = {"abc"[i]: shape[i] for i in range(len(shape) - 1)}
            a = a.rearrange("p (%s) -> p %s" % (names, names), **kw)
        return a

    def MM(out, lhsT, rhs, start, stop, reads, writes, sig):
        S.op("pe", lambda e: e.matmul(out, lhsT=lhsT, rhs=rhs, start=start, stop=stop), reads, writes, sig)

    def ACT(out, in_, func, reads, writes, bias=None, scale=None):
        kw = {}
        if bias is not None:
            kw["bias"] = bias
        if scale is not None:
            kw["scale"] = scale
        S.op("act", lambda e: e.activation(out=out, in_=in_, func=func, **kw), reads, writes)

    def TT(out, in0, in1, op, reads, writes, eng="dve"):
        S.op(eng, lambda e: e.tensor_tensor(out=out, in0=in0, in1=in1, op=op), reads, writes)

    def TS(out, in0, s1, s2, op0, op1, reads, writes):
        if op1 is None:
            S.op("dve", lambda e: e.tensor_scalar(out=out, in0=in0, scalar1=s1, scalar2=None, op0=op0), reads, writes)
        else:
            S.op("dve", lambda e: e.tensor_scalar(out=out, in0=in0, scalar1=s1, scalar2=s2, op0=op0, op1=op1), reads, writes)

    def STT(out, in0, scalar, in1, op0, op1, reads, writes):
        S.op("dve", lambda e: e.scalar_tensor_tensor(out=out, in0=in0, scalar=scalar, in1=in1, op0=op0, op1=op1), reads, writes)

    def CP(eng, out, in_, reads, writes):
        if eng == "act":
            S.op("act", lambda e: e.copy(out=out, in_=in_), reads, writes)
        else:
            S.op(eng, lambda e: e.tensor_copy(out, in_), reads, writes)

    def DMA(eng, out, in_, key, reads, writes):
        S.dma(eng, lambda e: e.dma_start(out=out, in_=in_), key, reads, writes)

    def bc(ap, shape, axis):
        return ap.unsqueeze(axis).to_broadcast(shape)

    def emit():
        sems = {n: stack.enter_context(nc.semaphore(n)) for n in S.sem_names()}
        with stack:
            with nc.Block() as block:
                def replay(name, e):
                    for it in S.q[name]:
                        if it[0] == "wait":
                            e.wait_ge(sems[it[1]], it[2])
                        else:
                            ins = it[1](e)
                            if it[2] is not None:
                                ins.then_inc(sems[it[2]], 16 if it[0] == "dma" else 1)

                @block.tensor
                def _(e):
                    replay("pe", e)

                @block.scalar
                def _(e):
                    replay("act", e)

                @block.vector
                def _(e):
                    replay("dve", e)

                @block.gpsimd
                def _(e):
                    replay("pool", e)

                @block.sync
                def _(e):
                    replay("sp", e)
        return nc

    cst = alloc(770)
    identb = alloc(128, BF16)
    colst = alloc(16)
    small = alloc(16)
    DMA("sp", cst, cst_d[:, :], "c0", [], ["cst"])
    DMA("pool", identb, cst_d[:, 0:128], "c1", [], ["identb"])
    DMA("sp", colst, cols[:, :], "c2", [], ["colst"])
    maskT = cst[:, 128:256]
    rpat = cst[:, 256:768]
    rm = cst[:, 768:770]
    S.op("dve", lambda e: e.memset(small[:, 6:7], 1.0), [], ["small"])
    S.op("dve", lambda e: e.memset(small[:, 7:8], EPS), ["small"], ["small"])
    onec = small[:, 6:7]
    TT(small[:, 8:12], colst[:, 4:8], colst[:, 0:4], ALU.subtract, ["colst", "small"], ["small"])
    ACT(small[:, 0:4], small[:, 8:12], AF.Sigmoid, ["small"], ["small"])
    TS(small[:, 4:6], colst[:, 8:10], -1.0, None, ALU.mult, None, ["colst", "small"], ["small"])
    omlc = small[:, 0:4]
    nbc = small[:, 4:6]
    gnc = colst[:, 10:12]
    base_off = off[0]

    def rstd_of(src, rs, n, tag, sq, st):
        TT(sq[:, 0:n], src, src, ALU.mult, [rs], ["sq"])
        S.op("dve", lambda e: e.reduce_sum(out=st[:, 1:2], in_=sq[:, 0:n], axis=AX.X), ["sq"], [tag])
        TS(st[:, 2:3], st[:, 1:2], 1.0 / n, small[:, 7:8] if False else EPS, ALU.mult, ALU.add, [tag], [tag])
        ACT(st[:, 3:4], st[:, 2:3], AF.Sqrt, [tag], [tag])
        S.op("dve", lambda e: e.reciprocal(out=st[:, 0:1], in_=st[:, 3:4]), [tag], [tag])

    win = alloc(8 * 3600, BF16, [8, 3600])
    wz = alloc(8 * 256, BF16, [8, 256])
    wout = alloc(8 * 1024, BF16, [8, 1024])
    g1r = alloc(1024)
    S32 = alloc(1024, F32, [8, 128])
    Sbf = alloc(1024, BF16, [8, 128])
    glT = alloc(1024)
    wupt = alloc(256)
    xt = [alloc(1024), alloc(1024)]
    sq = alloc(1024)
    st1 = alloc(8)
    st8 = alloc(40, F32, [5, 8])
    xn = alloc(1024, BF16)
    tT = alloc(1024, BF16, [8, 128])
    V = alloc(1024, BF16)
    gsil = alloc(1024)
    nsig = alloc(512, F32, [4, 128])
    kk = alloc(512, F32, [4, 128])
    lf = alloc(768, F32, [6, 128])
    cum = alloc(768, F32, [6, 128])
    ecum = alloc(768, F32, [6, 128])
    encum = alloc(768, F32, [6, 128])
    ez = alloc(256, F32, [2, 128])
    qtilT = alloc(768, BF16, [6, 128])
    ktilT = alloc(768, BF16, [6, 128])
    ktok = alloc(768, BF16, [6, 128])
    eclb = alloc(8)
    sT = alloc(1024, BF16, [8, 128])
    mixed = alloc(1024, BF16)
    qg = alloc(512, BF16, [4, 128])

    w_in_v = w_in.rearrange("(k p) c -> p k c", p=128)
    for k in range(8):
        DMA("pool", win[:, k, :], w_in_v[:, k, :], "w%d" % (k % 4), [], ["win"])
    DMA("pool", wout, w_out.rearrange("(k p) c -> p k c", p=128), "w0", [], ["wout"])
    DMA("sp", g1r, g1r_d[:, :], "c0", [], ["g1r"])
    DMA("sp", glT[0:16, :], glowT[:, :], "c2", [], ["glT"])
    DMA("sp", wupt[0:16, :], w_up[:, :], "c2", [], ["wupt"])
    for k in range(8):
        TS(wout[:, k, :], wout[:, k, :], gnc[:, (k // 4):(k // 4) + 1], None, ALU.mult, None, ["wout", "colst"], ["wout"])
    for k in range(8):
        b = bank(k // 2, F32, [2, 256])
        MM(b[:, k % 2, :], glT[0:16, k * 128:(k + 1) * 128], wupt[0:16, :], True, True,
           ["glT", "wupt"], ["ps%d" % (k // 2)], True)
    for kb in range(4):
        CP("dve", wz[:, 2 * kb:2 * kb + 2, :], bank(kb, F32, [2, 256]), ["ps%d" % kb], ["wz"])
    S.op("dve", lambda e: e.memset(S32, 0.0), [], ["S32"])
    S.op("dve", lambda e: e.memset(Sbf, 0.0), [], ["Sbf"])

    C_HQ, C_HF, C_HI, C_HG, C_GQ, C_GK, C_GV, C_GG = 0, 512, 1024, 1536, 2048, 2304, 2560, 3088

    def fm_block(dst, wsrc, col, wres, pres, last):
        for k in range(8):
            MM(dst, wsrc[:, k, col:col + 128], tT[:, k, :], k == 0, k == 7, [wres, "tT"], [pres], (k == 7) and last)

    def tm_block(bk, col, pres):
        for k in range(8):
            MM(bank(bk), tT[:, k, :], win[:, k, col:col + 512], k == 0, k == 7, ["win", "tT"], [pres], k == 7)

    if STOP == 0:
        S.barrier()
        return emit()
    for c in range(NT):
        main = c >= NPRE
        x_ = xt[c % 2]
        xr = "xt%d" % (c % 2)
        DMA("sp", x_, xa[c * 128:(c + 1) * 128, :], "x%d" % (c % 2), [], [xr])
        rstd_of(x_, xr, 1024, "st1", sq, st1)
        STT(xn, x_, st1[:, 0:1], g1r, ALU.mult, ALU.mult, [xr, "st1", "g1r"], ["xn"])
        for k in range(8):
            S.op("pe", lambda e, k=k: e.transpose(bank(0, BF16, [8, 128])[:, k, :], xn[:, k * 128:(k + 1) * 128], identb),
                 ["xn", "identb"], ["ps0"], k == 7)
        CP("act", tT, bank(0, BF16, [8, 128]), ["ps0"], ["tT"])
        if LIM == 1:
            continue
        tm_block(1, C_HI, "ps1")
        tm_block(2, C_GV, "ps2")
        CP("act", V[:, 0:512], bank(1), ["ps1"], ["V"])
        CP("act", V[:, 512:1024], bank(2), ["ps2"], ["V"])
        if main:
            tm_block(3, C_HG, "ps3")
            tm_block(4, C_GG, "ps4")
            ACT(gsil[:, 0:512], bank(3), AF.Silu, ["ps3"], ["gsil"])
            ACT(gsil[:, 512:1024], bank(4), AF.Silu, ["ps4"], ["gsil"])
        if LIM == 2:
            continue
        b5 = bank(5, F32, [4, 128])
        b6 = bank(6, F32, [4, 128])
        b7 = bank(7, F32, [4, 128])
        b1 = bank(1, F32, [4, 128])
        for b in range(4):
            fm_block(b5[:, b, :], win, C_HF + b * 128, "win", "ps5", b == 3)
        for b in range(2):
            fm_block(b6[:, b, :], win, C_GK + b * 128, "win", "ps6", (b == 1) and not main)
        for b in range(2):
            fm_block(b7[:, b, :], wz, b * 128, "wz", "ps7", b == 1)
        if main:
            for b in range(2):
                fm_block(b6[:, 2 + b, :], win, C_GQ + b * 128, "win", "ps6", b == 1)
            for b in range(4):
                fm_block(b1[:, b, :], win, C_HQ + b * 128, "win", "ps1", b == 3)
        if LIM == 3:
            continue
        ACT(nsig, b5, AF.Sigmoid, ["ps5"], ["nsig"], scale=-1.0)
        TT(kk, nsig, bc(omlc, [128, 4, 128], 2), ALU.mult, ["nsig", "small"], ["kk"])
        ACT(lf[:, 0:4, :], kk, AF.Ln, ["kk", "small"], ["lf"], bias=onec, scale=-1.0)
        for b in range(2):
            ACT(ez[:, b, :], b7[:, b, :], AF.Exp, ["ps7", "small"], ["ez"], bias=nbc[:, b:b + 1], scale=-1.0)
        ACT(lf[:, 4:6, :], ez, AF.Ln, ["ez", "small"], ["lf"], bias=onec, scale=1.0)
        lf2 = lf.rearrange("p a b -> p (a b)")
        cum2 = cum.rearrange("p a b -> p (a b)")
        S.op("dve", lambda e: e.tensor_tensor_scan(out=cum2[:, 0:512], data0=rpat, data1=lf2[:, 0:512], initial=0.0,
                                                   op0=ALU.mult, op1=ALU.add), ["lf", "cst"], ["cum"])
        S.op("dve", lambda e: e.tensor_tensor_scan(out=cum2[:, 512:768], data0=rpat[:, 0:256], data1=lf2[:, 512:768],
                                                   initial=0.0, op0=ALU.mult, op1=ALU.add), ["lf", "cst", "cum"], ["cum"])
        ACT(ecum[:, 0:4, :], cum[:, 0:4, :], AF.Exp, ["cum"], ["ecum"])
        ACT(encum[:, 0:4, :], cum[:, 0:4, :], AF.Exp, ["cum"], ["encum"], scale=-1.0)
        ACT(ecum[:, 4:6, :], cum[:, 4:6, :], AF.Exp, ["cum", "ecum"], ["ecum"], scale=-1.0 / 16.0)
        ACT(encum[:, 4:6, :], cum[:, 4:6, :], AF.Exp, ["cum", "encum"], ["encum"], scale=1.0 / 16.0)
        TT(ktilT[:, 0:4, :], kk, encum[:, 0:4, :], ALU.mult, ["kk", "encum"], ["ktilT"])
        TT(ktilT[:, 4:6, :], b6[:, 0:2, :], encum[:, 4:6, :], ALU.mult, ["ps6", "encum", "ktilT"], ["ktilT"])
        CP("dve", eclb[:, 0:4], ecum[:, 0:4, 127], ["ecum"], ["eclb"])
        CP("dve", eclb[:, 4:8].rearrange("p (a b) -> p a b", a=2), bc(ecum[:, 4:6, 127], [128, 2, 2], 2), ["ecum", "eclb"], ["eclb"])
        if main:
            ACT(nsig, b1, AF.Silu, ["ps1", "kk"], ["nsig"])
            TT(qtilT[:, 0:4, :], nsig, ecum[:, 0:4, :], ALU.mult, ["nsig", "ecum"], ["qtilT"])
            STT(qtilT[:, 4:6, :], b6[:, 2:4, :], 0.125, ecum[:, 4:6, :], ALU.mult, ALU.mult, ["ps6", "ecum", "qtilT"], ["qtilT"])
        if LIM == 4:
            continue
        b2b = bank(2, BF16, [8, 128])
        for b in range(6):
            S.op("pe", lambda e, b=b: e.transpose(b2b[:, b, :], ktilT[:, b, :], identb), ["ktilT", "identb"], ["ps2"], b == 5)
        CP("act", ktok, b2b[:, 0:6, :], ["ps2"], ["ktok"])
        if LIM == 5:
            continue
        if main:
            b3 = bank(3, F32, [4, 128])
            b4 = bank(4, F32, [4, 128])
            for b in range(4):
                MM(b3[:, b, :], ktilT[:, b, :], qtilT[:, b, :], True, True, ["ktilT", "qtilT"], ["ps3"], b == 3)
            for g in range(4):
                TS(qg[:, g, :], qtilT[:, 4 + g // 2, :], rm[:, (g % 2):(g % 2) + 1], None, ALU.mult, None,
                   ["qtilT", "cst", "qg"], ["qg"])
            for g in range(4):
                MM(b4[:, g, :], ktilT[:, 4 + g // 2, :], qg[:, g, :], True, True, ["ktilT", "qg"], ["ps4"], g == 3)
            TT(sT[:, 0:4, :], b3, bc(maskT, [128, 4, 128], 1), ALU.mult, ["ps3", "cst"], ["sT"])
            TT(sT[:, 4:8, :], b4, bc(maskT, [128, 4, 128], 1), ALU.mult, ["ps4", "cst", "sT"], ["sT"])
            if LIM == 55:
                continue
            for hh in range(8):
                ob = (b5 if hh < 4 else b6)[:, hh % 4, :]
                pres = "ps5" if hh < 4 else "ps6"
                MM(ob, sT[:, hh, :], V[:, hh * 128:(hh + 1) * 128], True, False, ["sT", "V"], [pres], False)
                if hh < 4:
                    MM(ob, qtilT[:, hh, :], Sbf[:, hh, :], False, True, ["qtilT", "Sbf"], [pres], hh == 3)
                else:
                    g = hh - 4
                    MM(ob, qg[:, g, :], Sbf[:, hh, :], False, True, ["qg", "Sbf"], [pres], hh == 7)
        if LIM == 6:
            continue
        b0 = bank(0, F32, [4, 128])
        for hh in range(8):
            ub = (b7 if hh < 4 else b0)[:, hh % 4, :]
            pres = "ps7" if hh < 4 else "ps0"
            kb = hh if hh < 4 else 4 + (hh - 4) // 2
            MM(ub, ktok[:, kb, :], V[:, hh * 128:(hh + 1) * 128], True, True, ["ktok", "V"], [pres], hh in (3, 7))
        Us = sq.rearrange("p (a b) -> p a b", a=8)
        TT(Us[:, 0:4, :], b7, bc(eclb[:, 0:4], [128, 4, 128], 2), ALU.mult, ["ps7", "eclb"], ["sq"])
        TT(Us[:, 4:8, :], b0, bc(eclb[:, 4:8], [128, 4, 128], 2), ALU.mult, ["ps0", "eclb", "sq"], ["sq"])
        TT(S32, S32, bc(eclb[:, 0:8], [128, 8, 128], 2), ALU.mult, ["S32", "eclb"], ["S32"])
        TT(S32, S32, Us, ALU.add, ["S32", "sq"], ["S32"])
        CP("act", Sbf, S32, ["S32"], ["Sbf"])
        if LIM == 7:
            continue
        if main:
            sq3 = sq.rearrange("p (a b) -> p a b", a=8)
            ACT(sq3[:, 0:4, :], b5, AF.Square, ["ps5"], ["sq"])
            ACT(sq3[:, 4:8, :], b6, AF.Square, ["ps6", "sq"], ["sq"])
            S.op("dve", lambda e: e.reduce_sum(out=st8[:, 0, :], in_=sq3, axis=AX.X), ["sq"], ["st8"])
            TS(st8[:, 1, :], st8[:, 0, :], 1.0 / 128.0, EPS, ALU.mult, ALU.add, ["st8"], ["st8"])
            ACT(st8[:, 2, :], st8[:, 1, :], AF.Sqrt, ["st8"], ["st8"])
            S.op("dve", lambda e: e.reciprocal(out=st8[:, 3, :], in_=st8[:, 2, :]), ["st8"], ["st8"])
            TT(sq3[:, 0:4, :], b5, bc(st8[:, 3, 0:4], [128, 4, 128], 2), ALU.mult, ["ps5", "st8", "sq"], ["sq"])
            TT(sq3[:, 4:8, :], b6, bc(st8[:, 3, 4:8], [128, 4, 128], 2), ALU.mult, ["ps6", "st8", "sq"], ["sq"])
            TT(mixed, sq, gsil, ALU.mult, ["sq", "gsil"], ["mixed"])
            b1b = bank(1, BF16, [8, 128])
            for k in range(8):
                S.op("pe", lambda e, k=k: e.transpose(b1b[:, k, :], mixed[:, k * 128:(k + 1) * 128], identb),
                     ["mixed", "identb"], ["ps1"], k == 7)
            CP("act", tT, b1b, ["ps1"], ["tT"])
            for hf_ in range(2):
                for k in range(8):
                    MM(bank(2 + hf_), tT[:, k, :], wout[:, k, hf_ * 512:(hf_ + 1) * 512], k == 0, k == 7,
                       ["tT", "wout"], ["ps%d" % (2 + hf_)], k == 7)
            TT(x_[:, 0:512], x_[:, 0:512], bank(2), ALU.add, [xr, "ps2"], [xr])
            TT(x_[:, 512:1024], x_[:, 512:1024], bank(3), ALU.add, [xr, "ps3"], [xr])
            t = c - NPRE
            DMA("sp", hs[t], x_, "hs", [xr], [])
            if debug:
                DMA("sp", dbg["h"][t * 128:(t + 1) * 128, :], x_, "dbgh", [xr], [])

    S.barrier()
    if STOP == 1:
        return emit()
    off[0] = base_off
    wq = alloc(8 * 2048, BF16, [8, 2048])
    kt = alloc(2048, BF16, [16, 128])
    g2r = alloc(1024)
    hl = [alloc(1024), alloc(1024)]
    sq = alloc(1024)
    st1 = alloc(8)
    hn = alloc(1024, BF16)
    hnT = alloc(1024, BF16, [8, 128])
    qT = alloc(2048, BF16, [16, 128])
    ssb = alloc(2048, F32, [16, 128])
    tmpA = alloc(2048, F32, [16, 128])
    m16 = alloc(256, F32, [16, 16])
    candA = alloc(2048, F32, [8, 256])
    ctmpA = alloc(2048, F32, [8, 256])
    c16 = alloc(128, F32, [8, 16])
    e16 = alloc(128, F32, [8, 16])
    zz = alloc(48, F32, [6, 8])
    w_q_v = w_q.rearrange("(k p) c -> p k c", p=128)
    for k in range(8):
        DMA("pool", wq[:, k, :], w_q_v[:, k, :], "w%d" % (k % 4), [], ["wq"])
    DMA("pool", kt.rearrange("p a b -> p (a b)"), kt_d[:, :], "w0", [], ["kt"])
    DMA("sp", g2r, g2r_d[:, :], "c0", [], ["g2r"])
    for t in range(NMAIN):
        h_ = hl[t % 2]
        hr = "hl%d" % (t % 2)
        DMA("sp", h_, hs[t], "x%d" % (t % 2), [], [hr])
        rstd_of(h_, hr, 1024, "st1", sq, st1)
        STT(hn, h_, st1[:, 0:1], g2r, ALU.mult, ALU.mult, [hr, "st1", "g2r"], ["hn"])
        b0b = bank(0, BF16, [8, 128])
        for k in range(8):
            S.op("pe", lambda e, k=k: e.transpose(b0b[:, k, :], hn[:, k * 128:(k + 1) * 128], identb),
                 ["hn", "identb"], ["ps0"], k == 7)
        CP("act", hnT, b0b, ["ps0"], ["hnT"])
        DMA("sp", hnTs[t], hnT.rearrange("p a b -> p (a b)"), "hnTs", ["hnT"], [])
        for cb in range(16):
            bk = 1 + cb // 4
            dst = bank(bk, F32, [4, 128])[:, cb % 4, :]
            for k in range(8):
                MM(dst, wq[:, k, cb * 128:(cb + 1) * 128], hnT[:, k, :], k == 0, k == 7, ["wq", "hnT"], ["ps%d" % bk],
                   (k == 7) and (cb % 4 == 3))
        for q4 in range(4):
            CP("act", qT[:, 4 * q4:4 * q4 + 4, :], bank(1 + q4, F32, [4, 128]), ["ps%d" % (1 + q4)], ["qT"])
        sbk = [5, 6, 7, 0]
        for cb in range(16):
            bk = sbk[cb // 4]
            MM(bank(bk, F32, [4, 128])[:, cb % 4, :], qT[:, cb, :], kt[:, cb, :], True, True, ["qT", "kt"], ["ps%d" % bk],
               cb % 4 == 3)
        for q4 in range(4):
            CP("act", ssb[:, 4 * q4:4 * q4 + 4, :], bank(sbk[q4], F32, [4, 128]), ["ps%d" % sbk[q4]], ["ssb"])
        for cb in range(16):
            S.op("dve", lambda e, cb=cb: e.max(out=m16[:, cb, 0:8], in_=ssb[:, cb, :]), ["ssb"], ["m16a%d" % cb])
        for cb in range(16):
            S.op("dve", lambda e, cb=cb: e.match_replace(out=tmpA[:, cb, :], in_to_replace=m16[:, cb, 0:8],
                                                         in_values=ssb[:, cb, :], imm_value=NEG),
                 ["ssb", "m16a%d" % cb], ["tmp%d" % cb])
        for cb in range(16):
            S.op("dve", lambda e, cb=cb: e.max(out=m16[:, cb, 8:16], in_=tmpA[:, cb, :]), ["tmp%d" % cb], ["m16b%d" % cb])
        for h in range(8):
            TT(candA[:, h, :].rearrange("p (a b) -> p a b", a=16), bc(m16[:, 2 * h, :], [128, 16, 16], 2),
               bc(m16[:, 2 * h + 1, :], [128, 16, 16], 1), ALU.add,
               ["m16a%d" % (2 * h), "m16b%d" % (2 * h), "m16a%d" % (2 * h + 1), "m16b%d" % (2 * h + 1)], ["cand%d" % h])
        for h in range(8):
            S.op("dve", lambda e, h=h: e.max(out=c16[:, h, 0:8], in_=candA[:, h, :]), ["cand%d" % h], ["c16a%d" % h])
        for h in range(8):
            S.op("dve", lambda e, h=h: e.match_replace(out=ctmpA[:, h, :], in_to_replace=c16[:, h, 0:8],
                                                       in_values=candA[:, h, :], imm_value=NEG),
                 ["cand%d" % h, "c16a%d" % h], ["ctmp%d" % h])
        for h in range(8):
            S.op("dve", lambda e, h=h: e.max(out=c16[:, h, 8:16], in_=ctmpA[:, h, :]), ["ctmp%d" % h], ["c16b%d" % h])
        C16R = ["c16a%d" % h for h in range(8)] + ["c16b%d" % h for h in range(8)]
        M16R = ["m16a%d" % cb for cb in range(16)]
        TT(e16, c16, bc(c16[:, :, 0], [128, 8, 16], 2), ALU.subtract, C16R, ["e16"])
        ACT(e16, e16, AF.Exp, ["e16"], ["e16"])
        S.op("dve", lambda e: e.reduce_sum(out=zz[:, 0, :], in_=e16, axis=AX.X), ["e16"], ["zz"])
        S.op("dve", lambda e: e.reciprocal(out=zz[:, 1, :], in_=zz[:, 0, :]), ["zz"], ["zz"])
        STT(zz[:, 2, :], e16[:, :, 15], 1.0 - MARGIN, zz[:, 1, :], ALU.mult, ALU.mult, ["e16", "zz"], ["zz"])
        ACT(zz[:, 3, :], zz[:, 0, :], AF.Ln, ["zz"], ["zz"])
        TS(zz[:, 4, :], c16[:, :, 15], -MARGIN, None, ALU.add, None, C16R + ["zz"], ["zz"])
        TT(zz[:, 5, :], zz[:, 4, :], c16[:, :, 0], ALU.subtract, C16R + ["zz"], ["zz"])
        TT(zz[:, 5, :], zz[:, 5, :], zz[:, 3, :], ALU.subtract, ["zz"], ["zz"])
        if NA > 0:
            CP("dve", zz[:, 2, 0:NA], zz[:, 5, 0:NA], ["zz"], ["zz"])
        for h in range(NA):
            TS(ssb[:, 2 * h, :], ssb[:, 2 * h, :], zz[:, 4, h:h + 1], None, ALU.subtract, None, ["ssb", "zz"], ["ssb"])
        if NA < 8:
            lo = 2 * NA
            TT(ssb[:, lo:16, :], ssb[:, lo:16, :], bc(m16[:, lo:16, 0], [128, 16 - lo, 128], 2), ALU.subtract,
               ["ssb"] + M16R, ["ssb"])
            ACT(ssb[:, lo:16, :], ssb[:, lo:16, :], AF.Exp, ["ssb"], ["ssb"])
            ssb4 = ssb.rearrange("p (h two) n -> p h two n", two=2)
            TT(ssb4[:, NA:8, 0, :], ssb4[:, NA:8, 0, :], bc(zz[:, 1, NA:8], [128, 8 - NA, 128], 2), ALU.mult,
               ["ssb", "zz"], ["ssb"])
        DMA("sp", ABs[t], ssb.rearrange("p a b -> p (a b)"), "ABs", ["ssb"], [])
        DMA("sp", ths[t], zz[:, 2, :], "ths", ["zz"], [])

    S.barrier()
    if STOP == 2:
        return emit()
    off[0] = base_off
    uS = [alloc(8 * 1024, BF16, [8, 1024]) for _ in range(2)]
    vS = [alloc(8 * 1024, BF16, [8, 1024]) for _ in range(2)]
    ABb = alloc(TB * 2048, F32, [TB, 2048])
    thb = alloc(TB * 8, F32, [TB, 8])
    hnTb = alloc(TB * 1024, BF16, [TB, 8, 128])
    acc = alloc(TB * 1024, F32, [TB, 1024])
    Eb = alloc(1024, F32, [8, 128])
    Cb = [alloc(1024, F32, [8, 128]) for _ in range(3)]
    C2 = [alloc(1024, F32, [8, 128]) for _ in range(2)]
    Gh = [alloc(1024, BF16) for _ in range(16)]
    WT = [alloc(1024, BF16, [8, 128]) for _ in range(2)]
    hl = alloc(1024)
    sq = Eb.rearrange("p a b -> p (a b)")
    st1 = alloc(8)
    gfr = alloc(1024)
    DMA("sp", gfr, gfr_d[:, :], "c0", [], ["gfr"])
    uT_v = uT.rearrange("(k p) e -> p k e", p=128)
    v_v = v_d.rearrange("(g t p) d -> g p t d", t=8, p=128)
    geTa = [alloc(8 * TB * 128, BF16, [8, TB, 128]) for _ in range(2)]
    step = 0
    def emit_WT(par, tt, sl):
        gT = geTa[sl]
        pgb = [2, 3] if par == 0 else [4, 5]
        for nb in range(2):
            TT(WT[par][:, 4 * nb:4 * nb + 4, :], bank(pgb[nb], F32, [4, 128]), gT[:, 4 * nb:4 * nb + 4, tt, :], ALU.mult,
               ["ps%d" % pgb[nb], "geTa%d" % sl, "WT%d" % par], ["WT%d" % par])

    def emit_out(par, tt, sl):
        for hf_ in range(2):
            for et in range(8):
                MM(bank(6 + hf_), WT[par][:, et, :], vS[sl][:, et, hf_ * 512:(hf_ + 1) * 512], et == 0, et == 7,
                   ["WT%d" % par, "vS%d" % sl], ["ps%d" % (6 + hf_)], et == 7)

    def emit_B2(tt):
        TT(acc[:, tt, 0:512], acc[:, tt, 0:512], bank(6), ALU.add, ["acc", "ps6"], ["acc"])
        TT(acc[:, tt, 512:1024], acc[:, tt, 512:1024], bank(7), ALU.add, ["acc", "ps7"], ["acc"])

    ccnt = [0]
    q1 = []
    q2 = []

    def drain(keep1, keep2):
        while len(q1) > keep1:
            a = q1.pop(0)
            emit_WT(*a)
            while q2:
                emit_B2(q2.pop(0))
            emit_out(*a)
            q2.append(a[1])
        while len(q2) > keep2:
            emit_B2(q2.pop(0))

    for blk in range(NB):
        t0 = blk * TB
        DMA("sp", ABb, ABs[t0:t0 + TB].rearrange("t p c -> p t c"), "ldAB", [], ["ABb"])
        DMA("sp", thb, ths[t0:t0 + TB].rearrange("t p c -> p t c"), "ldth", [], ["thb"])
        DMA("sp", hnTb.rearrange("p t a b -> p t (a b)"), hnTs[t0:t0 + TB].rearrange("t p c -> p t c"), "ldhn", [], ["hnTb"])
        S.op("dve", lambda e: e.memset(acc, 0.0), [], ["acc"])
        def load_u(eg):
            sl = eg % 2
            DMA("pool", uS[sl], uT_v[:, :, eg * 1024:(eg + 1) * 1024], "u%d" % sl, [], ["uS%d" % sl])

        def load_v(eg):
            sl = eg % 2
            DMA("pool", vS[sl], v_v[eg], "v%d" % sl, [], ["vS%d" % sl])

        def actT_mm(eg, ets):
            sl = eg % 2
            for et in ets:
                bk = et % 2
                for k in range(8):
                    MM(bank(bk)[:, 0:TB * 128].rearrange("p (a b) -> p a b", a=TB), uS[sl][:, k, et * 128:(et + 1) * 128],
                       hnTb[:, :, k, :], k == 0, k == 7, ["hnTb", "uS%d" % sl], ["ps%d" % bk], k == 7)

        def actT_gelu(eg, ets):
            sl = eg % 2
            for et in ets:
                bk = et % 2
                ACT(geTa[sl][:, et, :, :], bank(bk)[:, 0:TB * 128].rearrange("p (a b) -> p a b", a=TB), AF.Gelu,
                    ["ps%d" % bk, "geTa%d" % sl], ["geTa%d" % sl])

        drain(0, 0)
        load_u(0)
        load_v(0)
        for et in range(8):
            actT_mm(0, [et])
            actT_gelu(0, [et])
        load_u(1)
        GPT = 8 // TB if TB <= 8 else 1
        for eg in range(16):
            sl = eg % 2
            for tt in range(TB):
                par = step % 2
                step += 1
                pgb = [2, 3] if par == 0 else [4, 5]
                pre = list(range(GPT * tt, GPT * tt + GPT)) if eg + 1 < 16 else []
                actT_mm(eg + 1, pre[:2])
                a_heads = list(range(NA))
                d_heads = list(range(NA, 8))
                order = []
                while a_heads or d_heads:
                    for _ in range(3):
                        if a_heads:
                            order.append(a_heads.pop(0))
                    if d_heads:
                        order.append(d_heads.pop(0))
                for h in order:
                    a_ = ABb[:, tt, (2 * h) * 128 + 8 * eg:(2 * h) * 128 + 8 * eg + 8]
                    b_ = ABb[:, tt, (2 * h + 1) * 128:(2 * h + 2) * 128]
                    gs = 8 * par + h
                    g_ = Gh[gs]
                    gr = "Gh%d" % gs
                    if h < NA:
                        cs = ccnt[0] % 3
                        c2 = ccnt[0] % 2
                        ccnt[0] += 1
                        TT(Cb[cs], bc(a_, [128, 8, 128], 2), bc(b_, [128, 8, 128], 1), ALU.add, ["ABb"], ["Cb%d" % cs])
                        S.op("act", lambda e, cs=cs, c2=c2: e.activation(out=C2[c2], in_=Cb[cs], func=AF.Prelu, alpha=1.0e6),
                             ["Cb%d" % cs], ["C2%d" % c2])
                        ACT(g_, C2[c2].rearrange("p a b -> p (a b)"), AF.Exp, ["C2%d" % c2, "thb"], [gr],
                            bias=thb[:, tt, h:h + 1])
                    else:
                        TT(Eb, bc(a_, [128, 8, 128], 2), bc(b_, [128, 8, 128], 1), ALU.mult, ["ABb"], ["sq"])
                        E2 = Eb.rearrange("p a b -> p (a b)")
                        STT(g_, E2, thb[:, tt, h:h + 1], E2, ALU.is_ge, ALU.mult, ["sq", "thb"], [gr])
                for et in range(8):
                    pg = bank(pgb[et // 4], F32, [4, 128])[:, et % 4, :]
                    for h in range(8):
                        MM(pg, Gh[8 * par + h][:, et * 128:(et + 1) * 128], identb, h == 0, h == 7,
                           ["Gh%d" % (8 * par + h), "identb"], ["ps%d" % pgb[et // 4]], (h == 7) and (et % 4 == 3))
                q1.append((par, tt, sl))
                drain(1, 1)
                actT_gelu(eg + 1, pre[:2])
                for et in pre[2:]:
                    actT_mm(eg + 1, [et])
                    actT_gelu(eg + 1, [et])
                if tt == 0:
                    if eg + 1 < 16:
                        load_v(eg + 1)
                    if eg + 2 < 16:
                        load_u(eg + 2)
        drain(0, 0)
        for tt in range(TB):
            t = t0 + tt
            DMA("sp", hl, hs[t], "x0", [], ["hl"])
            TT(hl, hl, acc[:, tt, :], ALU.add, ["hl", "acc"], ["hl"])
            rstd_of(hl, "hl", 1024, "st1", sq, st1)
            STT(hl, hl, st1[:, 0:1], gfr, ALU.mult, ALU.mult, ["hl", "st1", "gfr"], ["hl"])
            DMA("sp", out_d[t * 128:(t + 1) * 128, :], hl, "out", ["hl"], [])
    S.barrier()

    return emit()


def host_inputs(x_main, x_pre, P):
    d = dict(P)
    d["xa"] = np.ascontiguousarray(np.concatenate([x_pre, x_main], axis=0))
    return d


def prep_params(norm1_g, w_in, hg_lower_logits, hg_norm_g, gla_w_gate_up, gla_b_gate, gla_norm_g, w_out, norm2_g,
                peer_w_q, peer_sub_keys, peer_u, peer_v, norm_f_g):
    f = np.float32
    cols = np.zeros((128, 16), f)
    cols[:, 0:4] = hg_lower_logits[0].reshape(4, 128).T
    cols[:, 4:8] = hg_lower_logits[1].reshape(4, 128).T
    cols[:, 8:10] = gla_b_gate[0].reshape(2, 128).T
    cols[:, 10] = hg_norm_g[0]
    cols[:, 11] = gla_norm_g[0]
    cst = np.zeros((128, 770), f)
    cst[0:64, 768] = 1.0
    cst[64:128, 769] = 1.0
    cst[:, 0:128] = np.eye(128, dtype=f)
    cst[:, 128:256] = np.triu(np.ones((128, 128), f))
    rp = np.ones((128, 512), f)
    rp[:, 0::128] = 0.0
    cst[:, 256:768] = rp
    rep = lambda g: np.ascontiguousarray(np.broadcast_to(g.reshape(1, -1), (128, g.size))).astype(f)
    kt = np.ascontiguousarray(peer_sub_keys[0].reshape(16, 128, 128).transpose(2, 0, 1).reshape(128, 2048))
    return {
        "w_in": np.ascontiguousarray(w_in[0]),
        "glowT": np.ascontiguousarray(w_in[0][:, 3072:3088].T),
        "w_up": np.ascontiguousarray(gla_w_gate_up[0]),
        "cols": cols,
        "g1r": rep(norm1_g[0]), "g2r": rep(norm2_g[0]), "gfr": rep(norm_f_g),
        "w_out": np.ascontiguousarray(w_out[0]),
        "w_q": np.ascontiguousarray(peer_w_q[0]),
        "kt": kt,
        "uT": np.ascontiguousarray(peer_u[0].T),
        "v": np.ascontiguousarray(peer_v[0]),
        "cst": cst,
    }


def kernel(x, norm1_g, w_in, hg_lower_logits, hg_norm_g, gla_w_gate_up, gla_b_gate, gla_norm_g, w_out, norm2_g,
           peer_w_q, peer_sub_keys, peer_u, peer_v, norm_f_g):
    args = [np.asarray(a, dtype=np.float32) for a in (norm1_g, w_in, hg_lower_logits, hg_norm_g, gla_w_gate_up,
            gla_b_gate, gla_norm_g, w_out, norm2_g, peer_w_q, peer_sub_keys, peer_u, peer_v, norm_f_g)]
    x = np.asarray(x, dtype=np.float32)
    P = prep_params(*args)
    B, T, D = x.shape
    half = T // 2
    in_maps = []
    for c in range(8):
        b, hf = c // 2, c % 2
        xm = x[b, hf * half:(hf + 1) * half]
        xp = x[b, 0:half] if hf == 1 else np.zeros((half, D), np.float32)
        in_maps.append(host_inputs(xm, xp, P))
    nc = build_program(32, 32, 4)
    res = run_bass_kernel_spmd(nc, in_maps, core_ids=list(range(8)))
    out = np.empty((B, T, D), np.float32)
    for c in range(8):
        b, hf = c // 2, c % 2
        out[b, hf * half:(hf + 1) * half] = res.results[c]["out"]
    return out
```

```python
import numpy as np
from contextlib import ExitStack
import concourse.bass as bass
import concourse.mybir as mybir
from concourse.bass_utils import run_bass_kernel_spmd

F32, BF16 = mybir.dt.float32, mybir.dt.bfloat16
AF = mybir.ActivationFunctionType
ALU = mybir.AluOpType
AX = mybir.AxisListType
EPS = 1e-6
STOP = 3
NA = 5
MARGIN = 2.0e-4
LIM = 99
EXP = 0
NEG = -1.0e30


class Sched:
    def __init__(self):
        self.q = {e: [] for e in ("pe", "act", "dve", "pool", "sp")}
        self.cnt = {e: 0 for e in self.q}
        self.dcnt = {}
        self.res = {}
        self.waited = {e: {} for e in self.q}

    def _deps(self, eng, reads, writes):
        deps = []
        for r in reads:
            st = self.res.get(r)
            if st and st[0]:
                deps.append(st[0])
        for w in writes:
            st = self.res.get(w)
            if st:
                if st[0]:
                    deps.append(st[0])
                deps.extend(st[1])
        if eng == "pe":
            deps = [d for d in deps if d[0] != "c_pe"]
        return deps

    def _commit(self, reads, writes, ticket):
        for r in reads:
            self.res.setdefault(r, [None, []])[1].append(ticket)
        for w in writes:
            self.res[w] = [ticket, []]

    def _waits(self, eng, deps):
        wl = self.waited[eng]
        need = {}
        for s, v in deps:
            if wl.get(s, 0) < v:
                need[s] = max(need.get(s, 0), v)
        for s, v in need.items():
            wl[s] = v
            self.q[eng].append(("wait", s, v))

    def op(self, eng, fn, reads=(), writes=(), sig=True):
        self._waits(eng, self._deps(eng, reads, writes))
        if sig:
            self.cnt[eng] += 1
            ticket = ("c_" + eng, self.cnt[eng])
            self.q[eng].append(("op", fn, "c_" + eng))
        else:
            ticket = ("c_" + eng, self.cnt[eng] + 1)
            self.q[eng].append(("op", fn, None))
        self._commit(reads, writes, ticket)

    def dma(self, eng, fn, key, reads=(), writes=()):
        deps = self._deps(eng, reads, writes)
        prev = self.dcnt.get(key, 0)
        if prev:
            deps.append(("d_" + key, prev * 16))
        self._waits(eng, deps)
        self.dcnt[key] = prev + 1
        self.q[eng].append(("dma", fn, "d_" + key))
        self._commit(reads, writes, ("d_" + key, (prev + 1) * 16))

    def barrier(self):
        allsem = [("c_" + e, c) for e, c in self.cnt.items() if c] + \
                 [("d_" + k, c * 16) for k, c in self.dcnt.items()]
        for e in self.q:
            self._waits(e, allsem)
        self.res = {}

    def sem_names(self):
        return ["c_" + e for e in self.cnt] + ["d_" + k for k in self.dcnt]


def build_program(NPRE, NMAIN, TB, debug=False):
    nc = bass.Bass("TRN2", target_bir_lowering=False)
    NT = NPRE + NMAIN
    NB = NMAIN // TB
    D = 1024

    def din(name, shape, dt=F32):
        return nc.dram_tensor(name, list(shape), dt, kind="ExternalInput").ap()

    xa = din("xa", [NT * 128, D])
    w_in = din("w_in", [D, 3600])
    glowT = din("glowT", [16, D])
    w_up = din("w_up", [16, 256])
    cols = din("cols", [128, 16])
    g1r_d = din("g1r", [128, D])
    g2r_d = din("g2r", [128, D])
    gfr_d = din("gfr", [128, D])
    w_out = din("w_out", [D, D])
    w_q = din("w_q", [D, 2048])
    kt_d = din("kt", [128, 2048])
    uT = din("uT", [D, 16384])
    v_d = din("v", [16384, D])
    cst_d = din("cst", [128, 770])
    out_d = nc.dram_tensor("out", [NMAIN * 128, D], F32, kind="ExternalOutput").ap()
    hs = nc.dram_tensor("hs", [NMAIN, 128, D], F32).ap()
    hnTs = nc.dram_tensor("hnTs", [NMAIN, 128, D], BF16).ap()
    ABs = nc.dram_tensor("ABs", [NMAIN, 128, 2048], F32).ap()
    ths = nc.dram_tensor("ths", [NMAIN, 128, 8], F32).ap()
    dbg = {}
    if debug:
        dbg["h"] = nc.dram_tensor("dbg_h", [NMAIN * 128, D], F32, kind="ExternalOutput").ap()

    S = Sched()
    stack = ExitStack()
    NW = 53200
    POOL = stack.enter_context(nc.sbuf_tensor("pool", [128, NW], F32))
    PS = stack.enter_context(nc.psum_tensor("ps", [128, 8, 512], F32))
    off = [0]

    def alloc(n, dt=F32, shape=None):
        nw = n if dt == F32 else (n + 1) // 2
        a = POOL[:, off[0]:off[0] + nw]
        off[0] += nw
        assert off[0] <= NW, ("sbuf overflow", off[0])
        if dt == BF16:
            a = a.bitcast(BF16)
        if shape is not None:
            names = " ".join("abc"[: len(shape)])
            kw = {"abc"[i]: shape[i] for i in range(len(shape) - 1)}
            a = a.rearrange("p (%s) -> p %s" % (names, names), **kw)
        return a

    def bank(k, dt=F32, shape=None):
        a = PS[:, k, :]
        if dt == BF16:
            a = a.bitcast(BF16)
        if shape is not None:
            names = " ".join("abc"[: len(shape)])
            kw = {"abc"[i]: shape[i] for i in range(len(shape) - 1)}
            a = a.rearrange("p (%s) -> p %s" % (names, names), **kw)
        return a

    def MM(out, lhsT, rhs, start, stop, reads, writes, sig):
        S.op("pe", lambda e: e.matmul(out, lhsT=lhsT, rhs=rhs, start=start, stop=stop), reads, writes, sig)

    def ACT(out, in_, func, reads, writes, bias=None, scale=None):
        kw = {}
        if bias is not None:
            kw["bias"] = bias
        if scale is not None:
            kw["scale"] = scale
        S.op("act", lambda e: e.activation(out=out, in_=in_, func=func, **kw), reads, writes)

    def TT(out, in0, in1, op, reads, writes, eng="dve"):
        S.op(eng, lambda e: e.tensor_tensor(out=out, in0=in0, in1=in1, op=op), reads, writes)

    def TS(out, in0, s1, s2, op0, op1, reads, writes):
        if op1 is None:
            S.op("dve", lambda e: e.tensor_scalar(out=out, in0=in0, scalar1=s1, scalar2=None, op0=op0), reads, writes)
        else:
            S.op("dve", lambda e: e.tensor_scalar(out=out, in0=in0, scalar1=s1, scalar2=s2, op0=op0, op1=op1), reads, writes)

    def STT(out, in0, scalar, in1, op0, op1, reads, writes):
        S.op("dve", lambda e: e.scalar_tensor_tensor(out=out, in0=in0, scalar=scalar, in1=in1, op0=op0, op1=op1), reads, writes)

    def CP(eng, out, in_, reads, writes):
        if eng == "act":
            S.op("act", lambda e: e.copy(out=out, in_=in_), reads, writes)
        else:
            S.op(eng, lambda e: e.tensor_copy(out, in_), reads, writes)

    def DMA(eng, out, in_, key, reads, writes):
        S.dma(eng, lambda e: e.dma_start(out=out, in_=in_), key, reads, writes)

    def bc(ap, shape, axis):
        return ap.unsqueeze(axis).to_broadcast(shape)

    def emit():
        sems = {n: stack.enter_context(nc.semaphore(n)) for n in S.sem_names()}
        with stack:
            with nc.Block() as block:
                def replay(name, e):
                    for it in S.q[name]:
                        if it[0] == "wait":
                            e.wait_ge(sems[it[1]], it[2])
                        else:
                            ins = it[1](e)
                            if it[2] is not None:
                                ins.then_inc(sems[it[2]], 16 if it[0] == "dma" else 1)

                @block.tensor
                def _(e):
                    replay("pe", e)

                @block.scalar
                def _(e):
                    replay("act", e)

                @block.vector
                def _(e):
                    replay("dve", e)

                @block.gpsimd
                def _(e):
                    replay("pool", e)

                @block.sync
                def _(e):
                    replay("sp", e)
        return nc

    cst = alloc(770)
    identb = alloc(128, BF16)
    colst = alloc(16)
    small = alloc(16)
    DMA("sp", cst, cst_d[:, :], "c0", [], ["cst"])
    DMA("pool", identb, cst_d[:, 0:128], "c1", [], ["identb"])
    DMA("sp", colst, cols[:, :], "c2", [], ["colst"])
    maskT = cst[:, 128:256]
    rpat = cst[:, 256:768]
    rm = cst[:, 768:770]
    S.op("dve", lambda e: e.memset(small[:, 6:7], 1.0), [], ["small"])
    S.op("dve", lambda e: e.memset(small[:, 7:8], EPS), ["small"], ["small"])
    onec = small[:, 6:7]
    TT(small[:, 8:12], colst[:, 4:8], colst[:, 0:4], ALU.subtract, ["colst", "small"], ["small"])
    ACT(small[:, 0:4], small[:, 8:12], AF.Sigmoid, ["small"], ["small"])
    TS(small[:, 4:6], colst[:, 8:10], -1.0, None, ALU.mult, None, ["colst", "small"], ["small"])
    omlc = small[:, 0:4]
    nbc = small[:, 4:6]
    gnc = colst[:, 10:12]
    base_off = off[0]

    def rstd_of(src, rs, n, tag, sq, st):
        TT(sq[:, 0:n], src, src, ALU.mult, [rs], ["sq"])
        S.op("dve", lambda e: e.reduce_sum(out=st[:, 1:2], in_=sq[:, 0:n], axis=AX.X), ["sq"], [tag])
        TS(st[:, 2:3], st[:, 1:2], 1.0 / n, small[:, 7:8] if False else EPS, ALU.mult, ALU.add, [tag], [tag])
        ACT(st[:, 3:4], st[:, 2:3], AF.Sqrt, [tag], [tag])
        S.op("dve", lambda e: e.reciprocal(out=st[:, 0:1], in_=st[:, 3:4]), [tag], [tag])

    win = alloc(8 * 3600, BF16, [8, 3600])
    wz = alloc(8 * 256, BF16, [8, 256])
    wout = alloc(8 * 1024, BF16, [8, 1024])
    g1r = alloc(1024)
    S32 = alloc(1024, F32, [8, 128])
    Sbf = alloc(1024, BF16, [8, 128])
    glT = alloc(1024)
    wupt = alloc(256)
    xt = [alloc(1024), alloc(1024)]
    sq = alloc(1024)
    st1 = alloc(8)
    st8 = alloc(40, F32, [5, 8])
    xn = alloc(1024, BF16)
    tT = alloc(1024, BF16, [8, 128])
    V = alloc(1024, BF16)
    gsil = alloc(1024)
    nsig = alloc(512, F32, [4, 128])
    kk = alloc(512, F32, [4, 128])
    lf = alloc(768, F32, [6, 128])
    cum = alloc(768, F32, [6, 128])
    ecum = alloc(768, F32, [6, 128])
    encum = alloc(768, F32, [6, 128])
    ez = alloc(256, F32, [2, 128])
    qtilT = alloc(768, BF16, [6, 128])
    ktilT = alloc(768, BF16, [6, 128])
    ktok = alloc(768, BF16, [6, 128])
    eclb = alloc(8)
    sT = alloc(1024, BF16, [8, 128])
    mixed = alloc(1024, BF16)
    qg = alloc(512, BF16, [4, 128])

    w_in_v = w_in.rearrange("(k p) c -> p k c", p=128)
    for k in range(8):
        DMA("pool", win[:, k, :], w_in_v[:, k, :], "w%d" % (k % 4), [], ["win"])
    DMA("pool", wout, w_out.rearrange("(k p) c -> p k c", p=128), "w0", [], ["wout"])
    DMA("sp", g1r, g1r_d[:, :], "c0", [], ["g1r"])
    DMA("sp", glT[0:16, :], glowT[:, :], "c2", [], ["glT"])
    DMA("sp", wupt[0:16, :], w_up[:, :], "c2", [], ["wupt"])
    for k in range(8):
        TS(wout[:, k, :], wout[:, k, :], gnc[:, (k // 4):(k // 4) + 1], None, ALU.mult, None, ["wout", "colst"], ["wout"])
    for k in range(8):
        b = bank(k // 2, F32, [2, 256])
        MM(b[:, k % 2, :], glT[0:16, k * 128:(k + 1) * 128], wupt[0:16, :], True, True,
           ["glT", "wupt"], ["ps%d" % (k // 2)], True)
    for kb in range(4):
        CP("dve", wz[:, 2 * kb:2 * kb + 2, :], bank(kb, F32, [2, 256]), ["ps%d" % kb], ["wz"])
    S.op("dve", lambda e: e.memset(S32, 0.0), [], ["S32"])
    S.op("dve", lambda e: e.memset(Sbf, 0.0), [], ["Sbf"])

    C_HQ, C_HF, C_HI, C_HG, C_GQ, C_GK, C_GV, C_GG = 0, 512, 1024, 1536, 2048, 2304, 2560, 3088

    def fm_block(dst, wsrc, col, wres, pres, last):
        for k in range(8):
            MM(dst, wsrc[:, k, col:col + 128], tT[:, k, :], k == 0, k == 7, [wres, "tT"], [pres], (k == 7) and last)

    def tm_block(bk, col, pres):
        for k in range(8):
            MM(bank(bk), tT[:, k, :], win[:, k, col:col + 512], k == 0, k == 7, ["win", "tT"], [pres], k == 7)

    if STOP == 0:
        S.barrier()
        return emit()
    for c in range(NT):
        main = c >= NPRE
        x_ = xt[c % 2]
        xr = "xt%d" % (c % 2)
        DMA("sp", x_, xa[c * 128:(c + 1) * 128, :], "x%d" % (c % 2), [], [xr])
        rstd_of(x_, xr, 1024, "st1", sq, st1)
        STT(xn, x_, st1[:, 0:1], g1r, ALU.mult, ALU.mult, [xr, "st1", "g1r"], ["xn"])
        for k in range(8):
            S.op("pe", lambda e, k=k: e.transpose(bank(0, BF16, [8, 128])[:, k, :], xn[:, k * 128:(k + 1) * 128], identb),
                 ["xn", "identb"], ["ps0"], k == 7)
        CP("act", tT, bank(0, BF16, [8, 128]), ["ps0"], ["tT"])
        if LIM == 1:
            continue
        tm_block(1, C_HI, "ps1")
        tm_block(2, C_GV, "ps2")
        CP("act", V[:, 0:512], bank(1), ["ps1"], ["V"])
        CP("act", V[:, 512:1024], bank(2), ["ps2"], ["V"])
        if main:
            tm_block(3, C_HG, "ps3")
            tm_block(4, C_GG, "ps4")
            ACT(gsil[:, 0:512], bank(3), AF.Silu, ["ps3"], ["gsil"])
            ACT(gsil[:, 512:1024], bank(4), AF.Silu, ["ps4"], ["gsil"])
        if LIM == 2:
            continue
        b5 = bank(5, F32, [4, 128])
        b6 = bank(6, F32, [4, 128])
        b7 = bank(7, F32, [4, 128])
        b1 = bank(1, F32, [4, 128])
        for b in range(4):
            fm_block(b5[:, b, :], win, C_HF + b * 128, "win", "ps5", b == 3)
        for b in range(2):
            fm_block(b6[:, b, :], win, C_GK + b * 128, "win", "ps6", (b == 1) and not main)
        for b in range(2):
            fm_block(b7[:, b, :], wz, b * 128, "wz", "ps7", b == 1)
        if main:
            for b in range(2):
                fm_block(b6[:, 2 + b, :], win, C_GQ + b * 128, "win", "ps6", b == 1)
            for b in range(4):
                fm_block(b1[:, b, :], win, C_HQ + b * 128, "win", "ps1", b == 3)
        if LIM == 3:
            continue
        ACT(nsig, b5, AF.Sigmoid, ["ps5"], ["nsig"], scale=-1.0)
        TT(kk, nsig, bc(omlc, [128, 4, 128], 2), ALU.mult, ["nsig", "small"], ["kk"])
        ACT(lf[:, 0:4, :], kk, AF.Ln, ["kk", "small"], ["lf"], bias=onec, scale=-1.0)
        for b in range(2):
            ACT(ez[:, b, :], b7[:, b, :], AF.Exp, ["ps7", "small"], ["ez"], bias=nbc[:, b:b + 1], scale=-1.0)
        ACT(lf[:, 4:6, :], ez, AF.Ln, ["ez", "small"], ["lf"], bias=onec, scale=1.0)
        lf2 = lf.rearrange("p a b -> p (a b)")
        cum2 = cum.rearrange("p a b -> p (a b)")
        S.op("dve", lambda e: e.tensor_tensor_scan(out=cum2[:, 0:512], data0=rpat, data1=lf2[:, 0:512], initial=0.0,
                                                   op0=ALU.mult, op1=ALU.add), ["lf", "cst"], ["cum"])
        S.op("dve", lambda e: e.tensor_tensor_scan(out=cum2[:, 512:768], data0=rpat[:, 0:256], data1=lf2[:, 512:768],
                                                   initial=0.0, op0=ALU.mult, op1=ALU.add), ["lf", "cst", "cum"], ["cum"])
        ACT(ecum[:, 0:4, :], cum[:, 0:4, :], AF.Exp, ["cum"], ["ecum"])
        ACT(encum[:, 0:4, :], cum[:, 0:4, :], AF.Exp, ["cum"], ["encum"], scale=-1.0)
        ACT(ecum[:, 4:6, :], cum[:, 4:6, :], AF.Exp, ["cum", "ecum"], ["ecum"], scale=-1.0 / 16.0)
        ACT(encum[:, 4:6, :], cum[:, 4:6, :], AF.Exp, ["cum", "encum"], ["encum"], scale=1.0 / 16.0)
        TT(ktilT[:, 0:4, :], kk, encum[:, 0:4, :], ALU.mult, ["kk", "encum"], ["ktilT"])
        TT(ktilT[:, 4:6, :], b6[:, 0:2, :], encum[:, 4:6, :], ALU.mult, ["ps6", "encum", "ktilT"], ["ktilT"])
        CP("dve", eclb[:, 0:4], ecum[:, 0:4, 127], ["ecum"], ["eclb"])
        CP("dve", eclb[:, 4:8].rearrange("p (a b) -> p a b", a=2), bc(ecum[:, 4:6, 127], [128, 2, 2], 2), ["ecum", "eclb"], ["eclb"])
        if main:
            ACT(nsig, b1, AF.Silu, ["ps1", "kk"], ["nsig"])
            TT(qtilT[:, 0:4, :], nsig, ecum[:, 0:4, :], ALU.mult, ["nsig", "ecum"], ["qtilT"])
            STT(qtilT[:, 4:6, :], b6[:, 2:4, :], 0.125, ecum[:, 4:6, :], ALU.mult, ALU.mult, ["ps6", "ecum", "qtilT"], ["qtilT"])
        if LIM == 4:
            continue
        b2b = bank(2, BF16, [8, 128])
        for b in range(6):
            S.op("pe", lambda e, b=b: e.transpose(b2b[:, b, :], ktilT[:, b, :], identb), ["ktilT", "identb"], ["ps2"], b == 5)
        CP("act", ktok, b2b[:, 0:6, :], ["ps2"], ["ktok"])
        if LIM == 5:
            continue
        if main:
            b3 = bank(3, F32, [4, 128])
            b4 = bank(4, F32, [4, 128])
            for b in range(4):
                MM(b3[:, b, :], ktilT[:, b, :], qtilT[:, b, :], True, True, ["ktilT", "qtilT"], ["ps3"], b == 3)
            for g in range(4):
                TS(qg[:, g, :], qtilT[:, 4 + g // 2, :], rm[:, (g % 2):(g % 2) + 1], None, ALU.mult, None,
                   ["qtilT", "cst", "qg"], ["qg"])
            for g in range(4):
                MM(b4[:, g, :], ktilT[:, 4 + g // 2, :], qg[:, g, :], True, True, ["ktilT", "qg"], ["ps4"], g == 3)
            TT(sT[:, 0:4, :], b3, bc(maskT, [128, 4, 128], 1), ALU.mult, ["ps3", "cst"], ["sT"])
            TT(sT[:, 4:8, :], b4, bc(maskT, [128, 4, 128], 1), ALU.mult, ["ps4", "cst", "sT"], ["sT"])
            if LIM == 55:
                continue
            for hh in range(8):
                ob = (b5 if hh < 4 else b6)[:, hh % 4, :]
                pres = "ps5" if hh < 4 else "ps6"
                MM(ob, sT[:, hh, :], V[:, hh * 128:(hh + 1) * 128], True, False, ["sT", "V"], [pres], False)
                if hh < 4:
                    MM(ob, qtilT[:, hh, :], Sbf[:, hh, :], False, True, ["qtilT", "Sbf"], [pres], hh == 3)
                else:
                    g = hh - 4
                    MM(ob, qg[:, g, :], Sbf[:, hh, :], False, True, ["qg", "Sbf"], [pres], hh == 7)
        if LIM == 6:
            continue
        b0 = bank(0, F32, [4, 128])
        for hh in range(8):
            ub = (b7 if hh < 4 else b0)[:, hh % 4, :]
            pres = "ps7" if hh < 4 else "ps0"
            kb = hh if hh < 4 else 4 + (hh - 4) // 2
            MM(ub, ktok[:, kb, :], V[:, hh * 128:(hh + 1) * 128], True, True, ["ktok", "V"], [pres], hh in (3, 7))
        Us = sq.rearrange("p (a b) -> p a b", a=8)
        TT(Us[:, 0:4, :], b7, bc(eclb[:, 0:4], [128, 4, 128], 2), ALU.mult, ["ps7", "eclb"], ["sq"])
        TT(Us[:, 4:8, :], b0, bc(eclb[:, 4:8], [128, 4, 128], 2), ALU.mult, ["ps0", "eclb", "sq"], ["sq"])
        TT(S32, S32, bc(eclb[:, 0:8], [128, 8, 128], 2), ALU.mult, ["S32", "eclb"], ["S32"])
        TT(S32, S32, Us, ALU.add, ["S32", "sq"], ["S32"])
        CP("act", Sbf, S32, ["S32"], ["Sbf"])
        if LIM == 7:
            continue
        if main:
            sq3 = sq.rearrange("p (a b) -> p a b", a=8)
            ACT(sq3[:, 0:4, :], b5, AF.Square, ["ps5"], ["sq"])
            ACT(sq3[:, 4:8, :], b6, AF.Square, ["ps6", "sq"], ["sq"])
            S.op("dve", lambda e: e.reduce_sum(out=st8[:, 0, :], in_=sq3, axis=AX.X), ["sq"], ["st8"])
            TS(st8[:, 1, :], st8[:, 0, :], 1.0 / 128.0, EPS, ALU.mult, ALU.add, ["st8"], ["st8"])
            ACT(st8[:, 2, :], st8[:, 1, :], AF.Sqrt, ["st8"], ["st8"])
            S.op("dve", lambda e: e.reciprocal(out=st8[:, 3, :], in_=st8[:, 2, :]), ["st8"], ["st8"])
            TT(sq3[:, 0:4, :], b5, bc(st8[:, 3, 0:4], [128, 4, 128], 2), ALU.mult, ["ps5", "st8", "sq"], ["sq"])
            TT(sq3[:, 4:8, :], b6, bc(st8[:, 3, 4:8], [128, 4, 128], 2), ALU.mult, ["ps6", "st8", "sq"], ["sq"])
            TT(mixed, sq, gsil, ALU.mult, ["sq", "gsil"], ["mixed"])
            b1b = bank(1, BF16, [8, 128])
            for k in range(8):
                S.op("pe", lambda e, k=k: e.transpose(b1b[:, k, :], mixed[:, k * 128:(k + 1) * 128], identb),
                     ["mixed", "identb"], ["ps1"], k == 7)
            CP("act", tT, b1b, ["ps1"], ["tT"])
            for hf_ in range(2):
                for k in range(8):
                    MM(bank(2 + hf_), tT[:, k, :], wout[:, k, hf_ * 512:(hf_ + 1) * 512], k == 0, k == 7,
                       ["tT", "wout"], ["ps%d" % (2 + hf_)], k == 7)
            TT(x_[:, 0:512], x_[:, 0:512], bank(2), ALU.add, [xr, "ps2"], [xr])
            TT(x_[:, 512:1024], x_[:, 512:1024], bank(3), ALU.add, [xr, "ps3"], [xr])
            t = c - NPRE
            DMA("sp", hs[t], x_, "hs", [xr], [])
            if debug:
                DMA("sp", dbg["h"][t * 128:(t + 1) * 128, :], x_, "dbgh", [xr], [])

    S.barrier()
    if STOP == 1:
        return emit()
    off[0] = base_off
    wq = alloc(8 * 2048, BF16, [8, 2048])
    kt = alloc(2048, BF16, [16, 128])
    g2r = alloc(1024)
    hl = [alloc(1024), alloc(1024)]
    sq = alloc(1024)
    st1 = alloc(8)
    hn = alloc(1024, BF16)
    hnT = alloc(1024, BF16, [8, 128])
    qT = alloc(2048, BF16, [16, 128])
    ssb = alloc(2048, F32, [16, 128])
    tmpA = alloc(2048, F32, [16, 128])
    m16 = alloc(256, F32, [16, 16])
    candA = alloc(2048, F32, [8, 256])
    ctmpA = alloc(2048, F32, [8, 256])
    c16 = alloc(128, F32, [8, 16])
    e16 = alloc(128, F32, [8, 16])
    zz = alloc(48, F32, [6, 8])
    w_q_v = w_q.rearrange("(k p) c -> p k c", p=128)
    for k in range(8):
        DMA("pool", wq[:, k, :], w_q_v[:, k, :], "w%d" % (k % 4), [], ["wq"])
    DMA("pool", kt.rearrange("p a b -> p (a b)"), kt_d[:, :], "w0", [], ["kt"])
    DMA("sp", g2r, g2r_d[:, :], "c0", [], ["g2r"])
    for t in range(NMAIN):
        h_ = hl[t % 2]
        hr = "hl%d" % (t % 2)
        DMA("sp", h_, hs[t], "x%d" % (t % 2), [], [hr])
        rstd_of(h_, hr, 1024, "st1", sq, st1)
        STT(hn, h_, st1[:, 0:1], g2r, ALU.mult, ALU.mult, [hr, "st1", "g2r"], ["hn"])
        b0b = bank(0, BF16, [8, 128])
        for k in range(8):
            S.op("pe", lambda e, k=k: e.transpose(b0b[:, k, :], hn[:, k * 128:(k + 1) * 128], identb),
                 ["hn", "identb"], ["ps0"], k == 7)
        CP("act", hnT, b0b, ["ps0"], ["hnT"])
        DMA("sp", hnTs[t], hnT.rearrange("p a b -> p (a b)"), "hnTs", ["hnT"], [])
        for cb in range(16):
            bk = 1 + cb // 4
            dst = bank(bk, F32, [4, 128])[:, cb % 4, :]
            for k in range(8):
                MM(dst, wq[:, k, cb * 128:(cb + 1) * 128], hnT[:, k, :], k == 0, k == 7, ["wq", "hnT"], ["ps%d" % bk],
                   (k == 7) and (cb % 4 == 3))
        for q4 in range(4):
            CP("act", qT[:, 4 * q4:4 * q4 + 4, :], bank(1 + q4, F32, [4, 128]), ["ps%d" % (1 + q4)], ["qT"])
        sbk = [5, 6, 7, 0]
        for cb in range(16):
            bk = sbk[cb // 4]
            MM(bank(bk, F32, [4, 128])[:, cb % 4, :], qT[:, cb, :], kt[:, cb, :], True, True, ["qT", "kt"], ["ps%d" % bk],
               cb % 4 == 3)
        for q4 in range(4):
            CP("act", ssb[:, 4 * q4:4 * q4 + 4, :], bank(sbk[q4], F32, [4, 128]), ["ps%d" % sbk[q4]], ["ssb"])
        for cb in range(16):
            S.op("dve", lambda e, cb=cb: e.max(out=m16[:, cb, 0:8], in_=ssb[:, cb, :]), ["ssb"], ["m16a%d" % cb])
        for cb in range(16):
            S.op("dve", lambda e, cb=cb: e.match_replace(out=tmpA[:, cb, :], in_to_replace=m16[:, cb, 0:8],
                                                         in_values=ssb[:, cb, :], imm_value=NEG),
                 ["ssb", "m16a%d" % cb], ["tmp%d" % cb])
        for cb in range(16):
            S.op("dve", lambda e, cb=cb: e.max(out=m16[:, cb, 8:16], in_=tmpA[:, cb, :]), ["tmp%d" % cb], ["m16b%d" % cb])
        for h in range(8):
            TT(candA[:, h, :].rearrange("p (a b) -> p a b", a=16), bc(m16[:, 2 * h, :], [128, 16, 16], 2),
               bc(m16[:, 2 * h + 1, :], [128, 16, 16], 1), ALU.add,
               ["m16a%d" % (2 * h), "m16b%d" % (2 * h), "m16a%d" % (2 * h + 1), "m16b%d" % (2 * h + 1)], ["cand%d" % h])
        for h in range(8):
            S.op("dve", lambda e, h=h: e.max(out=c16[:, h, 0:8], in_=candA[:, h, :]), ["cand%d" % h], ["c16a%d" % h])
        for h in range(8):
            S.op("dve", lambda e, h=h: e.match_replace(out=ctmpA[:, h, :], in_to_replace=c16[:, h, 0:8],
                                                       in_values=candA[:, h, :], imm_value=NEG),
                 ["cand%d" % h, "c16a%d" % h], ["ctmp%d" % h])
        for h in range(8):
            S.op("dve", lambda e, h=h: e.max(out=c16[:, h, 8:16], in_=ctmpA[:, h, :]), ["ctmp%d" % h], ["c16b%d" % h])
        C16R = ["c16a%d" % h for h in range(8)] + ["c16b%d" % h for h in range(8)]
        M16R = ["m16a%d" % cb for cb in range(16)]
        TT(e16, c16, bc(c16[:, :, 0], [128, 8, 16], 2), ALU.subtract, C16R, ["e16"])
        ACT(e16, e16, AF.Exp, ["e16"], ["e16"])
        S.op("dve", lambda e: e.reduce_sum(out=zz[:, 0, :], in_=e16, axis=AX.X), ["e16"], ["zz"])
        S.op("dve", lambda e: e.reciprocal(out=zz[:, 1, :], in_=zz[:, 0, :]), ["zz"], ["zz"])
        STT(zz[:, 2, :], e16[:, :, 15], 1.0 - MARGIN, zz[:, 1, :], ALU.mult, ALU.mult, ["e16", "zz"], ["zz"])
        ACT(zz[:, 3, :], zz[:, 0, :], AF.Ln, ["zz"], ["zz"])
        TS(zz[:, 4, :], c16[:, :, 15], -MARGIN, None, ALU.add, None, C16R + ["zz"], ["zz"])
        TT(zz[:, 5, :], zz[:, 4, :], c16[:, :, 0], ALU.subtract, C16R + ["zz"], ["zz"])
        TT(zz[:, 5, :], zz[:, 5, :], zz[:, 3, :], ALU.subtract, ["zz"], ["zz"])
        if NA > 0:
            CP("dve", zz[:, 2, 0:NA], zz[:, 5, 0:NA], ["zz"], ["zz"])
        for h in range(NA):
            TS(ssb[:, 2 * h, :], ssb[:, 2 * h, :], zz[:, 4, h:h + 1], None, ALU.subtract, None, ["ssb", "zz"], ["ssb"])
        if NA < 8:
            lo = 2 * NA
            TT(ssb[:, lo:16, :], ssb[:, lo:16, :], bc(m16[:, lo:16, 0], [128, 16 - lo, 128], 2), ALU.subtract,
               ["ssb"] + M16R, ["ssb"])
            ACT(ssb[:, lo:16, :], ssb[:, lo:16, :], AF.Exp, ["ssb"], ["ssb"])
            ssb4 = ssb.rearrange("p (h two) n -> p h two n", two=2)
            TT(ssb4[:, NA:8, 0, :], ssb4[:, NA:8, 0, :], bc(zz[:, 1, NA:8], [128, 8 - NA, 128], 2), ALU.mult,
               ["ssb", "zz"], ["ssb"])
        DMA("sp", ABs[t], ssb.rearrange("p a b -> p (a b)"), "ABs", ["ssb"], [])
        DMA("sp", ths[t], zz[:, 2, :], "ths", ["zz"], [])

    S.barrier()
    if STOP == 2:
        return emit()
    off[0] = base_off
    uS = [alloc(8 * 1024, BF16, [8, 1024]) for _ in range(2)]
    vS = [alloc(8 * 1024, BF16, [8, 1024]) for _ in range(2)]
    ABb = alloc(TB * 2048, F32, [TB, 2048])
    thb = alloc(TB * 8, F32, [TB, 8])
    hnTb = alloc(TB * 1024, BF16, [TB, 8, 128])
    acc = alloc(TB * 1024, F32, [TB, 1024])
    Eb = alloc(1024, F32, [8, 128])
    Cb = [alloc(1024, F32, [8, 128]) for _ in range(3)]
    C2 = [alloc(1024, F32, [8, 128]) for _ in range(2)]
    Gh = [alloc(1024, BF16) for _ in range(16)]
    WT = [alloc(1024, BF16, [8, 128]) for _ in range(2)]
    hl = alloc(1024)
    sq = Eb.rearrange("p a b -> p (a b)")
    st1 = alloc(8)
    gfr = alloc(1024)
    DMA("sp", gfr, gfr_d[:, :], "c0", [], ["gfr"])
    uT_v = uT.rearrange("(k p) e -> p k e", p=128)
    v_v = v_d.rearrange("(g t p) d -> g p t d", t=8, p=128)
    geTa = [alloc(8 * TB * 128, BF16, [8, TB, 128]) for _ in range(2)]
    step = 0
    def emit_WT(par, tt, sl):
        gT = geTa[sl]
        pgb = [2, 3] if par == 0 else [4, 5]
        for nb in range(2):
            TT(WT[par][:, 4 * nb:4 * nb + 4, :], bank(pgb[nb], F32, [4, 128]), gT[:, 4 * nb:4 * nb + 4, tt, :], ALU.mult,
               ["ps%d" % pgb[nb], "geTa%d" % sl, "WT%d" % par], ["WT%d" % par])

    def emit_out(par, tt, sl):
        for hf_ in range(2):
            for et in range(8):
                MM(bank(6 + hf_), WT[par][:, et, :], vS[sl][:, et, hf_ * 512:(hf_ + 1) * 512], et == 0, et == 7,
                   ["WT%d" % par, "vS%d" % sl], ["ps%d" % (6 + hf_)], et == 7)

    def emit_B2(tt):
        TT(acc[:, tt, 0:512], acc[:, tt, 0:512], bank(6), ALU.add, ["acc", "ps6"], ["acc"])
        TT(acc[:, tt, 512:1024], acc[:, tt, 512:1024], bank(7), ALU.add, ["acc", "ps7"], ["acc"])

    ccnt = [0]
    q1 = []
    q2 = []

    def drain(keep1, keep2):
        while len(q1) > keep1:
            a = q1.pop(0)
            emit_WT(*a)
            while q2:
                emit_B2(q2.pop(0))
            emit_out(*a)
            q2.append(a[1])
        while len(q2) > keep2:
            emit_B2(q2.pop(0))

    for blk in range(NB):
        t0 = blk * TB
        DMA("sp", ABb, ABs[t0:t0 + TB].rearrange("t p c -> p t c"), "ldAB", [], ["ABb"])
        DMA("sp", thb, ths[t0:t0 + TB].rearrange("t p c -> p t c"), "ldth", [], ["thb"])
        DMA("sp", hnTb.rearrange("p t a b -> p t (a b)"), hnTs[t0:t0 + TB].rearrange("t p c -> p t c"), "ldhn", [], ["hnTb"])
        S.op("dve", lambda e: e.memset(acc, 0.0), [], ["acc"])
        def load_u(eg):
            sl = eg % 2
            DMA("pool", uS[sl], uT_v[:, :, eg * 1024:(eg + 1) * 1024], "u%d" % sl, [], ["uS%d" % sl])

        def load_v(eg):
            sl = eg % 2
            DMA("pool", vS[sl], v_v[eg], "v%d" % sl, [], ["vS%d" % sl])

        def actT_mm(eg, ets):
            sl = eg % 2
            for et in ets:
                bk = et % 2
                for k in range(8):
                    MM(bank(bk)[:, 0:TB * 128].rearrange("p (a b) -> p a b", a=TB), uS[sl][:, k, et * 128:(et + 1) * 128],
                       hnTb[:, :, k, :], k == 0, k == 7, ["hnTb", "uS%d" % sl], ["ps%d" % bk], k == 7)

        def actT_gelu(eg, ets):
            sl = eg % 2
            for et in ets:
                bk = et % 2
                ACT(geTa[sl][:, et, :, :], bank(bk)[:, 0:TB * 128].rearrange("p (a b) -> p a b", a=TB), AF.Gelu,
                    ["ps%d" % bk, "geTa%d" % sl], ["geTa%d" % sl])

        drain(0, 0)
        load_u(0)
        load_v(0)
        for et in range(8):
            actT_mm(0, [et])
            actT_gelu(0, [et])
        load_u(1)
        GPT = 8 // TB if TB <= 8 else 1
        for eg in range(16):
            sl = eg % 2
            for tt in range(TB):
                par = step % 2
                step += 1
                pgb = [2, 3] if par == 0 else [4, 5]
                pre = list(range(GPT * tt, GPT * tt + GPT)) if eg + 1 < 16 else []
                actT_mm(eg + 1, pre[:2])
                a_heads = list(range(NA))
                d_heads = list(range(NA, 8))
                order = []
                while a_heads or d_heads:
                    for _ in range(3):
                        if a_heads:
                            order.append(a_heads.pop(0))
                    if d_heads:
                        order.append(d_heads.pop(0))
                for h in order:
                    a_ = ABb[:, tt, (2 * h) * 128 + 8 * eg:(2 * h) * 128 + 8 * eg + 8]
                    b_ = ABb[:, tt, (2 * h + 1) * 128:(2 * h + 2) * 128]
                    gs = 8 * par + h
                    g_ = Gh[gs]
                    gr = "Gh%d" % gs
                    if h < NA:
                        cs = ccnt[0] % 3
                        c2 = ccnt[0] % 2
                        ccnt[0] += 1
                        TT(Cb[cs], bc(a_, [128, 8, 128], 2), bc(b_, [128, 8, 128], 1), ALU.add, ["ABb"], ["Cb%d" % cs])
                        S.op("act", lambda e, cs=cs, c2=c2: e.activation(out=C2[c2], in_=Cb[cs], func=AF.Prelu, alpha=1.0e6),
                             ["Cb%d" % cs], ["C2%d" % c2])
                        ACT(g_, C2[c2].rearrange("p a b -> p (a b)"), AF.Exp, ["C2%d" % c2, "thb"], [gr],
                            bias=thb[:, tt, h:h + 1])
                    else:
                        TT(Eb, bc(a_, [128, 8, 128], 2), bc(b_, [128, 8, 128], 1), ALU.mult, ["ABb"], ["sq"])
                        E2 = Eb.rearrange("p a b -> p (a b)")
                        STT(g_, E2, thb[:, tt, h:h + 1], E2, ALU.is_ge, ALU.mult, ["sq", "thb"], [gr])
                for et in range(8):
                    pg = bank(pgb[et // 4], F32, [4, 128])[:, et % 4, :]
                    for h in range(8):
                        MM(pg, Gh[8 * par + h][:, et * 128:(et + 1) * 128], identb, h == 0, h == 7,
                           ["Gh%d" % (8 * par + h), "identb"], ["ps%d" % pgb[et // 4]], (h == 7) and (et % 4 == 3))
                q1.append((par, tt, sl))
                drain(1, 1)
                actT_gelu(eg + 1, pre[:2])
                for et in pre[2:]:
                    actT_mm(eg + 1, [et])
                    actT_gelu(eg + 1, [et])
                if tt == 0:
                    if eg + 1 < 16:
                        load_v(eg + 1)
                    if eg + 2 < 16:
                        load_u(eg + 2)
        drain(0, 0)
        for tt in range(TB):
            t = t0 + tt
            DMA("sp", hl, hs[t], "x0", [], ["hl"])
            TT(hl, hl, acc[:, tt, :], ALU.add, ["hl", "acc"], ["hl"])
            rstd_of(hl, "hl", 1024, "st1", sq, st1)
            STT(hl, hl, st1[:, 0:1], gfr, ALU.mult, ALU.mult, ["hl", "st1", "gfr"], ["hl"])
            DMA("sp", out_d[t * 128:(t + 1) * 128, :], hl, "out", ["hl"], [])
    S.barrier()

    return emit()


def host_inputs(x_main, x_pre, P):
    d = dict(P)
    d["xa"] = np.ascontiguousarray(np.concatenate([x_pre, x_main], axis=0))
    return d


def prep_params(norm1_g, w_in, hg_lower_logits, hg_norm_g, gla_w_gate_up, gla_b_gate, gla_norm_g, w_out, norm2_g,
                peer_w_q, peer_sub_keys, peer_u, peer_v, norm_f_g):
    f = np.float32
    cols = np.zeros((128, 16), f)
    cols[:, 0:4] = hg_lower_logits[0].reshape(4, 128).T
    cols[:, 4:8] = hg_lower_logits[1].reshape(4, 128).T
    cols[:, 8:10] = gla_b_gate[0].reshape(2, 128).T
    cols[:, 10] = hg_norm_g[0]
    cols[:, 11] = gla_norm_g[0]
    cst = np.zeros((128, 770), f)
    cst[0:64, 768] = 1.0
    cst[64:128, 769] = 1.0
    cst[:, 0:128] = np.eye(128, dtype=f)
    cst[:, 128:256] = np.triu(np.ones((128, 128), f))
    rp = np.ones((128, 512), f)
    rp[:, 0::128] = 0.0
    cst[:, 256:768] = rp
    rep = lambda g: np.ascontiguousarray(np.broadcast_to(g.reshape(1, -1), (128, g.size))).astype(f)
    kt = np.ascontiguousarray(peer_sub_keys[0].reshape(16, 128, 128).transpose(2, 0, 1).reshape(128, 2048))
    return {
        "w_in": np.ascontiguousarray(w_in[0]),
        "glowT": np.ascontiguousarray(w_in[0][:, 3072:3088].T),
        "w_up": np.ascontiguousarray(gla_w_gate_up[0]),
        "cols": cols,
        "g1r": rep(norm1_g[0]), "g2r": rep(norm2_g[0]), "gfr": rep(norm_f_g),
        "w_out": np.ascontiguousarray(w_out[0]),
        "w_q": np.ascontiguousarray(peer_w_q[0]),
        "kt": kt,
        "uT": np.ascontiguousarray(peer_u[0].T),
        "v": np.ascontiguousarray(peer_v[0]),
        "cst": cst,
    }


def kernel(x, norm1_g, w_in, hg_lower_logits, hg_norm_g, gla_w_gate_up, gla_b_gate, gla_norm_g, w_out, norm2_g,
           peer_w_q, peer_sub_keys, peer_u, peer_v, norm_f_g):
    args = [np.asarray(a, dtype=np.float32) for a in (norm1_g, w_in, hg_lower_logits, hg_norm_g, gla_w_gate_up,
            gla_b_gate, gla_norm_g, w_out, norm2_g, peer_w_q, peer_sub_keys, peer_u, peer_v, norm_f_g)]
    x = np.asarray(x, dtype=np.float32)
    P = prep_params(*args)
    B, T, D = x.shape
    half = T // 2
    in_maps = []
    for c in range(8):
        b, hf = c // 2, c % 2
        xm = x[b, hf * half:(hf + 1) * half]
        xp = x[b, 0:half] if hf == 1 else np.zeros((half, D), np.float32)
        in_maps.append(host_inputs(xm, xp, P))
    nc = build_program(32, 32, 4)
    res = run_bass_kernel_spmd(nc, in_maps, core_ids=list(range(8)))
    out = np.empty((B, T, D), np.float32)
    for c in range(8):
        b, hf = c // 2, c % 2
        out[b, hf * half:(hf + 1) * half] = res.results[c]["out"]
    return out
```
